# Optimizing a Trainium2 kernel written in Bass

```python
import math
import jax, jax.numpy as jnp
from jax import lax
import numpy as np

D_MODEL = 4096
BATCH = 2
SEQ = 4096
DEPTH = 4

MIX_WIDTH = D_MODEL
SSM_WIDTH = MIX_WIDTH // 2
ATTN_WIDTH = MIX_WIDTH - SSM_WIDTH
SSM_GROUP = 16
SSM_GROUPS = SSM_WIDTH // SSM_GROUP
SSM_STATE = 64
HEAD_DIM = 64
N_Q_HEADS = ATTN_WIDTH // HEAD_DIM
N_KV_HEADS = max(N_Q_HEADS // 8, 1)
GQA_GROUP = N_Q_HEADS // N_KV_HEADS
KV_WIDTH = N_KV_HEADS * HEAD_DIM
IN_WIDTH = SSM_WIDTH + ATTN_WIDTH + 2 * KV_WIDTH
WINDOW = 128
PEER_HEADS = 8
PEER_KEYS = 128
PEER_EXPERTS = PEER_KEYS * PEER_KEYS
PEER_QDIM = 128
PEER_HALF = PEER_QDIM // 2
PEER_TOPK = 16
PEER_CHUNK = 128
N_MOD = 6
EPS = 1e-6

kernel_name = "hybrid_s5_swa_peer_adaln_trunk"


def rmsnorm(x, g):
    xf = x.astype(jnp.float32)
    y = xf * lax.rsqrt(jnp.mean(xf * xf, axis=-1, keepdims=True) + EPS)
    return (y * g.astype(jnp.float32)).astype(x.dtype)


def alibi_slopes(n):
    return jnp.exp2(-8.0 * jnp.arange(1, n + 1, dtype=jnp.float32) / n)


def _ssm_combine(e1, e2):
    a1r, a1i, b1r, b1i = e1
    a2r, a2i, b2r, b2i = e2
    ar = a2r * a1r - a2i * a1i
    ai = a2r * a1i + a2i * a1r
    br = a2r * b1r - a2i * b1i + b2r
    bi = a2r * b1i + a2i * b1r + b2i
    return (ar, ai, br, bi)


def s5_mixer(u, lam_re, lam_im, log_dt, b_re, b_im, c_re, c_im, d_skip, w_glu):
    bsz, seq, _ = u.shape
    f32 = jnp.float32
    ug = u.astype(f32).reshape(bsz, seq, SSM_GROUPS, SSM_GROUP)
    dt = jnp.exp(log_dt.astype(f32))[:, None]
    lr = lam_re.astype(f32)
    li = lam_im.astype(f32)
    mag = jnp.exp(lr * dt)
    ar = mag * jnp.cos(li * dt)
    ai = mag * jnp.sin(li * dt)
    den = lr * lr + li * li
    kr = ((ar - 1.0) * lr + ai * li) / den
    ki = (ai * lr - (ar - 1.0) * li) / den
    br = b_re.astype(f32)
    bi = b_im.astype(f32)
    bbr = kr[..., None] * br - ki[..., None] * bi
    bbi = kr[..., None] * bi + ki[..., None] * br
    xr = jnp.einsum('blgh,gph->blgp', ug, bbr)
    xi = jnp.einsum('blgh,gph->blgp', ug, bbi)
    arb = jnp.broadcast_to(ar, xr.shape)
    aib = jnp.broadcast_to(ai, xr.shape)
    _, _, sr, si = lax.associative_scan(_ssm_combine, (arb, aib, xr, xi), axis=1)
    y = (jnp.einsum('blgp,ghp->blgh', sr, c_re.astype(f32))
         - jnp.einsum('blgp,ghp->blgh', si, c_im.astype(f32))
         + d_skip.astype(f32) * ug)
    g = jax.nn.gelu(y.reshape(bsz, seq, SSM_WIDTH)).astype(u.dtype)
    return g * jax.nn.sigmoid(g @ w_glu)


def swa_attention(q, k, v, q_gain, k_gain, sinks):
    bsz, seq, _ = q.shape
    nb = seq // WINDOW
    q = rmsnorm(q.reshape(bsz, seq, N_Q_HEADS, HEAD_DIM), q_gain)
    k = rmsnorm(k.reshape(bsz, seq, N_KV_HEADS, HEAD_DIM), k_gain)
    v = v.reshape(bsz, seq, N_KV_HEADS, HEAD_DIM)
    qb = q.reshape(bsz, nb, WINDOW, N_KV_HEADS, GQA_GROUP, HEAD_DIM)

    def band(t):
        tp = jnp.concatenate([jnp.zeros_like(t[:, :WINDOW]), t], axis=1)
        tp = tp.reshape(bsz, nb + 1, WINDOW, N_KV_HEADS, HEAD_DIM)
        return jnp.concatenate([tp[:, :-1], tp[:, 1:]], axis=2)

    kb = band(k)
    vb = band(v)
    scores = jnp.einsum('bnqhgd,bnkhd->bnhgqk', qb, kb,
                        preferred_element_type=jnp.float32) * (HEAD_DIM ** -0.5)
    t_loc = jnp.arange(WINDOW)[:, None]
    s_loc = jnp.arange(2 * WINDOW)[None, :]
    dist = t_loc + WINDOW - s_loc
    in_win = (dist >= 0) & (dist < WINDOW)
    key_pos = jnp.arange(nb)[:, None, None] * WINDOW + s_loc[None] - WINDOW
    valid = in_win[None] & (key_pos >= 0)
    slopes = alibi_slopes(N_Q_HEADS).reshape(N_KV_HEADS, GQA_GROUP, 1, 1)
    bias = -slopes * dist.astype(jnp.float32)
    scores = jnp.where(valid[None, :, None, None], scores + bias, -jnp.inf)
    sink = jnp.broadcast_to(
        sinks.astype(jnp.float32).reshape(1, 1, N_KV_HEADS, GQA_GROUP, 1, 1),
        scores.shape[:-1] + (1,))
    probs = jax.nn.softmax(jnp.concatenate([scores, sink], axis=-1), axis=-1)[..., :-1]
    out = jnp.einsum('bnhgqk,bnkhd->bnqhgd', probs.astype(v.dtype), vb)
    return out.reshape(bsz, seq, ATTN_WIDTH)


def peer_ffn(h, w_q, k1, k2, u_tab, v_tab):
    bsz, seq, dm = h.shape
    t = h.reshape(-1, dm)
    n_tok = t.shape[0]
    q = (t @ w_q).astype(jnp.float32).reshape(n_tok, PEER_HEADS, PEER_QDIM)
    s1 = jnp.einsum('thd,nd->thn', q[..., :PEER_HALF], k1.astype(jnp.float32))
    s2 = jnp.einsum('thd,nd->thn', q[..., PEER_HALF:], k2.astype(jnp.float32))
    v1, i1 = lax.top_k(s1, PEER_TOPK)
    v2, i2 = lax.top_k(s2, PEER_TOPK)
    cand = (v1[..., :, None] + v2[..., None, :]).reshape(n_tok, PEER_HEADS, PEER_TOPK * PEER_TOPK)
    vs, ic = lax.top_k(cand, PEER_TOPK)
    ia = jnp.take_along_axis(i1, ic // PEER_TOPK, axis=-1)
    ib = jnp.take_along_axis(i2, ic % PEER_TOPK, axis=-1)
    ids = (ia * PEER_KEYS + ib).reshape(n_tok, PEER_HEADS * PEER_TOPK)
    gates = jax.nn.softmax(vs, axis=-1).reshape(n_tok, PEER_HEADS * PEER_TOPK)
    n_chunks = n_tok // PEER_CHUNK

    def chunk_fn(args):
        tc, idc, gc = args
        act = jax.nn.gelu(jnp.einsum('cd,ckd->ck', tc, u_tab[idc]).astype(jnp.float32))
        w = (gc * act).astype(tc.dtype)
        return jnp.einsum('ck,ckd->cd', w, v_tab[idc])

    out = lax.map(chunk_fn, (t.reshape(n_chunks, PEER_CHUNK, dm),
                             ids.reshape(n_chunks, PEER_CHUNK, -1),
                             gates.reshape(n_chunks, PEER_CHUNK, -1)))
    return out.reshape(bsz, seq, dm)


def setup_inputs(seed: int = 0) -> dict:
    key = jax.random.key(seed)
    ks = jax.random.split(key, 28)
    f32 = jnp.float32

    def nrm(k, shape, scale):
        return jax.random.normal(k, shape, f32) * scale

    n_idx = jnp.arange(SSM_STATE, dtype=f32)
    return {
        "x": nrm(ks[0], (BATCH, SEQ, D_MODEL), 1.0),
        "c": nrm(ks[1], (BATCH, D_MODEL), 1.0),
        "w_ada": nrm(ks[2], (D_MODEL, N_MOD * D_MODEL), 0.5 * D_MODEL ** -0.5),
        "b_ada": nrm(ks[3], (N_MOD * D_MODEL,), 0.01),
        "ada_layer": nrm(ks[4], (DEPTH, N_MOD, D_MODEL), 0.02),
        "norm1_g": 1.0 + nrm(ks[5], (DEPTH, D_MODEL), 0.02),
        "norm2_g": 1.0 + nrm(ks[6], (DEPTH, D_MODEL), 0.02),
        "w_in": nrm(ks[7], (DEPTH, D_MODEL, IN_WIDTH), D_MODEL ** -0.5),
        "lam_re": -0.5 + nrm(ks[8], (DEPTH, SSM_GROUPS, SSM_STATE), 0.01),
        "lam_im": jnp.pi * n_idx + nrm(ks[9], (DEPTH, SSM_GROUPS, SSM_STATE), 0.01),
        "log_dt": jax.random.uniform(ks[10], (DEPTH, SSM_GROUPS), f32, math.log(1e-3), math.log(1e-1)),
        "b_re": nrm(ks[11], (DEPTH, SSM_GROUPS, SSM_STATE, SSM_GROUP), (2 * SSM_GROUP) ** -0.5),
        "b_im": nrm(ks[12], (DEPTH, SSM_GROUPS, SSM_STATE, SSM_GROUP), (2 * SSM_GROUP) ** -0.5),
        "c_re": nrm(ks[13], (DEPTH, SSM_GROUPS, SSM_GROUP, SSM_STATE), (2 * SSM_STATE) ** -0.5),
        "c_im": nrm(ks[14], (DEPTH, SSM_GROUPS, SSM_GROUP, SSM_STATE), (2 * SSM_STATE) ** -0.5),
        "d_skip": 1.0 + nrm(ks[15], (DEPTH, SSM_GROUPS, SSM_GROUP), 0.1),
        "w_glu": nrm(ks[16], (DEPTH, SSM_WIDTH, SSM_WIDTH), SSM_WIDTH ** -0.5),
        "q_gain": 1.0 + nrm(ks[17], (DEPTH, HEAD_DIM), 0.02),
        "k_gain": 1.0 + nrm(ks[18], (DEPTH, HEAD_DIM), 0.02),
        "sinks": nrm(ks[19], (DEPTH, N_Q_HEADS), 0.5),
        "gn_ssm": 1.0 + nrm(ks[20], (DEPTH, SSM_WIDTH), 0.02),
        "gn_attn": 1.0 + nrm(ks[21], (DEPTH, ATTN_WIDTH), 0.02),
        "w_out": nrm(ks[22], (DEPTH, MIX_WIDTH, D_MODEL), MIX_WIDTH ** -0.5),
        "peer_wq": nrm(ks[23], (DEPTH, D_MODEL, PEER_HEADS * PEER_QDIM), D_MODEL ** -0.5),
        "peer_k1": nrm(ks[24], (DEPTH, PEER_KEYS, PEER_HALF), PEER_HALF ** -0.5),
        "peer_k2": nrm(ks[25], (DEPTH, PEER_KEYS, PEER_HALF), PEER_HALF ** -0.5),
        "peer_u": nrm(ks[26], (DEPTH, PEER_EXPERTS, D_MODEL), D_MODEL ** -0.5),
        "peer_v": nrm(ks[27], (DEPTH, PEER_EXPERTS, D_MODEL), 0.5),
    }


def reference(x, c, w_ada, b_ada, ada_layer, norm1_g, norm2_g, w_in, lam_re, lam_im, log_dt,
              b_re, b_im, c_re, c_im, d_skip, w_glu, q_gain, k_gain, sinks, gn_ssm, gn_attn,
              w_out, peer_wq, peer_k1, peer_k2, peer_u, peer_v):
    dtype = x.dtype
    f32 = jnp.float32
    cond = (jax.nn.silu(c.astype(f32)) @ w_ada.astype(f32) + b_ada.astype(f32)).reshape(
        c.shape[0], N_MOD, D_MODEL)
    q_end = SSM_WIDTH + ATTN_WIDTH
    k_end = q_end + KV_WIDTH
    for l in range(DEPTH):
        mod = (cond + ada_layer[l].astype(f32))[:, :, None, :].astype(dtype)
        shift1, scale1, gate1, shift2, scale2, gate2 = (mod[:, i] for i in range(N_MOD))
        h = rmsnorm(x, norm1_g[l]) * (1 + scale1) + shift1
        z = h @ w_in[l]
        y_ssm = s5_mixer(z[..., :SSM_WIDTH], lam_re[l], lam_im[l], log_dt[l], b_re[l], b_im[l],
                         c_re[l], c_im[l], d_skip[l], w_glu[l])
        y_attn = swa_attention(z[..., SSM_WIDTH:q_end], z[..., q_end:k_end], z[..., k_end:],
                               q_gain[l], k_gain[l], sinks[l])
        mixed = jnp.concatenate([rmsnorm(y_ssm, gn_ssm[l]), rmsnorm(y_attn, gn_attn[l])],
                                axis=-1) @ w_out[l]
        x = x + gate1 * mixed
        h2 = rmsnorm(x, norm2_g[l]) * (1 + scale2) + shift2
        x = x + gate2 * peer_ffn(h2, peer_wq[l], peer_k1[l], peer_k2[l], peer_u[l], peer_v[l])
    return x
```

```python
import numpy as np
import concourse.bass as bass
import concourse.mybir as mybir
from concourse.bass_utils import run_bass_kernel_spmd

F32 = mybir.dt.float32
U32 = mybir.dt.uint32
AF = mybir.ActivationFunctionType
ALU = mybir.AluOpType
AX = mybir.AxisListType

D = 4096
DEPTH = 4
NG = 128
NH = 32
NKV = 4
INW = 4608
EPS = 1e-6
NEG = -30000.0


class Buf:
    __slots__ = ("w", "r", "name")

    def __init__(self, name=""):
        self.w = {}
        self.r = {}
        self.name = name


class Sched:
    ENG = ("pe", "act", "dve", "pool", "sp")

    def __init__(self, nc):
        self.nc = nc
        self.q = {e: [] for e in self.ENG}
        self.sems = {}
        self.latest = {}
        self.seen = {e: {} for e in self.ENG}
        for e in ("pe", "act", "dve", "pool"):
            self.sems[e] = nc.alloc_semaphore(name="done_" + e)
            self.latest[e] = 0
        self.bufs = []

    def buf(self, name=""):
        b = Buf(name)
        self.bufs.append(b)
        return b

    def _deps(self, reads, writes):
        deps = {}
        for b in reads:
            for k, v in b.w.items():
                if deps.get(k, 0) < v:
                    deps[k] = v
        for b in writes:
            for d in (b.w, b.r):
                for k, v in d.items():
                    if deps.get(k, 0) < v:
                        deps[k] = v
        return deps

    def _wait(self, eng, deps):
        seen = self.seen[eng]
        for k, v in deps.items():
            if k == "pe" and eng == "pe":
                continue
            if seen.get(k, 0) >= v:
                continue
            seen[k] = v
            self.q[eng].append(("wait", k, v))

    def _mark(self, tok, reads, writes):
        k, v = tok
        for b in reads:
            b.r[k] = v
        for b in writes:
            b.w = {k: v}
            b.r = {}

    def op(self, eng, fn, reads=(), writes=()):
        self._wait(eng, self._deps(reads, writes))
        self.latest[eng] += 1
        self.q[eng].append(("op", fn, eng, 1))
        self._mark((eng, self.latest[eng]), reads, writes)

    def dma(self, queue, fn, reads, writes, key):
        self._wait(queue, self._deps(reads, writes))
        if key not in self.sems:
            self.sems[key] = self.nc.alloc_semaphore(name="d_" + key)
            self.latest[key] = 0
        self.latest[key] += 16
        self.q[queue].append(("op", fn, key, 16))
        self._mark((key, self.latest[key]), reads, writes)

    def cc(self, fn, reads, writes):
        key = "cc"
        self._wait("pool", self._deps(reads, writes))
        if key not in self.sems:
            self.sems[key] = self.nc.alloc_semaphore(name="d_cc")
            self.latest[key] = 0
        self.latest[key] += 1
        self.q["pool"].append(("op", fn, key, 1))
        self._mark((key, self.latest[key]), reads, writes)

    def barrier(self):
        for e in self.ENG:
            self._wait(e, dict(self.latest))
        for b in self.bufs:
            b.w = {}
            b.r = {}

    def emit(self):
        nc = self.nc
        self.barrier()
        sems = self.sems

        def replay(name, e):
            for it in self.q[name]:
                if it[0] == "wait":
                    e.wait_ge(sems[it[1]], it[2])
                else:
                    it[1](e).then_inc(sems[it[2]], it[3])

        with nc.Block() as block:
            @block.tensor
            def _(e):
                replay("pe", e)

            @block.scalar
            def _(e):
                replay("act", e)

            @block.vector
            def _(e):
                replay("dve", e)

            @block.gpsimd
            def _(e):
                replay("pool", e)

            @block.sync
            def _(e):
                replay("sp", e)


def sb_ap(t, off, dims, np_=128, pstart=0):
    fs = 1
    for s in t.shape[1:]:
        fs *= s
    return bass.AP(t, pstart * fs + off, [[fs, np_]] + [list(d) for d in dims])


def build_program(T, depth, dbg=False, GC=4):
    nc = bass.Bass("TRN2", target_bir_lowering=False)
    S = Sched(nc)
    NTB = T // 512
    NB = T // 128
    NBL = NB // GC
    NIDX = 49
    NGL = NG // GC; NHL = NH // GC; NKL = NKV // GC
    CW = NGL * 16 + NHL * 64 + 2 * NKL * 64
    QOFF = NGL * 16; KOFF = QOFF + NHL * 64; VOFF = KOFF + NKL * 64

    def dint(name, shape, dt=F32):
        return nc.dram_tensor(name, list(shape), dt, kind="Internal").ap()

    def din(name, shape, dt=F32):
        return nc.dram_tensor(name, list(shape), dt, kind="ExternalInput").ap()

    def dscr(name, shape, dt=F32):
        return nc.dram_tensor(name, list(shape), dt, kind=("ExternalOutput" if dbg else "Internal")).ap()

    xtm_in = din("xtm_in", [NB * D, 128])
    idx_in = din("idx", [128, NBL * NIDX], U32)
    cT_in = din("cT", [128, 32])
    w_ada = din("w_ada", [D, 6 * D])
    badaT_in = din("b_adaT", [128, 192])
    adaT_in = din("adaT", [128, depth, 192])
    g1T_in = din("g1T", [128, depth, 32])
    g2T_in = din("g2T", [128, depth, 32])
    w_in = din("w_in", [depth, D, CW])
    w_glu = din("w_glu", [depth, 2048, 2048])
    w_out = din("w_out", [depth, D, D])
    w_q = din("w_q", [depth, D, 1024])
    lamS_re_in = din("lamS_re", [128, depth, NGL])
    lamS_im_in = din("lamS_im", [128, depth, NGL])
    ldtS_in = din("ldtS", [128, depth, NGL])
    lamR_re_in = din("lamR_re", [16, depth, NGL, 64])
    lamR_im_in = din("lamR_im", [16, depth, NGL, 64])
    ldtR_in = din("ldtR", [16, depth, NGL, 64])
    bT_re_in = din("bT_re", [16, depth, NGL, 64])
    bT_im_in = din("bT_im", [16, depth, NGL, 64])
    cW1_in = din("cW1", [128, depth, NGL, 16])
    cW2_in = din("cW2", [128, depth, NGL, 16])
    dT_in = din("dT", [16, depth, NGL])
    qgT_in = din("qgT", [depth, 64, 1])
    kgT_in = din("kgT", [depth, 64, 1])
    sinksB_in = din("sinksB", [128, depth, NHL])
    biasT_in = din("biasT", [128, NHL, 256])
    gnsT_in = din("gnsT", [128, depth, 16])
    gnaT_in = din("gnaT", [128, depth, 16])
    k12_in = din("k12", [128, depth, 128])
    NEXP = 16384 if dbg in (False, "full") else 128
    peer_u = [din("peer_u%d" % i, [NEXP, D]) for i in range(depth)]
    peer_v = [din("peer_v%d" % i, [NEXP, D]) for i in range(depth)]
    ident_in = din("ident", [128, 128])
    iota16_in = din("iota16", [128, 48])

    outT = nc.dram_tensor("outT", [NB * D, 128], F32, kind="ExternalOutput").ap()

    XTM = dint("XTM", [NB * D, 128])
    CCin = dint("CCin", [NBL * D, 128])
    CRg = max(1, (1 << 20) // (T * 4))
    CCg = [dint("CCg0", [GC * CRg, T]), dint("CCg1", [GC * CRg, T])]
    CCout = [dint("CCout0", [GC * (D // 2), 128]), dint("CCout1", [GC * (D // 2), 128])]
    zT = dscr("zT", [CW, T])
    gTl = dint("gTl", [NGL * 16, T])
    aTl = dint("aTl", [NHL * 64, T])
    gT = dscr("gT", [2048, T])
    aT = dscr("aT", [2048, T])
    h2tm = dscr("h2tm", [T, D])
    QTM = dscr("QTM", [NB * 1024, 128])
    dbgR = dscr("dbgR", [T // 128, 128, 1152]) if dbg else None

    def sb(name, shape, dt=F32):
        return nc.alloc_sbuf_tensor("sb_" + name, list(shape), dt)

    ident = sb("ident", [128, 128]); b_ident = S.buf()
    ones = sb("ones", [128, 128]); b_ones = S.buf()
    iota16 = sb("iota16", [128, 48]); b_iota = S.buf()
    condT = sb("condT", [128, 192]); b_cond = S.buf()
    modT = sb("modT", [128, 192]); b_mod = S.buf()
    AB = sb("AB", [128, 4, 32]); b_AB = S.buf()
    gpar = sb("gpar", [128, 2, 32]); b_gpar = S.buf()
    small = sb("small", [128, 64]); b_small = S.buf()
    BA = sb("BA", [128, 16384]); b_BA = S.buf("BA")
    BBC = sb("BBC", [128, 16384])

    class _View:
        def __init__(self, t, off):
            self.t = t; self.off = off

        def __getitem__(self, idx):
            p, c = idx
            return self.t[p, slice(c.start + self.off, c.stop + self.off)]
    BB = _View(BBC, 0); b_BB = S.buf("BB")
    BC = _View(BBC, 8192); b_BC = S.buf("BC")
    RSTD = sb("RSTD", [128, 512]); b_RSTD = S.buf("RSTD")
    k12 = sb("k12", [64, 256]); b_k12 = S.buf("k12")
    dtmp = sb("dtmp", [128, 8]); b_dtmp = S.buf("dtmp")
    IDX = sb("IDX", [128, NBL * NIDX], U32); b_IDX = S.buf("IDX")
    WT = [sb("WT0", [128, 32, 128]), sb("WT1", [128, 32, 128])]
    b_WT = [S.buf("WT0"), S.buf("WT1")]
    TM = [sb("TM%d" % i, [128, 512]) for i in range(4)]
    b_TM = [S.buf("TM%d" % i) for i in range(4)]
    PS = [nc.alloc_psum_tensor("ps%d" % i, [128, 512], F32) for i in range(8)]
    b_PS = [S.buf("ps%d" % i) for i in range(8)]
    st = {"ps": 0, "tm": 0, "wt": 0}

    def nps():
        i = st["ps"]; st["ps"] = (i + 1) % 8
        return PS[i], b_PS[i]

    def ntm():
        i = st["tm"]; st["tm"] = (i + 1) % 4
        return TM[i], b_TM[i]

    def dma(out, in_, reads, writes, key, queue="sp"):
        S.dma(queue, lambda e, o=out, i=in_: e.dma_start(out=o, in_=i), reads, writes, key)

    def act(out, in_, func, reads, writes, bias=None, scale=None, accum=None):
        kw = {}
        if bias is not None:
            kw["bias"] = bias
        if scale is not None:
            kw["scale"] = scale
        if accum is not None:
            kw["accum_out"] = accum
        S.op("act", lambda e, o=out, i=in_, f=func, kw=kw: e.activation(out=o, in_=i, func=f, **kw), reads, writes)

    def tt(out, a, b, op, reads, writes, eng="dve"):
        S.op(eng, lambda e, o=out, a=a, b=b, op=op: e.tensor_tensor(out=o, in0=a, in1=b, op=op), reads, writes)

    def ts(out, a, s1, s2, op0, op1, reads, writes, eng="dve"):
        if op1 is None:
            S.op(eng, lambda e, o=out, a=a, s1=s1, op0=op0: e.tensor_scalar(out=o, in0=a, scalar1=s1, scalar2=None, op0=op0), reads, writes)
        else:
            S.op(eng, lambda e, o=out, a=a, s1=s1, s2=s2, op0=op0, op1=op1: e.tensor_scalar(out=o, in0=a, scalar1=s1, scalar2=s2, op0=op0, op1=op1), reads, writes)

    def stt(out, a, s, b, op0, op1, reads, writes, eng="dve"):
        S.op(eng, lambda e, o=out, a=a, s=s, b=b, op0=op0, op1=op1: e.scalar_tensor_tensor(out=o, in0=a, scalar=s, in1=b, op0=op0, op1=op1), reads, writes)

    def cp(out, in_, reads, writes, eng="dve"):
        S.op(eng, lambda e, o=out, i=in_: e.tensor_copy(out=o, in_=i), reads, writes)

    def recip(out, in_, reads, writes):
        S.op("dve", lambda e, o=out, i=in_: e.reciprocal(out=o, in_=i), reads, writes)

    def mm(out, lhsT, rhs, start, stop, reads, writes):
        S.op("pe", lambda e, o=out, l=lhsT, r=rhs, s0=start, s1=stop: e.matmul(o, lhsT=l, rhs=r, start=s0, stop=s1), reads, writes)

    def tr(out, in_, idn, reads, writes):
        S.op("pe", lambda e, o=out, i=in_, d=idn: e.transpose(o, i, d), reads, writes)

    def memset(ap, val, writes, eng="dve"):
        S.op(eng, lambda e, a=ap, v=val: e.memset(a, v), [], writes)

    def rms_rstd(blk, b_blk, c0, nchunk, N, nfeat, rstd_ap, b_rstd):
        ps, bp = nps()
        for i in range(nchunk):
            sq, bsq = ntm()
            act(sq[:, 0:N], blk[:, c0 + i, 0:N], AF.Square, [b_blk], [bsq])
            mm(ps[:, 0:N], ones[:, :], sq[:, 0:N], i == 0, i == nchunk - 1, [b_ones, bsq], [bp])
        t1, bt1 = ntm()
        act(t1[:, 0:N], ps[:, 0:N], AF.Sqrt, [bp], [bt1], bias=small[:, 0:1], scale=1.0 / nfeat)
        recip(rstd_ap, t1[:, 0:N], [bt1, b_small], [b_rstd])

    def gemm(Wd, KC, ntiles, rhs_fn, rhs_bufs, N, evac):
        def load(j):
            s = st["wt"]; st["wt"] ^= 1
            dma(WT[s][:, 0:KC, :], Wd[:, j * 128:(j + 1) * 128].rearrange("(c p) n -> p c n", p=128),
                [], [b_WT[s]], "wt%d" % s)
            return s
        nxt = load(ntiles[0])
        for idx, j in enumerate(ntiles):
            s = nxt
            if idx + 1 < len(ntiles):
                nxt = load(ntiles[idx + 1])
            ps, bp = nps()
            for c in range(KC):
                mm(ps[:, 0:N], WT[s][:, c, :], rhs_fn(c), c == 0, c == KC - 1, [b_WT[s]] + rhs_bufs, [bp])
            evac(j, ps, bp)

    def xtile(T_):
        return XTM[T_ * D:(T_ + 1) * D, :].rearrange("(c p) t -> p c t", p=128)

    def xload(blk, tb, bufs, key):
        for q_ in range(4):
            dma(blk[:, :, q_ * 128:(q_ + 1) * 128], xtile(4 * tb + q_), [], bufs, key)

    def xstore(blk, tb, bufs, key):
        for q_ in range(4):
            dma(xtile(4 * tb + q_), blk[:, :, q_ * 128:(q_ + 1) * 128], bufs, [], key)

    dma(ident[:, :], ident_in, [], [b_ident], "c0")
    dma(iota16[:, :], iota16_in, [], [b_iota], "c1")
    memset(ones[:, :], 1.0, [b_ones])
    memset(small[:, 0:1], EPS, [b_small])
    memset(small[:, 1:2], float(np.pi / 2), [b_small])
    dma(XTM, xtm_in, [], [S.buf()], "xcp")
    dma(IDX[:, :], idx_in, [], [b_IDX], "c1")
    S.barrier()

    cT = BB[:, 0:32]
    dma(cT, cT_in, [], [b_BB], "ld0")
    scT = BB[:, 32:64]
    act(scT, cT, AF.Silu, [b_BB], [b_BB])
    psc, bpc = nps()

    def cond_evac(j, ps, bp):
        cp(condT[:, j:j + 1], ps[:, 0:1], [bp], [b_cond])
    gemm(w_ada, 32, list(range(192)), lambda c: BB[:, 32 + c:33 + c], [b_BB], 1, cond_evac)
    dma(BC[:, 0:192], badaT_in, [], [b_BC], "ld0")
    tt(condT[:, :], condT[:, :], BC[:, 0:192], ALU.add, [b_cond, b_BC], [b_cond])
    S.barrier()

    for l in range(depth):
        last = l == depth - 1
        dma(BC[:, 0:192], adaT_in[:, l, :], [], [b_BC], "ld0")
        dma(gpar[:, 0, :], g1T_in[:, l, :], [], [b_gpar], "ld1")
        dma(gpar[:, 1, :], g2T_in[:, l, :], [], [b_gpar], "ld1")
        tt(modT[:, :], condT[:, :], BC[:, 0:192], ALU.add, [b_cond, b_BC], [b_mod])
        stt(AB[:, 0, :], modT[:, 32:64], 1.0, gpar[:, 0, :], ALU.add, ALU.mult, [b_mod, b_gpar], [b_AB])
        cp(AB[:, 1, :], modT[:, 0:32], [b_mod], [b_AB])
        stt(AB[:, 2, :], modT[:, 128:160], 1.0, gpar[:, 1, :], ALU.add, ALU.mult, [b_mod, b_gpar], [b_AB])
        cp(AB[:, 3, :], modT[:, 96:128], [b_mod], [b_AB])
        S.barrier()

        xblk = BA[:, :].rearrange("p (c t) -> p c t", c=32)
        rstd = BB[:, 0:512]
        for tb in range(NTB):
            t0 = tb * 512
            xload(xblk, tb, [b_BA], "ldA")
            rms_rstd(xblk, b_BA, 0, 32, 512, D, rstd, b_BB)
            for c in range(32):
                tt(xblk[:, c, :], xblk[:, c, :], rstd, ALU.mult, [b_BA, b_BB], [b_BA])
                act(xblk[:, c, :], xblk[:, c, :], AF.Identity, [b_BA, b_AB], [b_BA],
                    bias=AB[:, 1, c:c + 1], scale=AB[:, 0, c:c + 1])

            def ev1(j, ps, bp, t0=t0):
                o, bo = ntm()
                act(o[:, :], ps[:, :], AF.Copy, [bp], [bo])
                dma(zT[j * 128:(j + 1) * 128, t0:t0 + 512], o[:, :], [bo], [], "stz")
            gemm(w_in[l], 32, list(range(CW // 128)), lambda c: xblk[:, c, :], [b_BA], 512, ev1)
        S.barrier()
        if dbg == "p1":
            break

        LG = T.bit_length() - 1
        CT = BA[:, 0:T]; ST_ = BA[:, 4096:4096 + T]; XT = BA[:, 8192:8192 + T]; SS = BA[:, 12288:12288 + T]
        b_TB = S.buf("tables"); b_XT = S.buf("xt"); b_SS = S.buf("ss"); b_U = S.buf("U"); b_YB = S.buf("YB")
        b_PR = S.buf("params"); b_W12 = S.buf("W12"); b_BBc = S.buf("BBc"); b_DD = S.buf("DD")
        U = BB[0:16, 0:T]; YB = BB[0:16, 4096:4096 + T]

        def P(k):
            return BC[:, k * 128:(k + 1) * 128]
        lre, lim, ldt, rr, th, cc, sn, t1_, t2_ = [P(k) for k in range(9)]
        PWc = BC[:, 2048:3584].rearrange("p (k g) -> p k g", k=12)
        PWs = BC[:, 3584:5120].rearrange("p (k g) -> p k g", k=12)
        W1 = WT[0][:, 0:16, :].rearrange("p a (b h) -> p (a b) h", h=16)
        W2 = WT[0][:, 16:32, :].rearrange("p a (b h) -> p (a b) h", h=16)
        BB1 = WT[1][0:16, 0:16, :]
        BB2 = WT[1][0:16, 16:32, :]
        Dd = BC[0:16, 5120:7168].rearrange("p (g h) -> p g h", h=16)
        dTs = BC[0:16, 7168:7296]
        dma(BC[:, 0:NGL], lamS_re_in[:, l, :], [], [b_PR], "ld0")
        dma(BC[:, 128:128 + NGL], lamS_im_in[:, l, :], [], [b_PR], "ld0")
        dma(BC[:, 256:256 + NGL], ldtS_in[:, l, :], [], [b_PR], "ld0")
        dma(W1[:, 0:NGL, :], cW1_in[:, l, :, :], [], [b_W12], "ld1")
        dma(W2[:, 0:NGL, :], cW2_in[:, l, :, :], [], [b_W12], "ld1")
        dma(BC[0:16, 7168:7168 + NGL], dT_in[:, l, :], [], [b_DD], "ld2")
        ts(WT[0][64:128, 0:16, :], WT[0][64:128, 0:16, :], -1.0, None, ALU.mult, None, [b_W12], [b_W12])
        tt(Dd, sb_ap(ident, 0, [[0, 128], [1, 16]], np_=16),
           bass.AP(BBC, 8192 + 7168, [[16384, 16], [1, 128], [0, 16]]), ALU.mult, [b_ident, b_DD], [b_DD])
        pr = [b_PR]
        act(t1_, ldt, AF.Exp, pr, pr)
        tt(t2_, lre, t1_, ALU.mult, pr, pr)
        act(rr, t2_, AF.Exp, pr, pr)
        tt(th, lim, t1_, ALU.mult, pr, pr)
        act(cc, th, AF.Sin, pr, pr, bias=small[:, 1:2], scale=-0.125)
        act(sn, th, AF.Sin, pr, pr, scale=0.125)

        def csq(c_, s_, a_, b_, bufs, eng="dve"):
            tt(a_, c_, c_, ALU.mult, bufs, bufs, eng)
            tt(b_, s_, s_, ALU.mult, bufs, bufs, eng)
            stt(s_, c_, 2.0, s_, ALU.mult, ALU.mult, bufs, bufs)
            tt(c_, a_, b_, ALU.subtract, bufs, bufs, eng)
        for _ in range(3):
            csq(cc, sn, t1_, t2_, pr)
        cp(PWc[:, 0, :], cc, pr, pr)
        ts(PWs[:, 0, :], sn, -1.0, None, ALU.mult, None, pr, pr)
        for k in range(1, LG):
            tt(t1_, PWc[:, k - 1, :], PWc[:, k - 1, :], ALU.mult, pr, pr)
            tt(t2_, PWs[:, k - 1, :], PWs[:, k - 1, :], ALU.mult, pr, pr)
            tt(PWc[:, k, :], t1_, t2_, ALU.subtract, pr, pr)
            stt(PWs[:, k, :], PWc[:, k - 1, :], 2.0, PWs[:, k - 1, :], ALU.mult, ALU.mult, pr, pr)

        for gc in range(NGL // 16):
            g0 = gc * 16
            A = [BA[0:16, k * 1024:(k + 1) * 1024] for k in range(16)]
            ba = [b_TB, b_XT, b_SS]

            def v3(a):
                return a.rearrange("p (g q) -> p g q", q=64)
            dma(v3(A[0]), lamR_re_in[:, l, g0:g0 + 16, :], [], ba, "ld0")
            dma(v3(A[1]), lamR_im_in[:, l, g0:g0 + 16, :], [], ba, "ld0")
            dma(v3(A[2]), ldtR_in[:, l, g0:g0 + 16, :], [], ba, "ld0")
            dma(v3(A[3]), bT_re_in[:, l, g0:g0 + 16, :], [], ba, "ld0")
            dma(v3(A[4]), bT_im_in[:, l, g0:g0 + 16, :], [], ba, "ld0")
            act(A[5], A[2], AF.Exp, ba, ba)
            tt(A[6], A[0], A[5], ALU.mult, ba, ba)
            act(A[6], A[6], AF.Exp, ba, ba)
            tt(A[7], A[1], A[5], ALU.mult, ba, ba)
            act(A[8], A[7], AF.Sin, ba + [b_small], ba, bias=small[0:16, 1:2], scale=-0.125)
            act(A[9], A[7], AF.Sin, ba, ba, scale=0.125)
            for _ in range(3):
                csq(A[8], A[9], A[10], A[11], ba)
            tt(A[8], A[6], A[8], ALU.mult, ba, ba)
            tt(A[9], A[6], A[9], ALU.mult, ba, ba)
            tt(A[10], A[0], A[0], ALU.mult, ba, ba)
            tt(A[11], A[1], A[1], ALU.mult, ba, ba)
            tt(A[10], A[10], A[11], ALU.add, ba, ba)
            recip(A[10], A[10], ba, ba)
            ts(A[8], A[8], -1.0, None, ALU.add, None, ba, ba)
            tt(A[11], A[8], A[0], ALU.mult, ba, ba)
            tt(A[12], A[9], A[1], ALU.mult, ba, ba)
            tt(A[11], A[11], A[12], ALU.add, ba, ba)
            tt(A[11], A[11], A[10], ALU.mult, ba, ba)
            tt(A[12], A[9], A[0], ALU.mult, ba, ba)
            tt(A[13], A[8], A[1], ALU.mult, ba, ba)
            tt(A[12], A[12], A[13], ALU.subtract, ba, ba)
            tt(A[12], A[12], A[10], ALU.mult, ba, ba)
            tt(A[13], A[11], A[3], ALU.mult, ba, ba)
            tt(A[14], A[12], A[4], ALU.mult, ba, ba)
            tt(A[13], A[13], A[14], ALU.subtract, ba, ba)
            tt(A[14], A[11], A[4], ALU.mult, ba, ba)
            tt(A[15], A[12], A[3], ALU.mult, ba, ba)
            tt(A[14], A[14], A[15], ALU.add, ba, ba)
            cp(BB1[:, :, 0:64], v3(A[13]), ba, [b_BBc])
            cp(BB1[:, :, 64:128], v3(A[14]), ba, [b_BBc])
            ts(BB2[:, :, 0:64], v3(A[14]), -1.0, None, ALU.mult, None, ba, [b_BBc])
            cp(BB2[:, :, 64:128], v3(A[13]), ba, [b_BBc])

            for gi in range(16):
                g = g0 + gi
                dma(U, zT[16 * g:16 * g + 16, 0:T], [], [b_U], "ldU")
                memset(CT[:, 0:1], 1.0, [b_TB])
                memset(ST_[:, 0:1], 0.0, [b_TB])
                for k in range(LG):
                    n = 1 << k
                    pc = PWc[:, k, g:g + 1]; ps_ = PWs[:, k, g:g + 1]
                    ta = BA[:, 8192:8192 + n]; tb_ = BA[:, 10240:10240 + n]
                    ts(ta, ST_[:, 0:n], ps_, None, ALU.mult, None, [b_TB, b_PR], [b_XT])
                    stt(CT[:, n:2 * n], CT[:, 0:n], pc, ta, ALU.mult, ALU.subtract, [b_TB, b_PR, b_XT], [b_TB])
                    ts(tb_, CT[:, 0:n], ps_, None, ALU.mult, None, [b_TB, b_PR], [b_XT])
                    stt(ST_[:, n:2 * n], ST_[:, 0:n], pc, tb_, ALU.mult, ALU.add, [b_TB, b_PR, b_XT], [b_TB])
                for i in range(NTB):
                    sl = slice(i * 512, (i + 1) * 512)
                    p1, bp1 = nps(); p2, bp2 = nps()
                    mm(p1[:, :], BB1[:, gi, :], U[:, sl], True, True, [b_BBc, b_U], [bp1])
                    mm(p2[:, :], BB2[:, gi, :], U[:, sl], True, True, [b_BBc, b_U], [bp2])
                    tmp, btmp = ntm()
                    tt(tmp[:, :], CT[:, sl], p1[:, :], ALU.mult, [b_TB, bp1], [btmp])
                    tt(XT[:, sl], ST_[:, sl], p2[:, :], ALU.mult, [b_TB, bp2], [b_XT])
                    tt(XT[:, sl], XT[:, sl], tmp[:, :], ALU.add, [b_XT, btmp], [b_XT])
                rb = bass.AP(BBC, 8192 + 3 * 128 + g, [[16384, 128], [0, T]])
                S.op("dve", lambda e, o=SS, d0=rb, d1=XT: e.tensor_tensor_scan(out=o, data0=d0, data1=d1, initial=0.0, op0=ALU.mult, op1=ALU.add),
                     [b_PR, b_XT], [b_SS])
                for i in range(NTB):
                    sl = slice(i * 512, (i + 1) * 512)
                    q1, bq1 = ntm(); q2, bq2 = ntm()
                    tt(q1[:, :], CT[:, sl], SS[:, sl], ALU.mult, [b_TB, b_SS], [bq1], "pool")
                    tt(q2[:, :], ST_[:, sl], SS[:, sl], ALU.mult, [b_TB, b_SS], [bq2], "pool")
                    py, bpy = nps()
                    mm(py[0:16, :], W1[:, g, :], q1[:, :], True, False, [b_W12, bq1], [bpy])
                    mm(py[0:16, :], W2[:, g, :], q2[:, :], False, False, [b_W12, bq2], [bpy])
                    mm(py[0:16, :], Dd[:, g, :], U[:, sl], False, True, [b_DD, b_U], [bpy])
                    act(YB[:, sl], py[0:16, :], AF.Gelu_apprx_tanh, [bpy], [b_YB])
                dma(gTl[16 * g:16 * g + 16, 0:T], YB, [b_YB], [], "stg")
        S.barrier()
        if dbg == "p2a":
            break

        b_bias = S.buf(); b_es = S.buf(); b_kh = S.buf(); b_va = S.buf(); b_qh = S.buf(); b_raw = S.buf()
        b_vr = S.buf(); b_yh = S.buf(); b_yT = S.buf(); b_g = S.buf()
        biasT = BBC[:, 8192:8192 + NHL * 256].rearrange("p (h k) -> p h k", k=256)
        khT = BB[0:64, 0:T]
        Vaug = BBC[:, 4096:4096 + NB * 65].rearrange("p (n d) -> p n d", d=65)
        qhT = BA[0:64, 0:T]; RAW = BA[0:64, 4096:4096 + T]; VR = BA[0:64, 8192:8192 + T]
        yh = BA[:, 12288:12288 + NB * 64].rearrange("p (n d) -> p n d", d=64)
        yhT = WT[0][0:64, :, :].rearrange("p a b -> p (a b)")
        esink = small[:, 16:16 + NHL]
        dma(biasT, biasT_in, [], [b_bias], "ld0")
        dma(esink, sinksB_in[:, l, :], [], [b_es], "ld1")
        act(esink, esink, AF.Exp, [b_es], [b_es])
        dma(small[0:64, 48:49], qgT_in[l], [], [b_g], "ld2")
        dma(small[0:64, 49:50], kgT_in[l], [], [b_g], "ld2")
        memset(Vaug[:, :, 64:65], 1.0, [b_va])

        def qknorm(dst, b_dst, gcol):
            for i in range(NTB):
                sl = slice(i * 512, (i + 1) * 512)
                sq, bsq = ntm()
                act(sq[0:64, :], RAW[:, sl], AF.Square, [b_raw], [bsq])
                ps, bp = nps()
                mm(ps[0:64, :], ones[0:64, 0:64], sq[0:64, :], True, True, [b_ones, bsq], [bp])
                t1, bt1 = ntm()
                act(t1[0:64, :], ps[0:64, :], AF.Sqrt, [bp, b_small], [bt1], bias=small[0:64, 0:1], scale=1.0 / 64)
                t2, bt2 = ntm()
                recip(t2[0:64, :], t1[0:64, :], [bt1], [bt2])
                stt(dst[:, sl], RAW[:, sl], small[0:64, gcol:gcol + 1], t2[0:64, :], ALU.mult, ALU.mult, [b_raw, b_g, bt2], [b_dst])

        for kv in range(NKL):
            dma(RAW, zT[KOFF + 64 * kv:KOFF + 64 * kv + 64, 0:T], [], [b_raw], "ldA")
            qknorm(khT, b_kh, 49)
            dma(VR, zT[VOFF + 64 * kv:VOFF + 64 * kv + 64, 0:T], [], [b_vr], "ldB")
            for n in range(NB):
                if n % 4 == 0:
                    pv, bpv = nps()
                tr(pv[:, (n % 4) * 64:(n % 4) * 64 + 64], VR[:, n * 128:(n + 1) * 128], ident[0:64, 0:64], [b_vr, b_ident], [bpv])
                if n % 4 == 3:
                    cp(Vaug[:, n - 3:n + 1, 0:64], pv[:, 0:256].rearrange("p (n d) -> p n d", d=64), [bpv], [b_va])
            for hq in range(8):
                h = kv * 8 + hq
                dma(RAW, zT[QOFF + 64 * h:QOFF + 64 * h + 64, 0:T], [], [b_raw], "ldA")
                qknorm(qhT, b_qh, 48)
                for n in range(NB):
                    blk = slice(n * 128, (n + 1) * 128)
                    sc, bsc = nps()
                    if n > 0:
                        mm(sc[:, 0:128], khT[:, (n - 1) * 128:n * 128], qhT[:, blk], True, True, [b_kh, b_qh], [bsc])
                    mm(sc[:, 128:256], khT[:, blk], qhT[:, blk], True, True, [b_kh, b_qh], [bsc])
                    cols = slice(0, 256) if n > 0 else slice(128, 256)
                    s_sb, bs = ntm()
                    stt(s_sb[:, cols], sc[:, cols], 0.125, biasT[:, h, cols], ALU.mult, ALU.add, [bsc, b_bias], [bs])
                    e_sb, be = ntm()
                    act(e_sb[:, cols], s_sb[:, cols], AF.Exp, [bs], [be])
                    po, bpo = nps()
                    if n > 0:
                        mm(po[:, 0:65], e_sb[:, 0:128], Vaug[:, n - 1, :], True, False, [be, b_va], [bpo])
                    mm(po[:, 0:65], e_sb[:, 128:256], Vaug[:, n, :], n == 0, True, [be, b_va], [bpo])
                    tt(dtmp[:, 0:1], po[:, 64:65], esink[:, h:h + 1], ALU.add, [bpo, b_es], [b_dtmp])
                    recip(dtmp[:, 1:2], dtmp[:, 0:1], [b_dtmp], [b_dtmp])
                    ts(yh[:, n, :], po[:, 0:64], dtmp[:, 1:2], None, ALU.mult, None, [bpo, b_dtmp], [b_yh])
                for n in range(NB):
                    if n % 4 == 0:
                        pt, bpt = nps()
                    tr(pt[0:64, (n % 4) * 128:(n % 4) * 128 + 128], yh[:, n, :], ident[:, :], [b_yh, b_ident], [bpt])
                    if n % 4 == 3:
                        act(yhT[:, (n - 3) * 128:(n + 1) * 128], pt[0:64, :], AF.Copy, [bpt], [b_yT])
                dma(aTl[64 * h:64 * h + 64, 0:T], yhT[:, 0:T], [b_yT], [], "sta")
        S.barrier()
        groups = [list(range(g_ * GC, (g_ + 1) * GC)) for g_ in range(2)]
        b_cco = [S.buf(), S.buf()]
        CR = max(1, (1 << 20) // (T * 4))
        cnt_ = 0
        for (srcT, dstT, rows_l) in ((gTl, gT, NGL * 16), (aTl, aT, NHL * 64)):
            for r0 in range(0, rows_l, CR):
                i_ = cnt_ % 2; cnt_ += 1
                co = CCg[i_]
                S.cc(lambda e, gr=groups, a=srcT[r0:r0 + CR, :], o=co: e.collective_compute("AllGather", ALU.bypass, replica_groups=gr, ins=[a], outs=[o]),
                     [], [b_cco[i_]])
                for r_ in range(GC):
                    dma(dstT[r_ * rows_l + r0:r_ * rows_l + r0 + CR, :], co[r_ * CR:(r_ + 1) * CR, :], [b_cco[i_]], [], "stcg")
        S.barrier()
        if dbg == "p2":
            break

        b_CLO = S.buf(); b_CHI = S.buf(); b_gn = S.buf()
        xblk = BA[:, :].rearrange("p (c t) -> p c t", c=32)
        cat = BBC[:, :].rearrange("p (c t) -> p c t", c=32)
        gns = small[:, 16:32]; gna = small[:, 32:48]
        dma(gns, gnsT_in[:, l, :], [], [b_gn], "ld0")
        dma(gna, gnaT_in[:, l, :], [], [b_gn], "ld0")
        for tb in range(NTB):
            t0 = tb * 512
            tsl = slice(t0, t0 + 512)
            xload(xblk, tb, [b_BA], "ldA")
            dma(cat[:, 16:32, :], gT[:, tsl].rearrange("(c p) t -> p c t", p=128), [], [b_CHI], "ldB")

            def ev_glu(j, ps, bp):
                sg, bsg = ntm()
                act(sg[:, :], ps[:, :], AF.Sigmoid, [bp], [bsg])
                tt(cat[:, j, :], cat[:, 16 + j, :], sg[:, :], ALU.mult, [b_CHI, bsg], [b_CLO])
            gemm(w_glu[l], 16, list(range(16)), lambda c: cat[:, 16 + c, :], [b_CHI], 512, ev_glu)
            dma(cat[:, 16:32, :], aT[:, tsl].rearrange("(c p) t -> p c t", p=128), [], [b_CHI], "ldB")
            rms_rstd(cat, b_CLO, 0, 16, 512, 2048, RSTD[:, :], b_RSTD)
            for c in range(16):
                stt(cat[:, c, :], cat[:, c, :], gns[:, c:c + 1], RSTD[:, :], ALU.mult, ALU.mult, [b_CLO, b_gn, b_RSTD], [b_CLO])
            rms_rstd(cat, b_CHI, 16, 16, 512, 2048, RSTD[:, :], b_RSTD)
            for c in range(16):
                stt(cat[:, 16 + c, :], cat[:, 16 + c, :], gna[:, c:c + 1], RSTD[:, :], ALU.mult, ALU.mult, [b_CHI, b_gn, b_RSTD], [b_CHI])

            def ev_out(j, ps, bp):
                stt(xblk[:, j, :], ps[:, :], modT[:, 64 + j:65 + j], xblk[:, j, :], ALU.mult, ALU.add, [bp, b_mod, b_BA], [b_BA])
            gemm(w_out[l], 32, list(range(32)), lambda c: cat[:, c, :], [b_CLO, b_CHI], 512, ev_out)
            xstore(xblk, tb, [b_BA], "stx")
            rms_rstd(xblk, b_BA, 0, 32, 512, D, RSTD[:, :], b_RSTD)
            bh = [b_CLO, b_CHI]
            for c in range(32):
                tt(cat[:, c, :], xblk[:, c, :], RSTD[:, :], ALU.mult, [b_BA, b_RSTD], bh)
                act(cat[:, c, :], cat[:, c, :], AF.Identity, bh + [b_AB], bh, bias=AB[:, 3, c:c + 1], scale=AB[:, 2, c:c + 1])

            def ev_q(j, ps, bp, t0=t0):
                o, bo = ntm()
                act(o[:, :], ps[:, :], AF.Copy, [bp], [bo])
                for q_ in range(4):
                    T_ = t0 // 128 + q_
                    dma(QTM[T_ * 1024 + j * 128:T_ * 1024 + (j + 1) * 128, :], o[:, q_ * 128:(q_ + 1) * 128], [bo], [], "stq")
            gemm(w_q[l], 32, list(range(8)), lambda c: cat[:, c, :], bh, 512, ev_q)
            for t4 in range(4):
                s_ = st["wt"]; st["wt"] ^= 1
                ROW = WT[s_][:, :, :].rearrange("p a b -> p (a b)")
                for c in range(32):
                    if c % 4 == 0:
                        pt, bpt = nps()
                    tr(pt[:, (c % 4) * 128:(c % 4) * 128 + 128], cat[:, c, t4 * 128:(t4 + 1) * 128], ident[:, :], bh + [b_ident], [bpt])
                    if c % 4 == 3:
                        act(ROW[:, (c - 3) * 128:(c + 1) * 128], pt[:, :], AF.Copy, [bpt], [b_WT[s_]])
                dma(h2tm[t0 + t4 * 128:t0 + (t4 + 1) * 128, :], ROW, [b_WT[s_]], [], "sth")
        S.barrier()
        if dbg == "p4":
            break

        b_G = [S.buf() for _ in range(4)]
        G = BA[:, :].rearrange("p (s d) -> p s d", s=4)
        h2t = BBC[:, 0:4096]; b_h2 = S.buf()
        acc = BBC[:, 4096:8192]; b_acc = S.buf()
        x1t = BA[:, 0:4096].rearrange("p (c t) -> p c t", c=32); b_x1 = b_G[0]
        RT0 = 11264
        qtile = BBC[0:64, RT0:RT0 + 2048].rearrange("p (s h t) -> p s h t", s=2, h=8); b_qt = S.buf()
        v12 = BBC[:, RT0 + 2048:RT0 + 2304]; b_v12 = S.buf()
        i12f = BBC[:, RT0 + 2304:RT0 + 2560]; b_i12f = S.buf()
        tmpS = BBC[:, RT0 + 2560:RT0 + 2688]; b_tmpS = S.buf()
        cand = BBC[:, RT0 + 2688:RT0 + 4736]; b_cand = S.buf()
        vs = BBC[:, RT0 + 4736:RT0 + 4864]; b_vs = S.buf()
        misc = BBC[:, RT0 + 4864:RT0 + 4992]; b_misc = S.buf()
        WF = WT[0][:, :, :].rearrange("p a b -> p (a b)")
        ev = WF[:, 0:128]; gates = WF[:, 128:256]; af = WF[:, 256:384]; bf = WF[:, 384:512]
        iaf = WF[:, 512:640]; ibf = WF[:, 640:768]; idsf = WF[:, 768:896]; actv = WF[:, 896:1024]; wv = WF[:, 1024:1152]
        b_r = S.buf()
        WF1 = WT[1][:, :, :].rearrange("p a b -> p (a b)")
        EQ = WF1[:, 0:2048]; EQ2 = WF1[:, 2048:4096]; b_eq = S.buf()
        i12u = WF[:, 1152:1408].bitcast(U32); b_i12u = S.buf()
        icu = WF[:, 1408:1536].bitcast(U32); b_icu = S.buf()
        idsu = WF[:, 1536:1664].bitcast(U32); b_ids = S.buf()
        dma(k12[0:64, 0:128], k12_in[0:64, l, :], [], [b_k12], "ld0")
        dma(k12[0:64, 128:256], k12_in[64:128, l, :], [], [b_k12], "ld0")

        def fr(ap_):
            return ap_.tensor, ap_.offset

        def ap4(ap_, off, d1, d2, d3):
            t_, o_ = fr(ap_)
            return bass.AP(t_, o_ + off, [list(ap_.ap[0]), d1, d2, d3])

        def ap3(ap_, off, d1, d2):
            t_, o_ = fr(ap_)
            return bass.AP(t_, o_ + off, [list(ap_.ap[0]), d1, d2])

        for tile_i in range(NBL):
            QG = BBC[:, RT0:RT0 + 2048].rearrange("p (a t) -> p a t", a=16)

            def igather(out_, tab_, col_, bufs, key):
                S.dma("pool", lambda e, o=out_, tb_=tab_, ix=IDX[:, col_:col_ + 1]: e.indirect_dma_start(
                    out=o, out_offset=None, in_=tb_, in_offset=bass.IndirectOffsetOnAxis(ap=ix, axis=0)),
                    [b_IDX], bufs, key)
            ib = tile_i * NIDX
            igather(h2t, h2tm, ib, [b_h2], "gh")
            for hs in range(16):
                igather(QG[:, hs, :], QTM, ib + 1 + hs, [b_qt], "gq")
            banks = [nps() for _ in range(4)]
            for hs in range(16):
                h, side = hs // 2, hs % 2
                pb, bpb = banks[hs // 4]
                col = (hs % 4) * 128
                mm(pb[:, col:col + 128], QG[0:64, hs, :], k12[0:64, side * 128:(side + 1) * 128], True, True,
                   [b_qt, b_k12], [bpb])
            for bi_ in range(4):
                act(WF1[:, bi_ * 512:(bi_ + 1) * 512], banks[bi_][0][:, :], AF.Copy, [banks[bi_][1]], [b_eq])
            for hs in range(16):
                bpb = b_eq
                src = WF1[:, hs * 128:hs * 128 + 128]
                va = v12[:, hs * 16:hs * 16 + 8]; vb = v12[:, hs * 16 + 8:hs * 16 + 16]
                ia_ = i12u[:, hs * 16:hs * 16 + 8]; ib_ = i12u[:, hs * 16 + 8:hs * 16 + 16]
                S.op("dve", lambda e, o=va, i=src: e.max(out=o, in_=i), [bpb], [b_v12])
                S.op("dve", lambda e, o=ia_, m=va, i=src: e.max_index(out=o, in_max=m, in_values=i), [bpb, b_v12], [b_i12u])
                S.op("dve", lambda e, o=tmpS, m=va, i=src: e.match_replace(out=o, in_to_replace=m, in_values=i, imm_value=-1e30), [bpb, b_v12], [b_tmpS])
                S.op("dve", lambda e, o=vb, i=tmpS: e.max(out=o, in_=i), [b_tmpS], [b_v12])
                S.op("dve", lambda e, o=ib_, m=vb, i=tmpS: e.max_index(out=o, in_max=m, in_values=i), [b_tmpS, b_v12], [b_i12u])
            tt(ap4(cand, 0, [256, 8], [16, 16], [1, 16]), ap4(v12, 0, [32, 8], [1, 16], [0, 16]), ap4(v12, 16, [32, 8], [0, 16], [1, 16]),
               ALU.add, [b_v12], [b_cand])
            for h in range(8):
                ch = cand[:, h * 256:(h + 1) * 256]
                va = vs[:, h * 16:h * 16 + 8]; vb = vs[:, h * 16 + 8:h * 16 + 16]
                S.op("dve", lambda e, o=va, i=ch: e.max(out=o, in_=i), [b_cand], [b_vs])
                S.op("dve", lambda e, o=icu[:, h * 16:h * 16 + 8], m=va, i=ch: e.max_index(out=o, in_max=m, in_values=i), [b_cand, b_vs], [b_icu])
                S.op("dve", lambda e, o=ch, m=va, i=ch: e.match_replace(out=o, in_to_replace=m, in_values=i, imm_value=-1e30), [b_vs], [b_cand])
                S.op("dve", lambda e, o=vb, i=ch: e.max(out=o, in_=i), [b_cand], [b_vs])
                S.op("dve", lambda e, o=icu[:, h * 16 + 8:h * 16 + 16], m=vb, i=ch: e.max_index(out=o, in_max=m, in_values=i), [b_cand, b_vs], [b_icu])
            negm = misc[:, 0:8]; sume = misc[:, 8:16]; rsum = misc[:, 16:24]
            ts(negm, bass.AP(vs.tensor, vs.offset, [list(vs.ap[0]), [16, 8]]), -1.0, None, ALU.mult, None, [b_vs], [b_misc])
            for h in range(8):
                act(ev[:, h * 16:(h + 1) * 16], vs[:, h * 16:(h + 1) * 16], AF.Exp, [b_vs, b_misc], [b_r],
                    bias=negm[:, h:h + 1])
            S.op("dve", lambda e, o=sume, i=ap3(ev, 0, [16, 8], [1, 16]): e.tensor_reduce(out=o, in_=i, axis=AX.X, op=ALU.add), [b_r], [b_misc])
            recip(rsum, sume, [b_r, b_misc], [b_misc])
            tt(ap3(gates, 0, [16, 8], [1, 16]), ap3(ev, 0, [16, 8], [1, 16]), ap3(rsum, 0, [1, 8], [0, 16]), ALU.mult, [b_r, b_misc], [b_r])
            icf = af
            cp(icf, icu[:, :], [b_icu], [b_r])
            cp(i12f, i12u[:, :], [b_i12u], [b_i12f])
            E4 = ap4(EQ, 0, [256, 8], [16, 16], [1, 16])
            E4b = ap4(EQ2, 0, [256, 8], [16, 16], [1, 16])
            icb = ap4(icf, 0, [16, 8], [1, 16], [0, 16])
            tt(E4, icb, ap4(iota16[:, :], 16, [0, 8], [0, 16], [1, 16]), ALU.is_ge, [b_r, b_iota], [b_eq])
            tt(E4b, icb, ap4(iota16[:, :], 32, [0, 8], [0, 16], [1, 16]), ALU.is_lt, [b_r, b_iota], [b_eq])
            tt(E4, E4, E4b, ALU.mult, [b_eq], [b_eq])
            tt(E4b, E4, ap4(iota16[:, :], 0, [0, 8], [0, 16], [1, 16]), ALU.mult, [b_eq, b_iota], [b_eq])
            S.op("dve", lambda e, o=bf, i=ap3(EQ2, 0, [16, 128], [1, 16]): e.tensor_reduce(out=o, in_=i, axis=AX.X, op=ALU.add), [b_eq], [b_r])
            tt(E4b, E4, ap4(i12f, 0, [32, 8], [0, 16], [1, 16]), ALU.mult, [b_eq, b_i12f], [b_eq])
            S.op("dve", lambda e, o=iaf, i=ap3(EQ2, 0, [16, 128], [1, 16]): e.tensor_reduce(out=o, in_=i, axis=AX.X, op=ALU.add), [b_eq], [b_r])
            stt(bf, bf, -16.0, icf, ALU.mult, ALU.add, [b_r], [b_r])
            tt(E4, ap4(bf, 0, [16, 8], [1, 16], [0, 16]), ap4(iota16[:, :], 0, [0, 8], [0, 16], [1, 16]), ALU.is_equal, [b_r, b_iota], [b_eq])
            tt(E4, E4, ap4(i12f, 16, [32, 8], [0, 16], [1, 16]), ALU.mult, [b_eq, b_i12f], [b_eq])
            S.op("dve", lambda e, o=ibf, i=ap3(EQ, 0, [16, 128], [1, 16]): e.tensor_reduce(out=o, in_=i, axis=AX.X, op=ALU.add), [b_eq], [b_r])
            stt(idsf, iaf, 128.0, ibf, ALU.mult, ALU.add, [b_r], [b_r])
            ts(idsf, idsf, 0.0, float(NEXP - 1), ALU.max, ALU.min, [b_r], [b_r])
            cp(idsu[:, :], idsf, [b_r], [b_ids])
            if dbg == "p5r":
                dma(dbgR[tile_i], WF[:, 0:1152], [b_r], [], "stdbg")
                continue
            for k in range(128):
                s_ = k % 4
                S.dma("pool", lambda e, o=G[:, s_, :], tb_=peer_u[l], ix=idsu[:, k:k + 1]: e.indirect_dma_start(
                    out=o, out_offset=None, in_=tb_, in_offset=bass.IndirectOffsetOnAxis(ap=ix, axis=0)),
                    [b_ids], [b_G[s_]], "g%d" % s_)
                S.op("dve", lambda e, o=G[:, s_, :], hh=h2t, ao=actv[:, k:k + 1]: e.scalar_tensor_tensor(
                    out=o, in0=o, scalar=1.0, in1=hh, op0=ALU.mult, op1=ALU.mult, accum_out=ao),
                    [b_h2, b_G[s_]], [b_G[s_], b_r])
            act(wv, actv, AF.Gelu_apprx_tanh, [b_r], [b_r])
            tt(wv, wv, gates, ALU.mult, [b_r], [b_r])
            for k in range(128):
                s_ = k % 4
                S.dma("pool", lambda e, o=G[:, s_, :], tb_=peer_v[l], ix=idsu[:, k:k + 1]: e.indirect_dma_start(
                    out=o, out_offset=None, in_=tb_, in_offset=bass.IndirectOffsetOnAxis(ap=ix, axis=0)),
                    [b_ids], [b_G[s_]], "g%d" % s_)
                if k == 0:
                    ts(acc, G[:, s_, :], wv[:, 0:1], None, ALU.mult, None, [b_G[s_], b_r], [b_acc])
                else:
                    stt(acc, G[:, s_, :], wv[:, k:k + 1], acc, ALU.mult, ALU.add, [b_G[s_], b_r, b_acc], [b_acc])
            if dbg:
                dma(dbgR[tile_i], WF[:, 0:1152], [b_r], [], "stdbg")
            for c in range(32):
                igather(x1t[:, c, :], XTM, ib + 17 + c, [b_x1], "gx")
            for c in range(32):
                if c % 4 == 0:
                    pt, bpt = nps()
                tr(pt[:, (c % 4) * 128:(c % 4) * 128 + 128], acc[:, c * 128:(c + 1) * 128], ident[:, :], [b_acc, b_ident], [bpt])
                if c % 4 == 3:
                    for c2 in range(c - 3, c + 1):
                        stt(x1t[:, c2, :], pt[:, (c2 % 4) * 128:(c2 % 4) * 128 + 128], modT[:, 160 + c2:161 + c2], x1t[:, c2, :],
                            ALU.mult, ALU.add, [bpt, b_mod, b_x1], [b_x1])
            dma(CCin[tile_i * D:(tile_i + 1) * D, :].rearrange("(c p) t -> p c t", p=128), x1t, [b_x1], [], "stx")
        S.barrier()
        b_cci = S.buf(); b_cco = [S.buf(), S.buf()]
        HR = D // 2
        for t_ in range(NBL):
            for hf in range(2):
                i_ = (t_ * 2 + hf) % 2
                src = CCin[t_ * D + hf * HR:t_ * D + (hf + 1) * HR, :]
                S.cc(lambda e, gr=groups, a=src, o=CCout[i_]: e.collective_compute("AllGather", ALU.bypass, replica_groups=gr, ins=[a], outs=[o]),
                     [b_cci], [b_cco[i_]])
                for r_ in range(GC):
                    r0 = (r_ * NBL + t_) * D + hf * HR
                    dma(XTM[r0:r0 + HR, :], CCout[i_][r_ * HR:(r_ + 1) * HR, :], [b_cco[i_]], [], "stcc")
        S.barrier()
    S.barrier()
    dma(outT, XTM, [], [S.buf()], "outcp")
    S.emit()
    return nc


def host_inputs(inp, T, depth, b, dbg=False, j=0, GC=4):
    NGL = NG // GC; NHL = NH // GC; NKL = NKV // GC
    gs = slice(j * NGL, (j + 1) * NGL); hsl = slice(j * NHL, (j + 1) * NHL)
    f = np.float32
    ca = np.ascontiguousarray
    d = {}
    NB = T // 128; NBL = NB // GC
    d["xtm_in"] = ca(inp["x"][b, :T].reshape(NB, 128, D).transpose(0, 2, 1).reshape(NB * D, 128))
    p = np.arange(128)
    idx = np.zeros((128, NBL * 49), np.uint32)
    for t in range(NBL):
        Tg = j * NBL + t
        idx[:, t * 49] = Tg * 128 + p
        for hs in range(16):
            idx[:, t * 49 + 1 + hs] = Tg * 1024 + (hs // 2) * 128 + (hs % 2) * 64 + (p % 64)
        for c in range(32):
            idx[:, t * 49 + 17 + c] = Tg * D + c * 128 + p
    d["cT"] = ca(inp["c"][b].reshape(32, 128).T)
    d["w_ada"] = inp["w_ada"]
    d["b_adaT"] = ca(inp["b_ada"].reshape(192, 128).T)
    d["adaT"] = ca(inp["ada_layer"][:depth].reshape(depth, 192, 128).transpose(2, 0, 1))
    d["g1T"] = ca(inp["norm1_g"][:depth].reshape(depth, 32, 128).transpose(2, 0, 1))
    d["g2T"] = ca(inp["norm2_g"][:depth].reshape(depth, 32, 128).transpose(2, 0, 1))
    wi = inp["w_in"][:depth]
    d["w_in"] = ca(np.concatenate([wi[:, :, j * NGL * 16:(j + 1) * NGL * 16], wi[:, :, 2048 + j * NHL * 64:2048 + (j + 1) * NHL * 64],
                                   wi[:, :, 4096 + j * NKL * 64:4096 + (j + 1) * NKL * 64], wi[:, :, 4352 + j * NKL * 64:4352 + (j + 1) * NKL * 64]], axis=2))
    d["w_glu"] = inp["w_glu"][:depth]
    d["w_out"] = inp["w_out"][:depth]
    d["w_q"] = inp["peer_wq"][:depth]
    lre = inp["lam_re"][:depth, gs]; lim = inp["lam_im"][:depth, gs]; ldt = inp["log_dt"][:depth, gs]
    lreT = lre.transpose(2, 0, 1); limT = lim.transpose(2, 0, 1)
    d["lamS_re"] = ca(np.concatenate([lreT, lreT], 0))
    d["lamS_im"] = ca(np.concatenate([limT, limT], 0))
    d["ldtS"] = ca(np.broadcast_to(ldt[None], (128, depth, NGL)))
    d["lamR_re"] = ca(np.broadcast_to(lre[None], (16, depth, NGL, 64)))
    d["lamR_im"] = ca(np.broadcast_to(lim[None], (16, depth, NGL, 64)))
    d["ldtR"] = ca(np.broadcast_to(ldt[None, :, :, None], (16, depth, NGL, 64)))
    d["bT_re"] = ca(inp["b_re"][:depth, gs].transpose(3, 0, 1, 2))
    d["bT_im"] = ca(inp["b_im"][:depth, gs].transpose(3, 0, 1, 2))
    cre = inp["c_re"][:depth, gs].transpose(3, 0, 1, 2); cim = inp["c_im"][:depth, gs].transpose(3, 0, 1, 2)
    d["cW1"] = ca(np.concatenate([cre, cim], 0))
    d["cW2"] = ca(np.concatenate([cim, cre], 0))
    d["dT"] = ca(inp["d_skip"][:depth, gs].transpose(2, 0, 1))
    d["qgT"] = ca(inp["q_gain"][:depth][:, :, None])
    d["kgT"] = ca(inp["k_gain"][:depth][:, :, None])
    d["sinksB"] = ca(np.broadcast_to(inp["sinks"][:depth, hsl][None], (128, depth, NHL)))
    slopes = np.exp2(-8.0 * np.arange(1, NH + 1, dtype=np.float64) / NH)
    kk = np.arange(128)[:, None]; qq = np.arange(128)[None, :]
    dist_prev = qq + 128 - kk
    dist_cur = qq - kk
    bt = np.full((128, NH, 256), NEG, np.float64)
    for h in range(NH):
        bt[:, h, 0:128] = np.where(dist_prev < 128, -slopes[h] * dist_prev, NEG)
        bt[:, h, 128:256] = np.where(dist_cur >= 0, -slopes[h] * dist_cur, NEG)
    d["biasT"] = ca(bt[:, hsl].astype(f))
    d["gnsT"] = ca(inp["gn_ssm"][:depth].reshape(depth, 16, 128).transpose(2, 0, 1))
    d["gnaT"] = ca(inp["gn_attn"][:depth].reshape(depth, 16, 128).transpose(2, 0, 1))
    d["k12"] = ca(np.concatenate([inp["peer_k1"][:depth].transpose(2, 0, 1), inp["peer_k2"][:depth].transpose(2, 0, 1)], 0))
    nexp = 16384 if dbg in (False, "full") else 128
    for i in range(depth):
        d["peer_u%d" % i] = inp["peer_u"][i, :nexp]
        d["peer_v%d" % i] = inp["peer_v"][i, :nexp]
    d["ident"] = np.eye(128, dtype=f)
    io = np.arange(16, dtype=f)
    d["iota16"] = ca(np.broadcast_to(np.concatenate([io, 16 * io, 16 * io + 16])[None], (128, 48)))
    out = {k: np.asarray(v, dtype=f) for k, v in d.items()}
    out["idx"] = idx
    return out


def run(inp, T, depth, dbg=False, GC=4):
    nc = build_program(T, depth, dbg, GC)
    in_maps = []
    for b in range(2):
        for j in range(GC):
            m = host_inputs(inp, T, depth, b, dbg, j, GC)
            m["idx"] = host_inputs_idx(T, j, GC)
            in_maps.append(m)
    res = run_bass_kernel_spmd(nc, in_maps, core_ids=list(range(2 * GC)))
    return res.results


def host_inputs_idx(T, j, GC):
    NB = T // 128; NBL = NB // GC
    p = np.arange(128)
    idx = np.zeros((128, NBL * 49), np.uint32)
    for t in range(NBL):
        Tg = j * NBL + t
        idx[:, t * 49] = Tg * 128 + p
        for hs in range(16):
            idx[:, t * 49 + 1 + hs] = Tg * 1024 + (hs // 2) * 128 + (hs % 2) * 64 + (p % 64)
        for c in range(32):
            idx[:, t * 49 + 17 + c] = Tg * D + c * 128 + p
    return idx


def untile(o, T):
    NB = T // 128
    return np.ascontiguousarray(o.reshape(NB, D, 128).transpose(0, 2, 1).reshape(T, D))


GCORES = 4


def kernel(**inputs):
    inp = {k: np.asarray(v) for k, v in inputs.items()}
    T = inp["x"].shape[1]
    res = run(inp, T, DEPTH, False, GCORES)
    out = np.stack([untile(res[b * GCORES]["outT"], T) for b in range(2)], 0)
    return out.astype(np.float32)
```

```python
import numpy as np
import concourse.bass as bass
import concourse.mybir as mybir
from concourse.bass_utils import run_bass_kernel_spmd

F32 = mybir.dt.float32
U32 = mybir.dt.uint32
AF = mybir.ActivationFunctionType
ALU = mybir.AluOpType
AX = mybir.AxisListType

D = 4096
DEPTH = 4
NG = 128
NH = 32
NKV = 4
INW = 4608
EPS = 1e-6
NEG = -30000.0


class Buf:
    __slots__ = ("w", "r", "name")

    def __init__(self, name=""):
        self.w = {}
        self.r = {}
        self.name = name


class Sched:
    ENG = ("pe", "act", "dve", "pool", "sp")

    def __init__(self, nc):
        self.nc = nc
        self.q = {e: [] for e in self.ENG}
        self.sems = {}
        self.latest = {}
        self.seen = {e: {} for e in self.ENG}
        for e in ("pe", "act", "dve", "pool"):
            self.sems[e] = nc.alloc_semaphore(name="done_" + e)
            self.latest[e] = 0
        self.bufs = []

    def buf(self, name=""):
        b = Buf(name)
        self.bufs.append(b)
        return b

    def _deps(self, reads, writes):
        deps = {}
        for b in reads:
            for k, v in b.w.items():
                if deps.get(k, 0) < v:
                    deps[k] = v
        for b in writes:
            for d in (b.w, b.r):
                for k, v in d.items():
                    if deps.get(k, 0) < v:
                        deps[k] = v
        return deps

    def _wait(self, eng, deps):
        seen = self.seen[eng]
        for k, v in deps.items():
            if k == "pe" and eng == "pe":
                continue
            if seen.get(k, 0) >= v:
                continue
            seen[k] = v
            self.q[eng].append(("wait", k, v))

    def _mark(self, tok, reads, writes):
        k, v = tok
        for b in reads:
            b.r[k] = v
        for b in writes:
            b.w = {k: v}
            b.r = {}

    def op(self, eng, fn, reads=(), writes=()):
        self._wait(eng, self._deps(reads, writes))
        self.latest[eng] += 1
        self.q[eng].append(("op", fn, eng, 1))
        self._mark((eng, self.latest[eng]), reads, writes)

    def dma(self, queue, fn, reads, writes, key):
        self._wait(queue, self._deps(reads, writes))
        if key not in self.sems:
            self.sems[key] = self.nc.alloc_semaphore(name="d_" + key)
            self.latest[key] = 0
        self.latest[key] += 16
        self.q[queue].append(("op", fn, key, 16))
        self._mark((key, self.latest[key]), reads, writes)

    def cc(self, fn, reads, writes):
        key = "cc"
        self._wait("pool", self._deps(reads, writes))
        if key not in self.sems:
            self.sems[key] = self.nc.alloc_semaphore(name="d_cc")
            self.latest[key] = 0
        self.latest[key] += 1
        self.q["pool"].append(("op", fn, key, 1))
        self._mark((key, self.latest[key]), reads, writes)

    def barrier(self):
        for e in self.ENG:
            self._wait(e, dict(self.latest))
        for b in self.bufs:
            b.w = {}
            b.r = {}

    def emit(self):
        nc = self.nc
        self.barrier()
        sems = self.sems

        def replay(name, e):
            for it in self.q[name]:
                if it[0] == "wait":
                    e.wait_ge(sems[it[1]], it[2])
                else:
                    it[1](e).then_inc(sems[it[2]], it[3])

        with nc.Block() as block:
            @block.tensor
            def _(e):
                replay("pe", e)

            @block.scalar
            def _(e):
                replay("act", e)

            @block.vector
            def _(e):
                replay("dve", e)

            @block.gpsimd
            def _(e):
                replay("pool", e)

            @block.sync
            def _(e):
                replay("sp", e)


def sb_ap(t, off, dims, np_=128, pstart=0):
    fs = 1
    for s in t.shape[1:]:
        fs *= s
    return bass.AP(t, pstart * fs + off, [[fs, np_]] + [list(d) for d in dims])


def build_program(T, depth, dbg=False, GC=4):
    nc = bass.Bass("TRN2", target_bir_lowering=False)
    S = Sched(nc)
    NTB = T // 512
    NB = T // 128
    NBL = NB // GC
    NIDX = 48
    NGL = NG // GC; NHL = NH // GC; NKL = NKV // GC
    CW = NGL * 16 + NHL * 64 + 2 * NKL * 64
    QOFF = NGL * 16; KOFF = QOFF + NHL * 64; VOFF = KOFF + NKL * 64

    def dint(name, shape, dt=F32):
        return nc.dram_tensor(name, list(shape), dt, kind="Internal").ap()

    def din(name, shape, dt=F32):
        return nc.dram_tensor(name, list(shape), dt, kind="ExternalInput").ap()

    def dscr(name, shape, dt=F32):
        return nc.dram_tensor(name, list(shape), dt, kind=("ExternalOutput" if dbg else "Internal")).ap()

    xtm_in = din("xtm_in", [NB * D, 128])
    idx_in = din("idx", [128, NBL * NIDX], U32)
    cT_in = din("cT", [128, 32])
    w_ada = din("w_ada", [D, 6 * D])
    badaT_in = din("b_adaT", [128, 192])
    adaT_in = din("adaT", [128, depth, 192])
    g1T_in = din("g1T", [128, depth, 32])
    g2T_in = din("g2T", [128, depth, 32])
    w_in = din("w_in", [depth, D, CW])
    w_glu = din("w_glu", [depth, 2048, 2048])
    w_out = din("w_out", [depth, D, D])
    w_q = din("w_q", [depth, D, 1024])
    lamS_re_in = din("lamS_re", [128, depth, NGL])
    lamS_im_in = din("lamS_im", [128, depth, NGL])
    ldtS_in = din("ldtS", [128, depth, NGL])
    lamR_re_in = din("lamR_re", [16, depth, NGL, 64])
    lamR_im_in = din("lamR_im", [16, depth, NGL, 64])
    ldtR_in = din("ldtR", [16, depth, NGL, 64])
    bT_re_in = din("bT_re", [16, depth, NGL, 64])
    bT_im_in = din("bT_im", [16, depth, NGL, 64])
    cW1_in = din("cW1", [128, depth, NGL, 16])
    cW2_in = din("cW2", [128, depth, NGL, 16])
    dT_in = din("dT", [16, depth, NGL])
    qgT_in = din("qgT", [depth, 64, 1])
    kgT_in = din("kgT", [depth, 64, 1])
    sinksB_in = din("sinksB", [128, depth, NHL])
    biasT_in = din("biasT", [128, NHL, 256])
    gnsT_in = din("gnsT", [128, depth, 16])
    gnaT_in = din("gnaT", [128, depth, 16])
    k12_in = din("k12", [128, depth, 128])
    NEXP = 16384 if dbg in (False, "full") else 128
    peer_u = [din("peer_u%d" % i, [NEXP, D]) for i in range(depth)]
    peer_v = [din("peer_v%d" % i, [NEXP, D]) for i in range(depth)]
    ident_in = din("ident", [128, 128])
    iota16_in = din("iota16", [128, 48])

    outT = nc.dram_tensor("outT", [NB * D, 128], F32, kind="ExternalOutput").ap()

    XTM = dint("XTM", [NB * D, 128])
    CCin = dint("CCin", [NBL * D, 128])
    CRg = max(1, (1 << 20) // (T * 4))
    CCg = [dint("CCg0", [GC * CRg, T]), dint("CCg1", [GC * CRg, T])]
    CCout = [dint("CCout0", [GC * (D // 2), 128]), dint("CCout1", [GC * (D // 2), 128])]
    zT = dscr("zT", [CW, T])
    gTl = dint("gTl", [NGL * 16, T])
    aTl = dint("aTl", [NHL * 64, T])
    GTM = dint("GTM", [NB * 2048, 128])
    ATM = dint("ATM", [NB * 2048, 128])
    X1L = dint("X1L", [NBL * D, 128])
    QL = dint("QL", [NBL * 1024, 128])
    h2tm = dscr("h2tm", [NBL * 128, D])
    dbgR = dscr("dbgR", [T // 128, 128, 1152]) if dbg else None

    def sb(name, shape, dt=F32):
        return nc.alloc_sbuf_tensor("sb_" + name, list(shape), dt)

    ident = sb("ident", [128, 128]); b_ident = S.buf()
    ones = sb("ones", [128, 128]); b_ones = S.buf()
    iota16 = sb("iota16", [128, 48]); b_iota = S.buf()
    condT = sb("condT", [128, 192]); b_cond = S.buf()
    modT = sb("modT", [128, 192]); b_mod = S.buf()
    AB = sb("AB", [128, 4, 32]); b_AB = S.buf()
    gpar = sb("gpar", [128, 2, 32]); b_gpar = S.buf()
    small = sb("small", [128, 64]); b_small = S.buf()
    BA = sb("BA", [128, 16384]); b_BA = S.buf("BA")
    BBC = sb("BBC", [128, 16384])

    class _View:
        def __init__(self, t, off):
            self.t = t; self.off = off

        def __getitem__(self, idx):
            p, c = idx
            return self.t[p, slice(c.start + self.off, c.stop + self.off)]
    BB = _View(BBC, 0); b_BB = S.buf("BB")
    BC = _View(BBC, 8192); b_BC = S.buf("BC")
    RSTD = sb("RSTD", [128, 512]); b_RSTD = S.buf("RSTD")
    k12 = sb("k12", [64, 256]); b_k12 = S.buf("k12")
    dtmp = sb("dtmp", [128, 8]); b_dtmp = S.buf("dtmp")
    IDX = sb("IDX", [128, NBL * NIDX], U32); b_IDX = S.buf("IDX")
    WT = [sb("WT0", [128, 32, 128]), sb("WT1", [128, 32, 128])]
    b_WT = [S.buf("WT0"), S.buf("WT1")]
    TM = [sb("TM%d" % i, [128, 512]) for i in range(4)]
    b_TM = [S.buf("TM%d" % i) for i in range(4)]
    PS = [nc.alloc_psum_tensor("ps%d" % i, [128, 512], F32) for i in range(8)]
    b_PS = [S.buf("ps%d" % i) for i in range(8)]
    st = {"ps": 0, "tm": 0, "wt": 0}

    def nps():
        i = st["ps"]; st["ps"] = (i + 1) % 8
        return PS[i], b_PS[i]

    def ntm():
        i = st["tm"]; st["tm"] = (i + 1) % 4
        return TM[i], b_TM[i]

    def dma(out, in_, reads, writes, key, queue="sp"):
        S.dma(queue, lambda e, o=out, i=in_: e.dma_start(out=o, in_=i), reads, writes, key)

    def act(out, in_, func, reads, writes, bias=None, scale=None, accum=None):
        kw = {}
        if bias is not None:
            kw["bias"] = bias
        if scale is not None:
            kw["scale"] = scale
        if accum is not None:
            kw["accum_out"] = accum
        S.op("act", lambda e, o=out, i=in_, f=func, kw=kw: e.activation(out=o, in_=i, func=f, **kw), reads, writes)

    def tt(out, a, b, op, reads, writes, eng="dve"):
        S.op(eng, lambda e, o=out, a=a, b=b, op=op: e.tensor_tensor(out=o, in0=a, in1=b, op=op), reads, writes)

    def ts(out, a, s1, s2, op0, op1, reads, writes, eng="dve"):
        if op1 is None:
            S.op(eng, lambda e, o=out, a=a, s1=s1, op0=op0: e.tensor_scalar(out=o, in0=a, scalar1=s1, scalar2=None, op0=op0), reads, writes)
        else:
            S.op(eng, lambda e, o=out, a=a, s1=s1, s2=s2, op0=op0, op1=op1: e.tensor_scalar(out=o, in0=a, scalar1=s1, scalar2=s2, op0=op0, op1=op1), reads, writes)

    def stt(out, a, s, b, op0, op1, reads, writes, eng="dve"):
        S.op(eng, lambda e, o=out, a=a, s=s, b=b, op0=op0, op1=op1: e.scalar_tensor_tensor(out=o, in0=a, scalar=s, in1=b, op0=op0, op1=op1), reads, writes)

    def cp(out, in_, reads, writes, eng="dve"):
        S.op(eng, lambda e, o=out, i=in_: e.tensor_copy(out=o, in_=i), reads, writes)

    def recip(out, in_, reads, writes):
        S.op("dve", lambda e, o=out, i=in_: e.reciprocal(out=o, in_=i), reads, writes)

    def mm(out, lhsT, rhs, start, stop, reads, writes):
        S.op("pe", lambda e, o=out, l=lhsT, r=rhs, s0=start, s1=stop: e.matmul(o, lhsT=l, rhs=r, start=s0, stop=s1), reads, writes)

    def tr(out, in_, idn, reads, writes):
        S.op("pe", lambda e, o=out, i=in_, d=idn: e.transpose(o, i, d), reads, writes)

    def memset(ap, val, writes, eng="dve"):
        S.op(eng, lambda e, a=ap, v=val: e.memset(a, v), [], writes)

    def rms_rstd(blk, b_blk, c0, nchunk, N, nfeat, rstd_ap, b_rstd):
        ps, bp = nps()
        for i in range(nchunk):
            sq, bsq = ntm()
            act(sq[:, 0:N], blk[:, c0 + i, 0:N], AF.Square, [b_blk], [bsq])
            mm(ps[:, 0:N], ones[:, :], sq[:, 0:N], i == 0, i == nchunk - 1, [b_ones, bsq], [bp])
        t1, bt1 = ntm()
        act(t1[:, 0:N], ps[:, 0:N], AF.Sqrt, [bp], [bt1], bias=small[:, 0:1], scale=1.0 / nfeat)
        recip(rstd_ap, t1[:, 0:N], [bt1, b_small], [b_rstd])

    def gemm(Wd, KC, ntiles, rhs_fn, rhs_bufs, N, evac):
        def load(j):
            s = st["wt"]; st["wt"] ^= 1
            dma(WT[s][:, 0:KC, :], Wd[:, j * 128:(j + 1) * 128].rearrange("(c p) n -> p c n", p=128),
                [], [b_WT[s]], "wt%d" % s)
            return s
        nxt = load(ntiles[0])
        for idx, j in enumerate(ntiles):
            s = nxt
            if idx + 1 < len(ntiles):
                nxt = load(ntiles[idx + 1])
            ps, bp = nps()
            for c in range(KC):
                mm(ps[:, 0:N], WT[s][:, c, :], rhs_fn(c), c == 0, c == KC - 1, [b_WT[s]] + rhs_bufs, [bp])
            evac(j, ps, bp)

    def xtile(T_):
        return XTM[T_ * D:(T_ + 1) * D, :].rearrange("(c p) t -> p c t", p=128)

    def xload(blk, tb, bufs, key):
        for q_ in range(4):
            dma(blk[:, :, q_ * 128:(q_ + 1) * 128], xtile(4 * tb + q_), [], bufs, key)

    def xstore(blk, tb, bufs, key):
        for q_ in range(4):
            dma(xtile(4 * tb + q_), blk[:, :, q_ * 128:(q_ + 1) * 128], bufs, [], key)

    def igather(out_, tab_, col_, bufs, key):
        S.dma("pool", lambda e, o=out_, tb_=tab_, ix=IDX[:, col_:col_ + 1]: e.indirect_dma_start(
            out=o, out_offset=None, in_=tb_, in_offset=bass.IndirectOffsetOnAxis(ap=ix, axis=0)),
            [b_IDX], bufs, key)

    dma(ident[:, :], ident_in, [], [b_ident], "c0")
    dma(iota16[:, :], iota16_in, [], [b_iota], "c1")
    memset(ones[:, :], 1.0, [b_ones])
    memset(small[:, 0:1], EPS, [b_small])
    memset(small[:, 1:2], float(np.pi / 2), [b_small])
    dma(XTM, xtm_in, [], [S.buf()], "xcp")
    dma(IDX[:, :], idx_in, [], [b_IDX], "c1")
    S.barrier()

    cT = BB[:, 0:32]
    dma(cT, cT_in, [], [b_BB], "ld0")
    scT = BB[:, 32:64]
    act(scT, cT, AF.Silu, [b_BB], [b_BB])
    psc, bpc = nps()

    def cond_evac(j, ps, bp):
        cp(condT[:, j:j + 1], ps[:, 0:1], [bp], [b_cond])
    gemm(w_ada, 32, list(range(192)), lambda c: BB[:, 32 + c:33 + c], [b_BB], 1, cond_evac)
    dma(BC[:, 0:192], badaT_in, [], [b_BC], "ld0")
    tt(condT[:, :], condT[:, :], BC[:, 0:192], ALU.add, [b_cond, b_BC], [b_cond])
    S.barrier()

    for l in range(depth):
        last = l == depth - 1
        dma(BC[:, 0:192], adaT_in[:, l, :], [], [b_BC], "ld0")
        dma(gpar[:, 0, :], g1T_in[:, l, :], [], [b_gpar], "ld1")
        dma(gpar[:, 1, :], g2T_in[:, l, :], [], [b_gpar], "ld1")
        tt(modT[:, :], condT[:, :], BC[:, 0:192], ALU.add, [b_cond, b_BC], [b_mod])
        stt(AB[:, 0, :], modT[:, 32:64], 1.0, gpar[:, 0, :], ALU.add, ALU.mult, [b_mod, b_gpar], [b_AB])
        cp(AB[:, 1, :], modT[:, 0:32], [b_mod], [b_AB])
        stt(AB[:, 2, :], modT[:, 128:160], 1.0, gpar[:, 1, :], ALU.add, ALU.mult, [b_mod, b_gpar], [b_AB])
        cp(AB[:, 3, :], modT[:, 96:128], [b_mod], [b_AB])
        S.barrier()

        xblk = BA[:, :].rearrange("p (c t) -> p c t", c=32)
        rstd = BB[:, 0:512]
        for tb in range(NTB):
            t0 = tb * 512
            xload(xblk, tb, [b_BA], "ldA")
            rms_rstd(xblk, b_BA, 0, 32, 512, D, rstd, b_BB)
            for c in range(32):
                tt(xblk[:, c, :], xblk[:, c, :], rstd, ALU.mult, [b_BA, b_BB], [b_BA])
                act(xblk[:, c, :], xblk[:, c, :], AF.Identity, [b_BA, b_AB], [b_BA],
                    bias=AB[:, 1, c:c + 1], scale=AB[:, 0, c:c + 1])

            def ev1(j, ps, bp, t0=t0):
                o, bo = ntm()
                act(o[:, :], ps[:, :], AF.Copy, [bp], [bo])
                dma(zT[j * 128:(j + 1) * 128, t0:t0 + 512], o[:, :], [bo], [], "stz")
            gemm(w_in[l], 32, list(range(CW // 128)), lambda c: xblk[:, c, :], [b_BA], 512, ev1)
        S.barrier()
        if dbg == "p1":
            break

        LG = T.bit_length() - 1
        CT = BA[:, 0:T]; ST_ = BA[:, 4096:4096 + T]; XT = BA[:, 8192:8192 + T]; SS = BA[:, 12288:12288 + T]
        b_TB = S.buf("tables"); b_XT = S.buf("xt"); b_SS = S.buf("ss"); b_U = S.buf("U"); b_YB = S.buf("YB")
        b_PR = S.buf("params"); b_W12 = S.buf("W12"); b_BBc = S.buf("BBc"); b_DD = S.buf("DD")
        U = BB[0:16, 0:T]; YB = BB[0:16, 4096:4096 + T]

        def P(k):
            return BC[:, k * 128:(k + 1) * 128]
        lre, lim, ldt, rr, th, cc, sn, t1_, t2_ = [P(k) for k in range(9)]
        PWc = BC[:, 2048:3584].rearrange("p (k g) -> p k g", k=12)
        PWs = BC[:, 3584:5120].rearrange("p (k g) -> p k g", k=12)
        W1 = WT[0][:, 0:16, :].rearrange("p a (b h) -> p (a b) h", h=16)
        W2 = WT[0][:, 16:32, :].rearrange("p a (b h) -> p (a b) h", h=16)
        BB1 = WT[1][0:16, 0:16, :]
        BB2 = WT[1][0:16, 16:32, :]
        Dd = BC[0:16, 5120:7168].rearrange("p (g h) -> p g h", h=16)
        dTs = BC[0:16, 7168:7296]
        dma(BC[:, 0:NGL], lamS_re_in[:, l, :], [], [b_PR], "ld0")
        dma(BC[:, 128:128 + NGL], lamS_im_in[:, l, :], [], [b_PR], "ld0")
        dma(BC[:, 256:256 + NGL], ldtS_in[:, l, :], [], [b_PR], "ld0")
        dma(W1[:, 0:NGL, :], cW1_in[:, l, :, :], [], [b_W12], "ld1")
        dma(W2[:, 0:NGL, :], cW2_in[:, l, :, :], [], [b_W12], "ld1")
        dma(BC[0:16, 7168:7168 + NGL], dT_in[:, l, :], [], [b_DD], "ld2")
        ts(WT[0][64:128, 0:16, :], WT[0][64:128, 0:16, :], -1.0, None, ALU.mult, None, [b_W12], [b_W12])
        tt(Dd, sb_ap(ident, 0, [[0, 128], [1, 16]], np_=16),
           bass.AP(BBC, 8192 + 7168, [[16384, 16], [1, 128], [0, 16]]), ALU.mult, [b_ident, b_DD], [b_DD])
        pr = [b_PR]
        act(t1_, ldt, AF.Exp, pr, pr)
        tt(t2_, lre, t1_, ALU.mult, pr, pr)
        act(rr, t2_, AF.Exp, pr, pr)
        tt(th, lim, t1_, ALU.mult, pr, pr)
        act(cc, th, AF.Sin, pr, pr, bias=small[:, 1:2], scale=-0.125)
        act(sn, th, AF.Sin, pr, pr, scale=0.125)

        def csq(c_, s_, a_, b_, bufs, eng="dve"):
            tt(a_, c_, c_, ALU.mult, bufs, bufs, eng)
            tt(b_, s_, s_, ALU.mult, bufs, bufs, eng)
            stt(s_, c_, 2.0, s_, ALU.mult, ALU.mult, bufs, bufs)
            tt(c_, a_, b_, ALU.subtract, bufs, bufs, eng)
        for _ in range(3):
            csq(cc, sn, t1_, t2_, pr)
        cp(PWc[:, 0, :], cc, pr, pr)
        ts(PWs[:, 0, :], sn, -1.0, None, ALU.mult, None, pr, pr)
        for k in range(1, LG):
            tt(t1_, PWc[:, k - 1, :], PWc[:, k - 1, :], ALU.mult, pr, pr)
            tt(t2_, PWs[:, k - 1, :], PWs[:, k - 1, :], ALU.mult, pr, pr)
            tt(PWc[:, k, :], t1_, t2_, ALU.subtract, pr, pr)
            stt(PWs[:, k, :], PWc[:, k - 1, :], 2.0, PWs[:, k - 1, :], ALU.mult, ALU.mult, pr, pr)

        for gc in range(NGL // 16):
            g0 = gc * 16
            A = [BA[0:16, k * 1024:(k + 1) * 1024] for k in range(16)]
            ba = [b_TB, b_XT, b_SS]

            def v3(a):
                return a.rearrange("p (g q) -> p g q", q=64)
            dma(v3(A[0]), lamR_re_in[:, l, g0:g0 + 16, :], [], ba, "ld0")
            dma(v3(A[1]), lamR_im_in[:, l, g0:g0 + 16, :], [], ba, "ld0")
            dma(v3(A[2]), ldtR_in[:, l, g0:g0 + 16, :], [], ba, "ld0")
            dma(v3(A[3]), bT_re_in[:, l, g0:g0 + 16, :], [], ba, "ld0")
            dma(v3(A[4]), bT_im_in[:, l, g0:g0 + 16, :], [], ba, "ld0")
            act(A[5], A[2], AF.Exp, ba, ba)
            tt(A[6], A[0], A[5], ALU.mult, ba, ba)
            act(A[6], A[6], AF.Exp, ba, ba)
            tt(A[7], A[1], A[5], ALU.mult, ba, ba)
            act(A[8], A[7], AF.Sin, ba + [b_small], ba, bias=small[0:16, 1:2], scale=-0.125)
            act(A[9], A[7], AF.Sin, ba, ba, scale=0.125)
            for _ in range(3):
                csq(A[8], A[9], A[10], A[11], ba)
            tt(A[8], A[6], A[8], ALU.mult, ba, ba)
            tt(A[9], A[6], A[9], ALU.mult, ba, ba)
            tt(A[10], A[0], A[0], ALU.mult, ba, ba)
            tt(A[11], A[1], A[1], ALU.mult, ba, ba)
            tt(A[10], A[10], A[11], ALU.add, ba, ba)
            recip(A[10], A[10], ba, ba)
            ts(A[8], A[8], -1.0, None, ALU.add, None, ba, ba)
            tt(A[11], A[8], A[0], ALU.mult, ba, ba)
            tt(A[12], A[9], A[1], ALU.mult, ba, ba)
            tt(A[11], A[11], A[12], ALU.add, ba, ba)
            tt(A[11], A[11], A[10], ALU.mult, ba, ba)
            tt(A[12], A[9], A[0], ALU.mult, ba, ba)
            tt(A[13], A[8], A[1], ALU.mult, ba, ba)
            tt(A[12], A[12], A[13], ALU.subtract, ba, ba)
            tt(A[12], A[12], A[10], ALU.mult, ba, ba)
            tt(A[13], A[11], A[3], ALU.mult, ba, ba)
            tt(A[14], A[12], A[4], ALU.mult, ba, ba)
            tt(A[13], A[13], A[14], ALU.subtract, ba, ba)
            tt(A[14], A[11], A[4], ALU.mult, ba, ba)
            tt(A[15], A[12], A[3], ALU.mult, ba, ba)
            tt(A[14], A[14], A[15], ALU.add, ba, ba)
            cp(BB1[:, :, 0:64], v3(A[13]), ba, [b_BBc])
            cp(BB1[:, :, 64:128], v3(A[14]), ba, [b_BBc])
            ts(BB2[:, :, 0:64], v3(A[14]), -1.0, None, ALU.mult, None, ba, [b_BBc])
            cp(BB2[:, :, 64:128], v3(A[13]), ba, [b_BBc])

            for gi in range(16):
                g = g0 + gi
                dma(U, zT[16 * g:16 * g + 16, 0:T], [], [b_U], "ldU")
                memset(CT[:, 0:1], 1.0, [b_TB])
                memset(ST_[:, 0:1], 0.0, [b_TB])
                for k in range(LG):
                    n = 1 << k
                    pc = PWc[:, k, g:g + 1]; ps_ = PWs[:, k, g:g + 1]
                    ta = BA[:, 8192:8192 + n]; tb_ = BA[:, 10240:10240 + n]
                    ts(ta, ST_[:, 0:n], ps_, None, ALU.mult, None, [b_TB, b_PR], [b_XT])
                    stt(CT[:, n:2 * n], CT[:, 0:n], pc, ta, ALU.mult, ALU.subtract, [b_TB, b_PR, b_XT], [b_TB])
                    ts(tb_, CT[:, 0:n], ps_, None, ALU.mult, None, [b_TB, b_PR], [b_XT])
                    stt(ST_[:, n:2 * n], ST_[:, 0:n], pc, tb_, ALU.mult, ALU.add, [b_TB, b_PR, b_XT], [b_TB])
                for i in range(NTB):
                    sl = slice(i * 512, (i + 1) * 512)
                    p1, bp1 = nps(); p2, bp2 = nps()
                    mm(p1[:, :], BB1[:, gi, :], U[:, sl], True, True, [b_BBc, b_U], [bp1])
                    mm(p2[:, :], BB2[:, gi, :], U[:, sl], True, True, [b_BBc, b_U], [bp2])
                    tmp, btmp = ntm()
                    tt(tmp[:, :], CT[:, sl], p1[:, :], ALU.mult, [b_TB, bp1], [btmp])
                    tt(XT[:, sl], ST_[:, sl], p2[:, :], ALU.mult, [b_TB, bp2], [b_XT])
                    tt(XT[:, sl], XT[:, sl], tmp[:, :], ALU.add, [b_XT, btmp], [b_XT])
                rb = bass.AP(BBC, 8192 + 3 * 128 + g, [[16384, 128], [0, T]])
                S.op("dve", lambda e, o=SS, d0=rb, d1=XT: e.tensor_tensor_scan(out=o, data0=d0, data1=d1, initial=0.0, op0=ALU.mult, op1=ALU.add),
                     [b_PR, b_XT], [b_SS])
                for i in range(NTB):
                    sl = slice(i * 512, (i + 1) * 512)
                    q1, bq1 = ntm(); q2, bq2 = ntm()
                    tt(q1[:, :], CT[:, sl], SS[:, sl], ALU.mult, [b_TB, b_SS], [bq1], "pool")
                    tt(q2[:, :], ST_[:, sl], SS[:, sl], ALU.mult, [b_TB, b_SS], [bq2], "pool")
                    py, bpy = nps()
                    mm(py[0:16, :], W1[:, g, :], q1[:, :], True, False, [b_W12, bq1], [bpy])
                    mm(py[0:16, :], W2[:, g, :], q2[:, :], False, False, [b_W12, bq2], [bpy])
                    mm(py[0:16, :], Dd[:, g, :], U[:, sl], False, True, [b_DD, b_U], [bpy])
                    act(YB[:, sl], py[0:16, :], AF.Gelu_apprx_tanh, [bpy], [b_YB])
                dma(gTl[16 * g:16 * g + 16, 0:T], YB, [b_YB], [], "stg")
        S.barrier()
        if dbg == "p2a":
            break

        b_bias = S.buf(); b_es = S.buf(); b_kh = S.buf(); b_va = S.buf(); b_qh = S.buf(); b_raw = S.buf()
        b_vr = S.buf(); b_yh = S.buf(); b_yT = S.buf(); b_g = S.buf()
        biasT = BBC[:, 8192:8192 + NHL * 256].rearrange("p (h k) -> p h k", k=256)
        khT = BB[0:64, 0:T]
        Vaug = BBC[:, 4096:4096 + NB * 65].rearrange("p (n d) -> p n d", d=65)
        qhT = BA[0:64, 0:T]; RAW = BA[0:64, 4096:4096 + T]; VR = BA[0:64, 8192:8192 + T]
        yh = BA[:, 12288:12288 + NB * 64].rearrange("p (n d) -> p n d", d=64)
        yhT = WT[0][0:64, :, :].rearrange("p a b -> p (a b)")
        esink = small[:, 16:16 + NHL]
        dma(biasT, biasT_in, [], [b_bias], "ld0")
        dma(esink, sinksB_in[:, l, :], [], [b_es], "ld1")
        act(esink, esink, AF.Exp, [b_es], [b_es])
        dma(small[0:64, 48:49], qgT_in[l], [], [b_g], "ld2")
        dma(small[0:64, 49:50], kgT_in[l], [], [b_g], "ld2")
        memset(Vaug[:, :, 64:65], 1.0, [b_va])

        def qknorm(dst, b_dst, gcol):
            for i in range(NTB):
                sl = slice(i * 512, (i + 1) * 512)
                sq, bsq = ntm()
                act(sq[0:64, :], RAW[:, sl], AF.Square, [b_raw], [bsq])
                ps, bp = nps()
                mm(ps[0:64, :], ones[0:64, 0:64], sq[0:64, :], True, True, [b_ones, bsq], [bp])
                t1, bt1 = ntm()
                act(t1[0:64, :], ps[0:64, :], AF.Sqrt, [bp, b_small], [bt1], bias=small[0:64, 0:1], scale=1.0 / 64)
                t2, bt2 = ntm()
                recip(t2[0:64, :], t1[0:64, :], [bt1], [bt2])
                stt(dst[:, sl], RAW[:, sl], small[0:64, gcol:gcol + 1], t2[0:64, :], ALU.mult, ALU.mult, [b_raw, b_g, bt2], [b_dst])

        for kv in range(NKL):
            dma(RAW, zT[KOFF + 64 * kv:KOFF + 64 * kv + 64, 0:T], [], [b_raw], "ldA")
            qknorm(khT, b_kh, 49)
            dma(VR, zT[VOFF + 64 * kv:VOFF + 64 * kv + 64, 0:T], [], [b_vr], "ldB")
            for n in range(NB):
                if n % 4 == 0:
                    pv, bpv = nps()
                tr(pv[:, (n % 4) * 64:(n % 4) * 64 + 64], VR[:, n * 128:(n + 1) * 128], ident[0:64, 0:64], [b_vr, b_ident], [bpv])
                if n % 4 == 3:
                    cp(Vaug[:, n - 3:n + 1, 0:64], pv[:, 0:256].rearrange("p (n d) -> p n d", d=64), [bpv], [b_va])
            for hq in range(8):
                h = kv * 8 + hq
                dma(RAW, zT[QOFF + 64 * h:QOFF + 64 * h + 64, 0:T], [], [b_raw], "ldA")
                qknorm(qhT, b_qh, 48)
                for n in range(NB):
                    blk = slice(n * 128, (n + 1) * 128)
                    sc, bsc = nps()
                    if n > 0:
                        mm(sc[:, 0:128], khT[:, (n - 1) * 128:n * 128], qhT[:, blk], True, True, [b_kh, b_qh], [bsc])
                    mm(sc[:, 128:256], khT[:, blk], qhT[:, blk], True, True, [b_kh, b_qh], [bsc])
                    cols = slice(0, 256) if n > 0 else slice(128, 256)
                    s_sb, bs = ntm()
                    stt(s_sb[:, cols], sc[:, cols], 0.125, biasT[:, h, cols], ALU.mult, ALU.add, [bsc, b_bias], [bs])
                    e_sb, be = ntm()
                    act(e_sb[:, cols], s_sb[:, cols], AF.Exp, [bs], [be])
                    po, bpo = nps()
                    if n > 0:
                        mm(po[:, 0:65], e_sb[:, 0:128], Vaug[:, n - 1, :], True, False, [be, b_va], [bpo])
                    mm(po[:, 0:65], e_sb[:, 128:256], Vaug[:, n, :], n == 0, True, [be, b_va], [bpo])
                    tt(dtmp[:, 0:1], po[:, 64:65], esink[:, h:h + 1], ALU.add, [bpo, b_es], [b_dtmp])
                    recip(dtmp[:, 1:2], dtmp[:, 0:1], [b_dtmp], [b_dtmp])
                    ts(yh[:, n, :], po[:, 0:64], dtmp[:, 1:2], None, ALU.mult, None, [bpo, b_dtmp], [b_yh])
                for n in range(NB):
                    if n % 4 == 0:
                        pt, bpt = nps()
                    tr(pt[0:64, (n % 4) * 128:(n % 4) * 128 + 128], yh[:, n, :], ident[:, :], [b_yh, b_ident], [bpt])
                    if n % 4 == 3:
                        act(yhT[:, (n - 3) * 128:(n + 1) * 128], pt[0:64, :], AF.Copy, [bpt], [b_yT])
                dma(aTl[64 * h:64 * h + 64, 0:T], yhT[:, 0:T], [b_yT], [], "sta")
        S.barrier()
        groups = [list(range(g_ * GC, (g_ + 1) * GC)) for g_ in range(2)]
        b_cco = [S.buf(), S.buf()]
        CR = max(1, (1 << 20) // (T * 4))
        cnt_ = 0
        for (srcT, dstT, rows_l) in ((gTl, GTM, NGL * 16), (aTl, ATM, NHL * 64)):
            for r0 in range(0, rows_l, CR):
                i_ = cnt_ % 2; cnt_ += 1
                co = CCg[i_]
                S.cc(lambda e, gr=groups, a=srcT[r0:r0 + CR, :], o=co: e.collective_compute("AllGather", ALU.bypass, replica_groups=gr, ins=[a], outs=[o]),
                     [], [b_cco[i_]])
                for r_ in range(GC):
                    dma(dstT.rearrange("(n r) t -> r n t", r=2048)[r_ * rows_l + r0:r_ * rows_l + r0 + CR, :, :],
                        co[r_ * CR:(r_ + 1) * CR, :].rearrange("r (n t) -> r n t", t=128), [b_cco[i_]], [], "stcg")
        S.barrier()
        if dbg == "p2":
            break

        b_CLO = S.buf(); b_CHI = S.buf(); b_gn = S.buf()
        xblk = BA[:, :].rearrange("p (c t) -> p c t", c=32)
        cat = BBC[:, :].rearrange("p (c t) -> p c t", c=32)
        gns = small[:, 16:32]; gna = small[:, 32:48]
        dma(gns, gnsT_in[:, l, :], [], [b_gn], "ld0")
        dma(gna, gnaT_in[:, l, :], [], [b_gn], "ld0")
        for tb in range(NBL // 4):
            t0 = tb * 512
            for q_ in range(4):
                ibq = (tb * 4 + q_) * NIDX
                for c in range(32):
                    igather(xblk[:, c, q_ * 128:(q_ + 1) * 128], XTM, ibq + c, [b_BA], "gx")
                for c in range(16):
                    igather(cat[:, 16 + c, q_ * 128:(q_ + 1) * 128], GTM, ibq + 32 + c, [b_CHI], "gg")

            def ev_glu(j, ps, bp):
                sg, bsg = ntm()
                act(sg[:, :], ps[:, :], AF.Sigmoid, [bp], [bsg])
                tt(cat[:, j, :], cat[:, 16 + j, :], sg[:, :], ALU.mult, [b_CHI, bsg], [b_CLO])
            gemm(w_glu[l], 16, list(range(16)), lambda c: cat[:, 16 + c, :], [b_CHI], 512, ev_glu)
            for q_ in range(4):
                ibq = (tb * 4 + q_) * NIDX
                for c in range(16):
                    igather(cat[:, 16 + c, q_ * 128:(q_ + 1) * 128], ATM, ibq + 32 + c, [b_CHI], "gg")
            rms_rstd(cat, b_CLO, 0, 16, 512, 2048, RSTD[:, :], b_RSTD)
            for c in range(16):
                stt(cat[:, c, :], cat[:, c, :], gns[:, c:c + 1], RSTD[:, :], ALU.mult, ALU.mult, [b_CLO, b_gn, b_RSTD], [b_CLO])
            rms_rstd(cat, b_CHI, 16, 16, 512, 2048, RSTD[:, :], b_RSTD)
            for c in range(16):
                stt(cat[:, 16 + c, :], cat[:, 16 + c, :], gna[:, c:c + 1], RSTD[:, :], ALU.mult, ALU.mult, [b_CHI, b_gn, b_RSTD], [b_CHI])

            def ev_out(j, ps, bp):
                stt(xblk[:, j, :], ps[:, :], modT[:, 64 + j:65 + j], xblk[:, j, :], ALU.mult, ALU.add, [bp, b_mod, b_BA], [b_BA])
            gemm(w_out[l], 32, list(range(32)), lambda c: cat[:, c, :], [b_CLO, b_CHI], 512, ev_out)
            for q_ in range(4):
                tl_ = tb * 4 + q_
                dma(X1L[tl_ * D:(tl_ + 1) * D, :].rearrange("(c p) t -> p c t", p=128), xblk[:, :, q_ * 128:(q_ + 1) * 128], [b_BA], [], "stx")
            rms_rstd(xblk, b_BA, 0, 32, 512, D, RSTD[:, :], b_RSTD)
            bh = [b_CLO, b_CHI]
            for c in range(32):
                tt(cat[:, c, :], xblk[:, c, :], RSTD[:, :], ALU.mult, [b_BA, b_RSTD], bh)
                act(cat[:, c, :], cat[:, c, :], AF.Identity, bh + [b_AB], bh, bias=AB[:, 3, c:c + 1], scale=AB[:, 2, c:c + 1])

            def ev_q(j, ps, bp, t0=t0):
                o, bo = ntm()
                act(o[:, :], ps[:, :], AF.Copy, [bp], [bo])
                for q_ in range(4):
                    T_ = t0 // 128 + q_
                    dma(QL[T_ * 1024 + j * 128:T_ * 1024 + (j + 1) * 128, :], o[:, q_ * 128:(q_ + 1) * 128], [bo], [], "stq")
            gemm(w_q[l], 32, list(range(8)), lambda c: cat[:, c, :], bh, 512, ev_q)
            for t4 in range(4):
                s_ = st["wt"]; st["wt"] ^= 1
                ROW = WT[s_][:, :, :].rearrange("p a b -> p (a b)")
                for c in range(32):
                    if c % 4 == 0:
                        pt, bpt = nps()
                    tr(pt[:, (c % 4) * 128:(c % 4) * 128 + 128], cat[:, c, t4 * 128:(t4 + 1) * 128], ident[:, :], bh + [b_ident], [bpt])
                    if c % 4 == 3:
                        act(ROW[:, (c - 3) * 128:(c + 1) * 128], pt[:, :], AF.Copy, [bpt], [b_WT[s_]])
                dma(h2tm[t0 + t4 * 128:t0 + (t4 + 1) * 128, :], ROW, [b_WT[s_]], [], "sth")
        S.barrier()
        if dbg == "p4":
            break

        b_G = [S.buf() for _ in range(4)]
        G = BA[:, :].rearrange("p (s d) -> p s d", s=4)
        h2t = BBC[:, 0:4096]; b_h2 = S.buf()
        acc = BBC[:, 4096:8192]; b_acc = S.buf()
        x1t = BA[:, 0:4096].rearrange("p (c t) -> p c t", c=32); b_x1 = b_G[0]
        RT0 = 11264
        qtile = BBC[0:64, RT0:RT0 + 2048].rearrange("p (s h t) -> p s h t", s=2, h=8); b_qt = S.buf()
        v12 = BBC[:, RT0 + 2048:RT0 + 2304]; b_v12 = S.buf()
        i12f = BBC[:, RT0 + 2304:RT0 + 2560]; b_i12f = S.buf()
        tmpS = BBC[:, RT0 + 2560:RT0 + 2688]; b_tmpS = S.buf()
        cand = BBC[:, RT0 + 2688:RT0 + 4736]; b_cand = S.buf()
        vs = BBC[:, RT0 + 4736:RT0 + 4864]; b_vs = S.buf()
        misc = BBC[:, RT0 + 4864:RT0 + 4992]; b_misc = S.buf()
        WF = WT[0][:, :, :].rearrange("p a b -> p (a b)")
        ev = WF[:, 0:128]; gates = WF[:, 128:256]; af = WF[:, 256:384]; bf = WF[:, 384:512]
        iaf = WF[:, 512:640]; ibf = WF[:, 640:768]; idsf = WF[:, 768:896]; actv = WF[:, 896:1024]; wv = WF[:, 1024:1152]
        b_r = S.buf()
        WF1 = WT[1][:, :, :].rearrange("p a b -> p (a b)")
        EQ = WF1[:, 0:2048]; EQ2 = WF1[:, 2048:4096]; b_eq = S.buf()
        i12u = WF[:, 1152:1408].bitcast(U32); b_i12u = S.buf()
        icu = WF[:, 1408:1536].bitcast(U32); b_icu = S.buf()
        idsu = WF[:, 1536:1664].bitcast(U32); b_ids = S.buf()
        dma(k12[0:64, 0:128], k12_in[0:64, l, :], [], [b_k12], "ld0")
        dma(k12[0:64, 128:256], k12_in[64:128, l, :], [], [b_k12], "ld0")

        def fr(ap_):
            return ap_.tensor, ap_.offset

        def ap4(ap_, off, d1, d2, d3):
            t_, o_ = fr(ap_)
            return bass.AP(t_, o_ + off, [list(ap_.ap[0]), d1, d2, d3])

        def ap3(ap_, off, d1, d2):
            t_, o_ = fr(ap_)
            return bass.AP(t_, o_ + off, [list(ap_.ap[0]), d1, d2])

        for tile_i in range(NBL):
            QG = BBC[:, RT0:RT0 + 2048].rearrange("p (a t) -> p a t", a=16)

            dma(h2t, h2tm[tile_i * 128:(tile_i + 1) * 128, :], [], [b_h2], "ldB")
            for hs in range(16):
                r0q = tile_i * 1024 + (hs // 2) * 128 + (hs % 2) * 64
                dma(QG[0:64, hs, :], QL[r0q:r0q + 64, :], [], [b_qt], "ldA")
            banks = [nps() for _ in range(4)]
            for hs in range(16):
                h, side = hs // 2, hs % 2
                pb, bpb = banks[hs // 4]
                col = (hs % 4) * 128
                mm(pb[:, col:col + 128], QG[0:64, hs, :], k12[0:64, side * 128:(side + 1) * 128], True, True,
                   [b_qt, b_k12], [bpb])
            for bi_ in range(4):
                act(WF1[:, bi_ * 512:(bi_ + 1) * 512], banks[bi_][0][:, :], AF.Copy, [banks[bi_][1]], [b_eq])
            for hs in range(16):
                bpb = b_eq
                src = WF1[:, hs * 128:hs * 128 + 128]
                va = v12[:, hs * 16:hs * 16 + 8]; vb = v12[:, hs * 16 + 8:hs * 16 + 16]
                ia_ = i12u[:, hs * 16:hs * 16 + 8]; ib_ = i12u[:, hs * 16 + 8:hs * 16 + 16]
                S.op("dve", lambda e, o=va, i=src: e.max(out=o, in_=i), [bpb], [b_v12])
                S.op("dve", lambda e, o=ia_, m=va, i=src: e.max_index(out=o, in_max=m, in_values=i), [bpb, b_v12], [b_i12u])
                S.op("dve", lambda e, o=tmpS, m=va, i=src: e.match_replace(out=o, in_to_replace=m, in_values=i, imm_value=-1e30), [bpb, b_v12], [b_tmpS])
                S.op("dve", lambda e, o=vb, i=tmpS: e.max(out=o, in_=i), [b_tmpS], [b_v12])
                S.op("dve", lambda e, o=ib_, m=vb, i=tmpS: e.max_index(out=o, in_max=m, in_values=i), [b_tmpS, b_v12], [b_i12u])
            tt(ap4(cand, 0, [256, 8], [16, 16], [1, 16]), ap4(v12, 0, [32, 8], [1, 16], [0, 16]), ap4(v12, 16, [32, 8], [0, 16], [1, 16]),
               ALU.add, [b_v12], [b_cand])
            for h in range(8):
                ch = cand[:, h * 256:(h + 1) * 256]
                va = vs[:, h * 16:h * 16 + 8]; vb = vs[:, h * 16 + 8:h * 16 + 16]
                S.op("dve", lambda e, o=va, i=ch: e.max(out=o, in_=i), [b_cand], [b_vs])
                S.op("dve", lambda e, o=icu[:, h * 16:h * 16 + 8], m=va, i=ch: e.max_index(out=o, in_max=m, in_values=i), [b_cand, b_vs], [b_icu])
                S.op("dve", lambda e, o=ch, m=va, i=ch: e.match_replace(out=o, in_to_replace=m, in_values=i, imm_value=-1e30), [b_vs], [b_cand])
                S.op("dve", lambda e, o=vb, i=ch: e.max(out=o, in_=i), [b_cand], [b_vs])
                S.op("dve", lambda e, o=icu[:, h * 16 + 8:h * 16 + 16], m=vb, i=ch: e.max_index(out=o, in_max=m, in_values=i), [b_cand, b_vs], [b_icu])
            negm = misc[:, 0:8]; sume = misc[:, 8:16]; rsum = misc[:, 16:24]
            ts(negm, bass.AP(vs.tensor, vs.offset, [list(vs.ap[0]), [16, 8]]), -1.0, None, ALU.mult, None, [b_vs], [b_misc])
            for h in range(8):
                act(ev[:, h * 16:(h + 1) * 16], vs[:, h * 16:(h + 1) * 16], AF.Exp, [b_vs, b_misc], [b_r],
                    bias=negm[:, h:h + 1])
            S.op("dve", lambda e, o=sume, i=ap3(ev, 0, [16, 8], [1, 16]): e.tensor_reduce(out=o, in_=i, axis=AX.X, op=ALU.add), [b_r], [b_misc])
            recip(rsum, sume, [b_r, b_misc], [b_misc])
            tt(ap3(gates, 0, [16, 8], [1, 16]), ap3(ev, 0, [16, 8], [1, 16]), ap3(rsum, 0, [1, 8], [0, 16]), ALU.mult, [b_r, b_misc], [b_r])
            icf = af
            cp(icf, icu[:, :], [b_icu], [b_r])
            cp(i12f, i12u[:, :], [b_i12u], [b_i12f])
            E4 = ap4(EQ, 0, [256, 8], [16, 16], [1, 16])
            E4b = ap4(EQ2, 0, [256, 8], [16, 16], [1, 16])
            icb = ap4(icf, 0, [16, 8], [1, 16], [0, 16])
            tt(E4, icb, ap4(iota16[:, :], 16, [0, 8], [0, 16], [1, 16]), ALU.is_ge, [b_r, b_iota], [b_eq])
            tt(E4b, icb, ap4(iota16[:, :], 32, [0, 8], [0, 16], [1, 16]), ALU.is_lt, [b_r, b_iota], [b_eq])
            tt(E4, E4, E4b, ALU.mult, [b_eq], [b_eq])
            tt(E4b, E4, ap4(iota16[:, :], 0, [0, 8], [0, 16], [1, 16]), ALU.mult, [b_eq, b_iota], [b_eq])
            S.op("dve", lambda e, o=bf, i=ap3(EQ2, 0, [16, 128], [1, 16]): e.tensor_reduce(out=o, in_=i, axis=AX.X, op=ALU.add), [b_eq], [b_r])
            tt(E4b, E4, ap4(i12f, 0, [32, 8], [0, 16], [1, 16]), ALU.mult, [b_eq, b_i12f], [b_eq])
            S.op("dve", lambda e, o=iaf, i=ap3(EQ2, 0, [16, 128], [1, 16]): e.tensor_reduce(out=o, in_=i, axis=AX.X, op=ALU.add), [b_eq], [b_r])
            stt(bf, bf, -16.0, icf, ALU.mult, ALU.add, [b_r], [b_r])
            tt(E4, ap4(bf, 0, [16, 8], [1, 16], [0, 16]), ap4(iota16[:, :], 0, [0, 8], [0, 16], [1, 16]), ALU.is_equal, [b_r, b_iota], [b_eq])
            tt(E4, E4, ap4(i12f, 16, [32, 8], [0, 16], [1, 16]), ALU.mult, [b_eq, b_i12f], [b_eq])
            S.op("dve", lambda e, o=ibf, i=ap3(EQ, 0, [16, 128], [1, 16]): e.tensor_reduce(out=o, in_=i, axis=AX.X, op=ALU.add), [b_eq], [b_r])
            stt(idsf, iaf, 128.0, ibf, ALU.mult, ALU.add, [b_r], [b_r])
            ts(idsf, idsf, 0.0, float(NEXP - 1), ALU.max, ALU.min, [b_r], [b_r])
            cp(idsu[:, :], idsf, [b_r], [b_ids])
            if dbg == "p5r":
                dma(dbgR[tile_i], WF[:, 0:1152], [b_r], [], "stdbg")
                continue
            for k in range(128):
                s_ = k % 4
                S.dma("pool", lambda e, o=G[:, s_, :], tb_=peer_u[l], ix=idsu[:, k:k + 1]: e.indirect_dma_start(
                    out=o, out_offset=None, in_=tb_, in_offset=bass.IndirectOffsetOnAxis(ap=ix, axis=0)),
                    [b_ids], [b_G[s_]], "g%d" % s_)
                S.op("dve", lambda e, o=G[:, s_, :], hh=h2t, ao=actv[:, k:k + 1]: e.scalar_tensor_tensor(
                    out=o, in0=o, scalar=1.0, in1=hh, op0=ALU.mult, op1=ALU.mult, accum_out=ao),
                    [b_h2, b_G[s_]], [b_G[s_], b_r])
            act(wv, actv, AF.Gelu_apprx_tanh, [b_r], [b_r])
            tt(wv, wv, gates, ALU.mult, [b_r], [b_r])
            for k in range(128):
                s_ = k % 4
                S.dma("pool", lambda e, o=G[:, s_, :], tb_=peer_v[l], ix=idsu[:, k:k + 1]: e.indirect_dma_start(
                    out=o, out_offset=None, in_=tb_, in_offset=bass.IndirectOffsetOnAxis(ap=ix, axis=0)),
                    [b_ids], [b_G[s_]], "g%d" % s_)
                if k == 0:
                    ts(acc, G[:, s_, :], wv[:, 0:1], None, ALU.mult, None, [b_G[s_], b_r], [b_acc])
                else:
                    stt(acc, G[:, s_, :], wv[:, k:k + 1], acc, ALU.mult, ALU.add, [b_G[s_], b_r, b_acc], [b_acc])
            if dbg:
                dma(dbgR[tile_i], WF[:, 0:1152], [b_r], [], "stdbg")
            dma(x1t, X1L[tile_i * D:(tile_i + 1) * D, :].rearrange("(c p) t -> p c t", p=128), [], [b_x1], "ldC")
            for c in range(32):
                if c % 4 == 0:
                    pt, bpt = nps()
                tr(pt[:, (c % 4) * 128:(c % 4) * 128 + 128], acc[:, c * 128:(c + 1) * 128], ident[:, :], [b_acc, b_ident], [bpt])
                if c % 4 == 3:
                    for c2 in range(c - 3, c + 1):
                        stt(x1t[:, c2, :], pt[:, (c2 % 4) * 128:(c2 % 4) * 128 + 128], modT[:, 160 + c2:161 + c2], x1t[:, c2, :],
                            ALU.mult, ALU.add, [bpt, b_mod, b_x1], [b_x1])
            dma(CCin[tile_i * D:(tile_i + 1) * D, :].rearrange("(c p) t -> p c t", p=128), x1t, [b_x1], [], "stx")
        S.barrier()
        b_cci = S.buf(); b_cco = [S.buf(), S.buf()]
        HR = D // 2
        for t_ in range(NBL):
            for hf in range(2):
                i_ = (t_ * 2 + hf) % 2
                src = CCin[t_ * D + hf * HR:t_ * D + (hf + 1) * HR, :]
                S.cc(lambda e, gr=groups, a=src, o=CCout[i_]: e.collective_compute("AllGather", ALU.bypass, replica_groups=gr, ins=[a], outs=[o]),
                     [b_cci], [b_cco[i_]])
                for r_ in range(GC):
                    r0 = (r_ * NBL + t_) * D + hf * HR
                    dma(XTM[r0:r0 + HR, :], CCout[i_][r_ * HR:(r_ + 1) * HR, :], [b_cco[i_]], [], "stcc")
        S.barrier()
    S.barrier()
    dma(outT, XTM, [], [S.buf()], "outcp")
    S.emit()
    return nc


def host_inputs(inp, T, depth, b, dbg=False, j=0, GC=4):
    NGL = NG // GC; NHL = NH // GC; NKL = NKV // GC
    gs = slice(j * NGL, (j + 1) * NGL); hsl = slice(j * NHL, (j + 1) * NHL)
    f = np.float32
    ca = np.ascontiguousarray
    d = {}
    NB = T // 128; NBL = NB // GC
    d["xtm_in"] = ca(inp["x"][b, :T].reshape(NB, 128, D).transpose(0, 2, 1).reshape(NB * D, 128))
    idx = host_inputs_idx(T, j, GC)
    d["cT"] = ca(inp["c"][b].reshape(32, 128).T)
    d["w_ada"] = inp["w_ada"]
    d["b_adaT"] = ca(inp["b_ada"].reshape(192, 128).T)
    d["adaT"] = ca(inp["ada_layer"][:depth].reshape(depth, 192, 128).transpose(2, 0, 1))
    d["g1T"] = ca(inp["norm1_g"][:depth].reshape(depth, 32, 128).transpose(2, 0, 1))
    d["g2T"] = ca(inp["norm2_g"][:depth].reshape(depth, 32, 128).transpose(2, 0, 1))
    wi = inp["w_in"][:depth]
    d["w_in"] = ca(np.concatenate([wi[:, :, j * NGL * 16:(j + 1) * NGL * 16], wi[:, :, 2048 + j * NHL * 64:2048 + (j + 1) * NHL * 64],
                                   wi[:, :, 4096 + j * NKL * 64:4096 + (j + 1) * NKL * 64], wi[:, :, 4352 + j * NKL * 64:4352 + (j + 1) * NKL * 64]], axis=2))
    d["w_glu"] = inp["w_glu"][:depth]
    d["w_out"] = inp["w_out"][:depth]
    d["w_q"] = inp["peer_wq"][:depth]
    lre = inp["lam_re"][:depth, gs]; lim = inp["lam_im"][:depth, gs]; ldt = inp["log_dt"][:depth, gs]
    lreT = lre.transpose(2, 0, 1); limT = lim.transpose(2, 0, 1)
    d["lamS_re"] = ca(np.concatenate([lreT, lreT], 0))
    d["lamS_im"] = ca(np.concatenate([limT, limT], 0))
    d["ldtS"] = ca(np.broadcast_to(ldt[None], (128, depth, NGL)))
    d["lamR_re"] = ca(np.broadcast_to(lre[None], (16, depth, NGL, 64)))
    d["lamR_im"] = ca(np.broadcast_to(lim[None], (16, depth, NGL, 64)))
    d["ldtR"] = ca(np.broadcast_to(ldt[None, :, :, None], (16, depth, NGL, 64)))
    d["bT_re"] = ca(inp["b_re"][:depth, gs].transpose(3, 0, 1, 2))
    d["bT_im"] = ca(inp["b_im"][:depth, gs].transpose(3, 0, 1, 2))
    cre = inp["c_re"][:depth, gs].transpose(3, 0, 1, 2); cim = inp["c_im"][:depth, gs].transpose(3, 0, 1, 2)
    d["cW1"] = ca(np.concatenate([cre, cim], 0))
    d["cW2"] = ca(np.concatenate([cim, cre], 0))
    d["dT"] = ca(inp["d_skip"][:depth, gs].transpose(2, 0, 1))
    d["qgT"] = ca(inp["q_gain"][:depth][:, :, None])
    d["kgT"] = ca(inp["k_gain"][:depth][:, :, None])
    d["sinksB"] = ca(np.broadcast_to(inp["sinks"][:depth, hsl][None], (128, depth, NHL)))
    slopes = np.exp2(-8.0 * np.arange(1, NH + 1, dtype=np.float64) / NH)
    kk = np.arange(128)[:, None]; qq = np.arange(128)[None, :]
    dist_prev = qq + 128 - kk
    dist_cur = qq - kk
    bt = np.full((128, NH, 256), NEG, np.float64)
    for h in range(NH):
        bt[:, h, 0:128] = np.where(dist_prev < 128, -slopes[h] * dist_prev, NEG)
        bt[:, h, 128:256] = np.where(dist_cur >= 0, -slopes[h] * dist_cur, NEG)
    d["biasT"] = ca(bt[:, hsl].astype(f))
    d["gnsT"] = ca(inp["gn_ssm"][:depth].reshape(depth, 16, 128).transpose(2, 0, 1))
    d["gnaT"] = ca(inp["gn_attn"][:depth].reshape(depth, 16, 128).transpose(2, 0, 1))
    d["k12"] = ca(np.concatenate([inp["peer_k1"][:depth].transpose(2, 0, 1), inp["peer_k2"][:depth].transpose(2, 0, 1)], 0))
    nexp = 16384 if dbg in (False, "full") else 128
    for i in range(depth):
        d["peer_u%d" % i] = inp["peer_u"][i, :nexp]
        d["peer_v%d" % i] = inp["peer_v"][i, :nexp]
    d["ident"] = np.eye(128, dtype=f)
    io = np.arange(16, dtype=f)
    d["iota16"] = ca(np.broadcast_to(np.concatenate([io, 16 * io, 16 * io + 16])[None], (128, 48)))
    out = {k: np.asarray(v, dtype=f) for k, v in d.items()}
    out["idx"] = idx
    return out


def run(inp, T, depth, dbg=False, GC=4):
    nc = build_program(T, depth, dbg, GC)
    in_maps = []
    for b in range(2):
        for j in range(GC):
            m = host_inputs(inp, T, depth, b, dbg, j, GC)
            m["idx"] = host_inputs_idx(T, j, GC)
            in_maps.append(m)
    res = run_bass_kernel_spmd(nc, in_maps, core_ids=list(range(2 * GC)))
    return res.results


def host_inputs_idx(T, j, GC):
    NB = T // 128; NBL = NB // GC
    p = np.arange(128)
    idx = np.zeros((128, NBL * 48), np.uint32)
    for t in range(NBL):
        Tg = j * NBL + t
        for c in range(32):
            idx[:, t * 48 + c] = Tg * D + c * 128 + p
        for c in range(16):
            idx[:, t * 48 + 32 + c] = Tg * 2048 + c * 128 + p
    return idx


def untile(o, T):
    NB = T // 128
    return np.ascontiguousarray(o.reshape(NB, D, 128).transpose(0, 2, 1).reshape(T, D))


GCORES = 4


def kernel(**inputs):
    inp = {k: np.asarray(v) for k, v in inputs.items()}
    T = inp["x"].shape[1]
    res = run(inp, T, DEPTH, False, GCORES)
    out = np.stack([untile(res[b * GCORES]["outT"], T) for b in range(2)], 0)
    return out.astype(np.float32)
```

```python
import numpy as np
import concourse.bass as bass
import concourse.mybir as mybir
from concourse.bass_utils import run_bass_kernel_spmd

F32 = mybir.dt.float32
U32 = mybir.dt.uint32
AF = mybir.ActivationFunctionType
ALU = mybir.AluOpType
AX = mybir.AxisListType

D = 4096
DEPTH = 4
NG = 128
NH = 32
NKV = 4
INW = 4608
EPS = 1e-6
NEG = -30000.0


class Buf:
    __slots__ = ("w", "r", "name")

    def __init__(self, name=""):
        self.w = {}
        self.r = {}
        self.name = name


class Sched:
    ENG = ("pe", "act", "dve", "pool", "sp")

    def __init__(self, nc):
        self.nc = nc
        self.q = {e: [] for e in self.ENG}
        self.sems = {}
        self.latest = {}
        self.seen = {e: {} for e in self.ENG}
        for e in ("pe", "act", "dve", "pool"):
            self.sems[e] = nc.alloc_semaphore(name="done_" + e)
            self.latest[e] = 0
        self.bufs = []

    def buf(self, name=""):
        b = Buf(name)
        self.bufs.append(b)
        return b

    def _deps(self, reads, writes):
        deps = {}
        for b in reads:
            for k, v in b.w.items():
                if deps.get(k, 0) < v:
                    deps[k] = v
        for b in writes:
            for d in (b.w, b.r):
                for k, v in d.items():
                    if deps.get(k, 0) < v:
                        deps[k] = v
        return deps

    def _wait(self, eng, deps):
        seen = self.seen[eng]
        for k, v in deps.items():
            if k == "pe" and eng == "pe":
                continue
            if seen.get(k, 0) >= v:
                continue
            seen[k] = v
            self.q[eng].append(("wait", k, v))

    def _mark(self, tok, reads, writes):
        k, v = tok
        for b in reads:
            b.r[k] = v
        for b in writes:
            b.w = {k: v}
            b.r = {}

    def op(self, eng, fn, reads=(), writes=()):
        self._wait(eng, self._deps(reads, writes))
        self.latest[eng] += 1
        self.q[eng].append(("op", fn, eng, 1))
        self._mark((eng, self.latest[eng]), reads, writes)

    def dma(self, queue, fn, reads, writes, key):
        self._wait(queue, self._deps(reads, writes))
        if key not in self.sems:
            self.sems[key] = self.nc.alloc_semaphore(name="d_" + key)
            self.latest[key] = 0
        self.latest[key] += 16
        self.q[queue].append(("op", fn, key, 16))
        self._mark((key, self.latest[key]), reads, writes)

    def cc(self, fn, reads, writes):
        key = "cc"
        self._wait("pool", self._deps(reads, writes))
        if key not in self.sems:
            self.sems[key] = self.nc.alloc_semaphore(name="d_cc")
            self.latest[key] = 0
        self.latest[key] += 1
        self.q["pool"].append(("op", fn, key, 1))
        self._mark((key, self.latest[key]), reads, writes)

    def barrier(self):
        for e in self.ENG:
            self._wait(e, dict(self.latest))
        for b in self.bufs:
            b.w = {}
            b.r = {}

    def emit(self):
        nc = self.nc
        self.barrier()
        sems = self.sems

        def replay(name, e):
            for it in self.q[name]:
                if it[0] == "wait":
                    e.wait_ge(sems[it[1]], it[2])
                else:
                    it[1](e).then_inc(sems[it[2]], it[3])

        with nc.Block() as block:
            @block.tensor
            def _(e):
                replay("pe", e)

            @block.scalar
            def _(e):
                replay("act", e)

            @block.vector
            def _(e):
                replay("dve", e)

            @block.gpsimd
            def _(e):
                replay("pool", e)

            @block.sync
            def _(e):
                replay("sp", e)


def sb_ap(t, off, dims, np_=128, pstart=0):
    fs = 1
    for s in t.shape[1:]:
        fs *= s
    return bass.AP(t, pstart * fs + off, [[fs, np_]] + [list(d) for d in dims])


def build_program(T, depth, dbg=False, GC=4):
    nc = bass.Bass("TRN2", target_bir_lowering=False)
    S = Sched(nc)
    NTB = T // 512
    NB = T // 128
    NBL = NB // GC
    NIDX = 48
    NGL = NG // GC; NHL = NH // GC; NKL = NKV // GC
    CW = NGL * 16 + NHL * 64 + 2 * NKL * 64
    QOFF = NGL * 16; KOFF = QOFF + NHL * 64; VOFF = KOFF + NKL * 64

    def dint(name, shape, dt=F32):
        return nc.dram_tensor(name, list(shape), dt, kind="Internal").ap()

    def din(name, shape, dt=F32):
        return nc.dram_tensor(name, list(shape), dt, kind="ExternalInput").ap()

    def dscr(name, shape, dt=F32):
        return nc.dram_tensor(name, list(shape), dt, kind=("ExternalOutput" if dbg else "Internal")).ap()

    xtm_in = din("xtm_in", [NB * D, 128])
    idx_in = din("idx", [128, NBL * NIDX], U32)
    cT_in = din("cT", [128, 32])
    w_ada = din("w_ada", [D, 6 * D])
    badaT_in = din("b_adaT", [128, 192])
    adaT_in = din("adaT", [128, depth, 192])
    g1T_in = din("g1T", [128, depth, 32])
    g2T_in = din("g2T", [128, depth, 32])
    w_in = din("w_in", [depth, D, CW])
    w_glu = din("w_glu", [depth, 2048, 2048])
    w_out = din("w_out", [depth, D, D])
    w_q = din("w_q", [depth, D, 1024])
    lamS_re_in = din("lamS_re", [128, depth, NGL])
    lamS_im_in = din("lamS_im", [128, depth, NGL])
    ldtS_in = din("ldtS", [128, depth, NGL])
    lamR_re_in = din("lamR_re", [16, depth, NGL, 64])
    lamR_im_in = din("lamR_im", [16, depth, NGL, 64])
    ldtR_in = din("ldtR", [16, depth, NGL, 64])
    bT_re_in = din("bT_re", [16, depth, NGL, 64])
    bT_im_in = din("bT_im", [16, depth, NGL, 64])
    cW1_in = din("cW1", [128, depth, NGL, 16])
    cW2_in = din("cW2", [128, depth, NGL, 16])
    dT_in = din("dT", [16, depth, NGL])
    qgT_in = din("qgT", [depth, 64, 1])
    kgT_in = din("kgT", [depth, 64, 1])
    sinksB_in = din("sinksB", [128, depth, NHL])
    biasT_in = din("biasT", [128, NHL, 256])
    gnsT_in = din("gnsT", [128, depth, 16])
    gnaT_in = din("gnaT", [128, depth, 16])
    k12_in = din("k12", [128, depth, 128])
    NEXP = 16384 if dbg in (False, "full") else 128
    peer_u = [din("peer_u%d" % i, [NEXP, D]) for i in range(depth)]
    peer_v = [din("peer_v%d" % i, [NEXP, D]) for i in range(depth)]
    ident_in = din("ident", [128, 128])
    iota16_in = din("iota16", [128, 48])

    outT = nc.dram_tensor("outT", [NB * D, 128], F32, kind="ExternalOutput").ap()

    XTM = dint("XTM", [NB * D, 128])
    CCin = dint("CCin", [NBL * D, 128])
    CRg = max(1, (1 << 20) // (T * 4))
    CCg = [dint("CCg0", [GC * CRg, T]), dint("CCg1", [GC * CRg, T])]
    CCout = [dint("CCout0", [GC * (D // 2), 128]), dint("CCout1", [GC * (D // 2), 128])]
    zT = dscr("zT", [CW, T])
    gTl = dint("gTl", [NGL * 16, T])
    aTl = dint("aTl", [NHL * 64, T])
    GTM = dint("GTM", [NB * 2048, 128])
    ATM = dint("ATM", [NB * 2048, 128])
    X1L = dint("X1L", [NBL * D, 128])
    QL = dint("QL", [NBL * 1024, 128])
    h2tm = dscr("h2tm", [NBL * 128, D])
    dbgR = dscr("dbgR", [T // 128, 128, 1152]) if dbg else None

    def sb(name, shape, dt=F32):
        return nc.alloc_sbuf_tensor("sb_" + name, list(shape), dt)

    ident = sb("ident", [128, 128]); b_ident = S.buf()
    ones = sb("ones", [128, 128]); b_ones = S.buf()
    iota16 = sb("iota16", [128, 48]); b_iota = S.buf()
    condT = sb("condT", [128, 192]); b_cond = S.buf()
    modT = sb("modT", [128, 192]); b_mod = S.buf()
    AB = sb("AB", [128, 4, 32]); b_AB = S.buf()
    gpar = sb("gpar", [128, 2, 32]); b_gpar = S.buf()
    small = sb("small", [128, 64]); b_small = S.buf()
    BA = sb("BA", [128, 16384]); b_BA = S.buf("BA")
    BBC = sb("BBC", [128, 16384])

    class _View:
        def __init__(self, t, off):
            self.t = t; self.off = off

        def __getitem__(self, idx):
            p, c = idx
            return self.t[p, slice(c.start + self.off, c.stop + self.off)]
    BB = _View(BBC, 0); b_BB = S.buf("BB")
    BC = _View(BBC, 8192); b_BC = S.buf("BC")
    RSTD = sb("RSTD", [128, 512]); b_RSTD = S.buf("RSTD")
    k12 = sb("k12", [64, 256]); b_k12 = S.buf("k12")
    dtmp = sb("dtmp", [128, 8]); b_dtmp = S.buf("dtmp")
    IDX = sb("IDX", [128, NBL * NIDX], U32); b_IDX = S.buf("IDX")
    WT = [sb("WT0", [128, 32, 128]), sb("WT1", [128, 32, 128])]
    b_WT = [S.buf("WT0"), S.buf("WT1")]
    TM = [sb("TM%d" % i, [128, 512]) for i in range(8)]
    b_TM = [S.buf("TM%d" % i) for i in range(8)]
    PS = [nc.alloc_psum_tensor("ps%d" % i, [128, 512], F32) for i in range(8)]
    b_PS = [S.buf("ps%d" % i) for i in range(8)]
    st = {"ps": 0, "tm": 0, "wt": 0}

    def nps():
        i = st["ps"]; st["ps"] = (i + 1) % 8
        return PS[i], b_PS[i]

    def ntm():
        i = st["tm"]; st["tm"] = (i + 1) % 8
        return TM[i], b_TM[i]

    def dma(out, in_, reads, writes, key, queue="sp"):
        S.dma(queue, lambda e, o=out, i=in_: e.dma_start(out=o, in_=i), reads, writes, key)

    def act(out, in_, func, reads, writes, bias=None, scale=None, accum=None):
        kw = {}
        if bias is not None:
            kw["bias"] = bias
        if scale is not None:
            kw["scale"] = scale
        if accum is not None:
            kw["accum_out"] = accum
        S.op("act", lambda e, o=out, i=in_, f=func, kw=kw: e.activation(out=o, in_=i, func=f, **kw), reads, writes)

    def tt(out, a, b, op, reads, writes, eng="dve"):
        S.op(eng, lambda e, o=out, a=a, b=b, op=op: e.tensor_tensor(out=o, in0=a, in1=b, op=op), reads, writes)

    def ts(out, a, s1, s2, op0, op1, reads, writes, eng="dve"):
        if op1 is None:
            S.op(eng, lambda e, o=out, a=a, s1=s1, op0=op0: e.tensor_scalar(out=o, in0=a, scalar1=s1, scalar2=None, op0=op0), reads, writes)
        else:
            S.op(eng, lambda e, o=out, a=a, s1=s1, s2=s2, op0=op0, op1=op1: e.tensor_scalar(out=o, in0=a, scalar1=s1, scalar2=s2, op0=op0, op1=op1), reads, writes)

    def stt(out, a, s, b, op0, op1, reads, writes, eng="dve"):
        S.op(eng, lambda e, o=out, a=a, s=s, b=b, op0=op0, op1=op1: e.scalar_tensor_tensor(out=o, in0=a, scalar=s, in1=b, op0=op0, op1=op1), reads, writes)

    def cp(out, in_, reads, writes, eng="dve"):
        S.op(eng, lambda e, o=out, i=in_: e.tensor_copy(out=o, in_=i), reads, writes)

    def recip(out, in_, reads, writes):
        S.op("dve", lambda e, o=out, i=in_: e.reciprocal(out=o, in_=i), reads, writes)

    def mm(out, lhsT, rhs, start, stop, reads, writes):
        S.op("pe", lambda e, o=out, l=lhsT, r=rhs, s0=start, s1=stop: e.matmul(o, lhsT=l, rhs=r, start=s0, stop=s1), reads, writes)

    def tr(out, in_, idn, reads, writes):
        S.op("pe", lambda e, o=out, i=in_, d=idn: e.transpose(o, i, d), reads, writes)

    def memset(ap, val, writes, eng="dve"):
        S.op(eng, lambda e, a=ap, v=val: e.memset(a, v), [], writes)

    def rms_rstd(blk, b_blk, c0, nchunk, N, nfeat, rstd_ap, b_rstd):
        ps, bp = nps()
        for i in range(nchunk):
            sq, bsq = ntm()
            act(sq[:, 0:N], blk[:, c0 + i, 0:N], AF.Square, [b_blk], [bsq])
            mm(ps[:, 0:N], ones[:, :], sq[:, 0:N], i == 0, i == nchunk - 1, [b_ones, bsq], [bp])
        t1, bt1 = ntm()
        act(t1[:, 0:N], ps[:, 0:N], AF.Sqrt, [bp], [bt1], bias=small[:, 0:1], scale=1.0 / nfeat)
        recip(rstd_ap, t1[:, 0:N], [bt1, b_small], [b_rstd])

    def gemm(Wd, KC, ntiles, rhs_fn, rhs_bufs, N, evac):
        def load(j):
            s = st["wt"]; st["wt"] ^= 1
            dma(WT[s][:, 0:KC, :], Wd[:, j * 128:(j + 1) * 128].rearrange("(c p) n -> p c n", p=128),
                [], [b_WT[s]], "wt%d" % s)
            return s
        nxt = load(ntiles[0])
        for idx, j in enumerate(ntiles):
            s = nxt
            if idx + 1 < len(ntiles):
                nxt = load(ntiles[idx + 1])
            ps, bp = nps()
            for c in range(KC):
                mm(ps[:, 0:N], WT[s][:, c, :], rhs_fn(c), c == 0, c == KC - 1, [b_WT[s]] + rhs_bufs, [bp])
            evac(j, ps, bp)

    def xtile(T_):
        return XTM[T_ * D:(T_ + 1) * D, :].rearrange("(c p) t -> p c t", p=128)

    def xload(blk, tb, bufs, key):
        for q_ in range(4):
            dma(blk[:, :, q_ * 128:(q_ + 1) * 128], xtile(4 * tb + q_), [], bufs, key)

    def xstore(blk, tb, bufs, key):
        for q_ in range(4):
            dma(xtile(4 * tb + q_), blk[:, :, q_ * 128:(q_ + 1) * 128], bufs, [], key)

    def igather(out_, tab_, col_, bufs, key):
        S.dma("pool", lambda e, o=out_, tb_=tab_, ix=IDX[:, col_:col_ + 1]: e.indirect_dma_start(
            out=o, out_offset=None, in_=tb_, in_offset=bass.IndirectOffsetOnAxis(ap=ix, axis=0)),
            [b_IDX], bufs, key)

    dma(ident[:, :], ident_in, [], [b_ident], "c0")
    dma(iota16[:, :], iota16_in, [], [b_iota], "c1")
    memset(ones[:, :], 1.0, [b_ones])
    memset(small[:, 0:1], EPS, [b_small])
    memset(small[:, 1:2], float(np.pi / 2), [b_small])
    dma(XTM, xtm_in, [], [S.buf()], "xcp")
    dma(IDX[:, :], idx_in, [], [b_IDX], "c1")
    S.barrier()

    cT = BB[:, 0:32]
    dma(cT, cT_in, [], [b_BB], "ld0")
    scT = BB[:, 32:64]
    act(scT, cT, AF.Silu, [b_BB], [b_BB])
    psc, bpc = nps()

    def cond_evac(j, ps, bp):
        cp(condT[:, j:j + 1], ps[:, 0:1], [bp], [b_cond])
    gemm(w_ada, 32, list(range(192)), lambda c: BB[:, 32 + c:33 + c], [b_BB], 1, cond_evac)
    dma(BC[:, 0:192], badaT_in, [], [b_BC], "ld0")
    tt(condT[:, :], condT[:, :], BC[:, 0:192], ALU.add, [b_cond, b_BC], [b_cond])
    S.barrier()

    for l in range(depth):
        last = l == depth - 1
        dma(BC[:, 0:192], adaT_in[:, l, :], [], [b_BC], "ld0")
        dma(gpar[:, 0, :], g1T_in[:, l, :], [], [b_gpar], "ld1")
        dma(gpar[:, 1, :], g2T_in[:, l, :], [], [b_gpar], "ld1")
        tt(modT[:, :], condT[:, :], BC[:, 0:192], ALU.add, [b_cond, b_BC], [b_mod])
        stt(AB[:, 0, :], modT[:, 32:64], 1.0, gpar[:, 0, :], ALU.add, ALU.mult, [b_mod, b_gpar], [b_AB])
        cp(AB[:, 1, :], modT[:, 0:32], [b_mod], [b_AB])
        stt(AB[:, 2, :], modT[:, 128:160], 1.0, gpar[:, 1, :], ALU.add, ALU.mult, [b_mod, b_gpar], [b_AB])
        cp(AB[:, 3, :], modT[:, 96:128], [b_mod], [b_AB])
        S.barrier()

        xblk = BA[:, :].rearrange("p (c t) -> p c t", c=32)
        rstd = BB[:, 0:512]
        for tb in range(NTB):
            t0 = tb * 512
            xload(xblk, tb, [b_BA], "ldA")
            rms_rstd(xblk, b_BA, 0, 32, 512, D, rstd, b_BB)
            for c in range(32):
                tt(xblk[:, c, :], xblk[:, c, :], rstd, ALU.mult, [b_BA, b_BB], [b_BA])
                act(xblk[:, c, :], xblk[:, c, :], AF.Identity, [b_BA, b_AB], [b_BA],
                    bias=AB[:, 1, c:c + 1], scale=AB[:, 0, c:c + 1])

            def ev1(j, ps, bp, t0=t0):
                o, bo = ntm()
                act(o[:, :], ps[:, :], AF.Copy, [bp], [bo])
                dma(zT[j * 128:(j + 1) * 128, t0:t0 + 512], o[:, :], [bo], [], "stz")
            gemm(w_in[l], 32, list(range(CW // 128)), lambda c: xblk[:, c, :], [b_BA], 512, ev1)
        S.barrier()
        if dbg == "p1":
            break

        LG = T.bit_length() - 1
        CT = BA[:, 0:T]; ST_ = BA[:, 4096:4096 + T]; XT = BA[:, 8192:8192 + T]; SS = BA[:, 12288:12288 + T]
        b_TB = S.buf("tables"); b_XT = S.buf("xt"); b_SS = S.buf("ss"); b_U = S.buf("U"); b_YB = S.buf("YB")
        b_PR = S.buf("params"); b_W12 = S.buf("W12"); b_BBc = S.buf("BBc"); b_DD = S.buf("DD")
        U = BB[0:16, 0:T]; YB = BB[0:16, 4096:4096 + T]

        def P(k):
            return BC[:, k * 128:(k + 1) * 128]
        lre, lim, ldt, rr, th, cc, sn, t1_, t2_ = [P(k) for k in range(9)]
        PWc = BC[:, 2048:3584].rearrange("p (k g) -> p k g", k=12)
        PWs = BC[:, 3584:5120].rearrange("p (k g) -> p k g", k=12)
        W1 = WT[0][:, 0:16, :].rearrange("p a (b h) -> p (a b) h", h=16)
        W2 = WT[0][:, 16:32, :].rearrange("p a (b h) -> p (a b) h", h=16)
        BB1 = WT[1][0:16, 0:16, :]
        BB2 = WT[1][0:16, 16:32, :]
        Dd = BC[0:16, 5120:7168].rearrange("p (g h) -> p g h", h=16)
        dTs = BC[0:16, 7168:7296]
        dma(BC[:, 0:NGL], lamS_re_in[:, l, :], [], [b_PR], "ld0")
        dma(BC[:, 128:128 + NGL], lamS_im_in[:, l, :], [], [b_PR], "ld0")
        dma(BC[:, 256:256 + NGL], ldtS_in[:, l, :], [], [b_PR], "ld0")
        dma(W1[:, 0:NGL, :], cW1_in[:, l, :, :], [], [b_W12], "ld1")
        dma(W2[:, 0:NGL, :], cW2_in[:, l, :, :], [], [b_W12], "ld1")
        dma(BC[0:16, 7168:7168 + NGL], dT_in[:, l, :], [], [b_DD], "ld2")
        ts(WT[0][64:128, 0:16, :], WT[0][64:128, 0:16, :], -1.0, None, ALU.mult, None, [b_W12], [b_W12])
        tt(Dd, sb_ap(ident, 0, [[0, 128], [1, 16]], np_=16),
           bass.AP(BBC, 8192 + 7168, [[16384, 16], [1, 128], [0, 16]]), ALU.mult, [b_ident, b_DD], [b_DD])
        pr = [b_PR]
        act(t1_, ldt, AF.Exp, pr, pr)
        tt(t2_, lre, t1_, ALU.mult, pr, pr)
        act(rr, t2_, AF.Exp, pr, pr)
        tt(th, lim, t1_, ALU.mult, pr, pr)
        act(cc, th, AF.Sin, pr, pr, bias=small[:, 1:2], scale=-0.125)
        act(sn, th, AF.Sin, pr, pr, scale=0.125)

        def csq(c_, s_, a_, b_, bufs, eng="dve"):
            tt(a_, c_, c_, ALU.mult, bufs, bufs, eng)
            tt(b_, s_, s_, ALU.mult, bufs, bufs, eng)
            stt(s_, c_, 2.0, s_, ALU.mult, ALU.mult, bufs, bufs)
            tt(c_, a_, b_, ALU.subtract, bufs, bufs, eng)
        for _ in range(3):
            csq(cc, sn, t1_, t2_, pr)
        cp(PWc[:, 0, :], cc, pr, pr)
        ts(PWs[:, 0, :], sn, -1.0, None, ALU.mult, None, pr, pr)
        for k in range(1, LG):
            tt(t1_, PWc[:, k - 1, :], PWc[:, k - 1, :], ALU.mult, pr, pr)
            tt(t2_, PWs[:, k - 1, :], PWs[:, k - 1, :], ALU.mult, pr, pr)
            tt(PWc[:, k, :], t1_, t2_, ALU.subtract, pr, pr)
            stt(PWs[:, k, :], PWc[:, k - 1, :], 2.0, PWs[:, k - 1, :], ALU.mult, ALU.mult, pr, pr)

        for gc in range(NGL // 16):
            g0 = gc * 16
            A = [BA[0:16, k * 1024:(k + 1) * 1024] for k in range(16)]
            ba = [b_TB, b_XT, b_SS]

            def v3(a):
                return a.rearrange("p (g q) -> p g q", q=64)
            dma(v3(A[0]), lamR_re_in[:, l, g0:g0 + 16, :], [], ba, "ld0")
            dma(v3(A[1]), lamR_im_in[:, l, g0:g0 + 16, :], [], ba, "ld0")
            dma(v3(A[2]), ldtR_in[:, l, g0:g0 + 16, :], [], ba, "ld0")
            dma(v3(A[3]), bT_re_in[:, l, g0:g0 + 16, :], [], ba, "ld0")
            dma(v3(A[4]), bT_im_in[:, l, g0:g0 + 16, :], [], ba, "ld0")
            act(A[5], A[2], AF.Exp, ba, ba)
            tt(A[6], A[0], A[5], ALU.mult, ba, ba)
            act(A[6], A[6], AF.Exp, ba, ba)
            tt(A[7], A[1], A[5], ALU.mult, ba, ba)
            act(A[8], A[7], AF.Sin, ba + [b_small], ba, bias=small[0:16, 1:2], scale=-0.125)
            act(A[9], A[7], AF.Sin, ba, ba, scale=0.125)
            for _ in range(3):
                csq(A[8], A[9], A[10], A[11], ba)
            tt(A[8], A[6], A[8], ALU.mult, ba, ba)
            tt(A[9], A[6], A[9], ALU.mult, ba, ba)
            tt(A[10], A[0], A[0], ALU.mult, ba, ba)
            tt(A[11], A[1], A[1], ALU.mult, ba, ba)
            tt(A[10], A[10], A[11], ALU.add, ba, ba)
            recip(A[10], A[10], ba, ba)
            ts(A[8], A[8], -1.0, None, ALU.add, None, ba, ba)
            tt(A[11], A[8], A[0], ALU.mult, ba, ba)
            tt(A[12], A[9], A[1], ALU.mult, ba, ba)
            tt(A[11], A[11], A[12], ALU.add, ba, ba)
            tt(A[11], A[11], A[10], ALU.mult, ba, ba)
            tt(A[12], A[9], A[0], ALU.mult, ba, ba)
            tt(A[13], A[8], A[1], ALU.mult, ba, ba)
            tt(A[12], A[12], A[13], ALU.subtract, ba, ba)
            tt(A[12], A[12], A[10], ALU.mult, ba, ba)
            tt(A[13], A[11], A[3], ALU.mult, ba, ba)
            tt(A[14], A[12], A[4], ALU.mult, ba, ba)
            tt(A[13], A[13], A[14], ALU.subtract, ba, ba)
            tt(A[14], A[11], A[4], ALU.mult, ba, ba)
            tt(A[15], A[12], A[3], ALU.mult, ba, ba)
            tt(A[14], A[14], A[15], ALU.add, ba, ba)
            cp(BB1[:, :, 0:64], v3(A[13]), ba, [b_BBc])
            cp(BB1[:, :, 64:128], v3(A[14]), ba, [b_BBc])
            ts(BB2[:, :, 0:64], v3(A[14]), -1.0, None, ALU.mult, None, ba, [b_BBc])
            cp(BB2[:, :, 64:128], v3(A[13]), ba, [b_BBc])

            for gi in range(16):
                g = g0 + gi
                dma(U, zT[16 * g:16 * g + 16, 0:T], [], [b_U], "ldU")
                memset(CT[:, 0:1], 1.0, [b_TB])
                memset(ST_[:, 0:1], 0.0, [b_TB])
                for k in range(LG):
                    n = 1 << k
                    pc = PWc[:, k, g:g + 1]; ps_ = PWs[:, k, g:g + 1]
                    ta = BA[:, 8192:8192 + n]; tb_ = BA[:, 10240:10240 + n]
                    ts(ta, ST_[:, 0:n], ps_, None, ALU.mult, None, [b_TB, b_PR], [b_XT])
                    stt(CT[:, n:2 * n], CT[:, 0:n], pc, ta, ALU.mult, ALU.subtract, [b_TB, b_PR, b_XT], [b_TB])
                    ts(tb_, CT[:, 0:n], ps_, None, ALU.mult, None, [b_TB, b_PR], [b_XT])
                    stt(ST_[:, n:2 * n], ST_[:, 0:n], pc, tb_, ALU.mult, ALU.add, [b_TB, b_PR, b_XT], [b_TB])
                for i in range(NTB):
                    sl = slice(i * 512, (i + 1) * 512)
                    p1, bp1 = nps(); p2, bp2 = nps()
                    mm(p1[:, :], BB1[:, gi, :], U[:, sl], True, True, [b_BBc, b_U], [bp1])
                    mm(p2[:, :], BB2[:, gi, :], U[:, sl], True, True, [b_BBc, b_U], [bp2])
                    tmp, btmp = ntm()
                    tt(tmp[:, :], CT[:, sl], p1[:, :], ALU.mult, [b_TB, bp1], [btmp])
                    tt(XT[:, sl], ST_[:, sl], p2[:, :], ALU.mult, [b_TB, bp2], [b_XT])
                    tt(XT[:, sl], XT[:, sl], tmp[:, :], ALU.add, [b_XT, btmp], [b_XT])
                rb = bass.AP(BBC, 8192 + 3 * 128 + g, [[16384, 128], [0, T]])
                S.op("dve", lambda e, o=SS, d0=rb, d1=XT: e.tensor_tensor_scan(out=o, data0=d0, data1=d1, initial=0.0, op0=ALU.mult, op1=ALU.add),
                     [b_PR, b_XT], [b_SS])
                for i in range(NTB):
                    sl = slice(i * 512, (i + 1) * 512)
                    q1, bq1 = ntm(); q2, bq2 = ntm()
                    tt(q1[:, :], CT[:, sl], SS[:, sl], ALU.mult, [b_TB, b_SS], [bq1], "pool")
                    tt(q2[:, :], ST_[:, sl], SS[:, sl], ALU.mult, [b_TB, b_SS], [bq2], "pool")
                    py, bpy = nps()
                    mm(py[0:16, :], W1[:, g, :], q1[:, :], True, False, [b_W12, bq1], [bpy])
                    mm(py[0:16, :], W2[:, g, :], q2[:, :], False, False, [b_W12, bq2], [bpy])
                    mm(py[0:16, :], Dd[:, g, :], U[:, sl], False, True, [b_DD, b_U], [bpy])
                    act(YB[:, sl], py[0:16, :], AF.Gelu_apprx_tanh, [bpy], [b_YB])
                dma(gTl[16 * g:16 * g + 16, 0:T], YB, [b_YB], [], "stg")
        S.barrier()
        if dbg == "p2a":
            break

        b_bias = S.buf(); b_es = S.buf(); b_kh = S.buf(); b_va = S.buf(); b_qh = S.buf(); b_raw = S.buf()
        b_vr = S.buf(); b_yh = S.buf(); b_yT = S.buf(); b_g = S.buf()
        biasT = BBC[:, 8192:8192 + NHL * 256].rearrange("p (h k) -> p h k", k=256)
        khT = BB[0:64, 0:T]
        Vaug = BBC[:, 4096:4096 + NB * 65].rearrange("p (n d) -> p n d", d=65)
        qhT = BA[0:64, 0:T]; RAW = BA[0:64, 4096:4096 + T]; VR = BA[0:64, 8192:8192 + T]
        yh = BA[:, 12288:12288 + NB * 64].rearrange("p (n d) -> p n d", d=64)
        yhT = WT[0][0:64, :, :].rearrange("p a b -> p (a b)")
        esink = small[:, 16:16 + NHL]
        dma(biasT, biasT_in, [], [b_bias], "ld0")
        dma(esink, sinksB_in[:, l, :], [], [b_es], "ld1")
        act(esink, esink, AF.Exp, [b_es], [b_es])
        dma(small[0:64, 48:49], qgT_in[l], [], [b_g], "ld2")
        dma(small[0:64, 49:50], kgT_in[l], [], [b_g], "ld2")
        memset(Vaug[:, :, 64:65], 1.0, [b_va])

        def qknorm(dst, b_dst, gcol):
            for i in range(NTB):
                sl = slice(i * 512, (i + 1) * 512)
                sq, bsq = ntm()
                act(sq[0:64, :], RAW[:, sl], AF.Square, [b_raw], [bsq])
                ps, bp = nps()
                mm(ps[0:64, :], ones[0:64, 0:64], sq[0:64, :], True, True, [b_ones, bsq], [bp])
                t1, bt1 = ntm()
                act(t1[0:64, :], ps[0:64, :], AF.Sqrt, [bp, b_small], [bt1], bias=small[0:64, 0:1], scale=1.0 / 64)
                t2, bt2 = ntm()
                recip(t2[0:64, :], t1[0:64, :], [bt1], [bt2])
                stt(dst[:, sl], RAW[:, sl], small[0:64, gcol:gcol + 1], t2[0:64, :], ALU.mult, ALU.mult, [b_raw, b_g, bt2], [b_dst])

        for kv in range(NKL):
            dma(RAW, zT[KOFF + 64 * kv:KOFF + 64 * kv + 64, 0:T], [], [b_raw], "ldA")
            qknorm(khT, b_kh, 49)
            dma(VR, zT[VOFF + 64 * kv:VOFF + 64 * kv + 64, 0:T], [], [b_vr], "ldB")
            for n in range(NB):
                if n % 4 == 0:
                    pv, bpv = nps()
                tr(pv[:, (n % 4) * 64:(n % 4) * 64 + 64], VR[:, n * 128:(n + 1) * 128], ident[0:64, 0:64], [b_vr, b_ident], [bpv])
                if n % 4 == 3:
                    cp(Vaug[:, n - 3:n + 1, 0:64], pv[:, 0:256].rearrange("p (n d) -> p n d", d=64), [bpv], [b_va])
            for hq in range(8):
                h = kv * 8 + hq
                dma(RAW, zT[QOFF + 64 * h:QOFF + 64 * h + 64, 0:T], [], [b_raw], "ldA")
                qknorm(qhT, b_qh, 48)
                for n in range(NB):
                    blk = slice(n * 128, (n + 1) * 128)
                    sc, bsc = nps()
                    if n > 0:
                        mm(sc[:, 0:128], khT[:, (n - 1) * 128:n * 128], qhT[:, blk], True, True, [b_kh, b_qh], [bsc])
                    mm(sc[:, 128:256], khT[:, blk], qhT[:, blk], True, True, [b_kh, b_qh], [bsc])
                    cols = slice(0, 256) if n > 0 else slice(128, 256)
                    s_sb, bs = ntm()
                    stt(s_sb[:, cols], sc[:, cols], 0.125, biasT[:, h, cols], ALU.mult, ALU.add, [bsc, b_bias], [bs])
                    e_sb, be = ntm()
                    act(e_sb[:, cols], s_sb[:, cols], AF.Exp, [bs], [be])
                    po, bpo = nps()
                    if n > 0:
                        mm(po[:, 0:65], e_sb[:, 0:128], Vaug[:, n - 1, :], True, False, [be, b_va], [bpo])
                    mm(po[:, 0:65], e_sb[:, 128:256], Vaug[:, n, :], n == 0, True, [be, b_va], [bpo])
                    tt(dtmp[:, 0:1], po[:, 64:65], esink[:, h:h + 1], ALU.add, [bpo, b_es], [b_dtmp])
                    recip(dtmp[:, 1:2], dtmp[:, 0:1], [b_dtmp], [b_dtmp])
                    ts(yh[:, n, :], po[:, 0:64], dtmp[:, 1:2], None, ALU.mult, None, [bpo, b_dtmp], [b_yh])
                for n in range(NB):
                    if n % 4 == 0:
                        pt, bpt = nps()
                    tr(pt[0:64, (n % 4) * 128:(n % 4) * 128 + 128], yh[:, n, :], ident[:, :], [b_yh, b_ident], [bpt])
                    if n % 4 == 3:
                        act(yhT[:, (n - 3) * 128:(n + 1) * 128], pt[0:64, :], AF.Copy, [bpt], [b_yT])
                dma(aTl[64 * h:64 * h + 64, 0:T], yhT[:, 0:T], [b_yT], [], "sta")
        S.barrier()
        groups = [list(range(g_ * GC, (g_ + 1) * GC)) for g_ in range(2)]
        b_cco = [S.buf(), S.buf()]
        CR = max(1, (1 << 20) // (T * 4))
        cnt_ = 0
        for (srcT, dstT, rows_l) in ((gTl, GTM, NGL * 16), (aTl, ATM, NHL * 64)):
            for r0 in range(0, rows_l, CR):
                i_ = cnt_ % 2; cnt_ += 1
                co = CCg[i_]
                S.cc(lambda e, gr=groups, a=srcT[r0:r0 + CR, :], o=co: e.collective_compute("AllGather", ALU.bypass, replica_groups=gr, ins=[a], outs=[o]),
                     [], [b_cco[i_]])
                for r_ in range(GC):
                    dma(dstT.rearrange("(n r) t -> r n t", r=2048)[r_ * rows_l + r0:r_ * rows_l + r0 + CR, :, :],
                        co[r_ * CR:(r_ + 1) * CR, :].rearrange("r (n t) -> r n t", t=128), [b_cco[i_]], [], "stcg")
        S.barrier()
        if dbg == "p2":
            break

        b_CLO = S.buf(); b_CHI = S.buf(); b_gn = S.buf()
        xblk = BA[:, :].rearrange("p (c t) -> p c t", c=32)
        cat = BBC[:, :].rearrange("p (c t) -> p c t", c=32)
        gns = small[:, 16:32]; gna = small[:, 32:48]
        dma(gns, gnsT_in[:, l, :], [], [b_gn], "ld0")
        dma(gna, gnaT_in[:, l, :], [], [b_gn], "ld0")
        for tb in range(NBL // 4):
            t0 = tb * 512
            for q_ in range(4):
                ibq = (tb * 4 + q_) * NIDX
                for c in range(32):
                    igather(xblk[:, c, q_ * 128:(q_ + 1) * 128], XTM, ibq + c, [b_BA], "gx")
                for c in range(16):
                    igather(cat[:, 16 + c, q_ * 128:(q_ + 1) * 128], GTM, ibq + 32 + c, [b_CHI], "gg")

            def ev_glu(j, ps, bp):
                sg, bsg = ntm()
                act(sg[:, :], ps[:, :], AF.Sigmoid, [bp], [bsg])
                tt(cat[:, j, :], cat[:, 16 + j, :], sg[:, :], ALU.mult, [b_CHI, bsg], [b_CLO])
            gemm(w_glu[l], 16, list(range(16)), lambda c: cat[:, 16 + c, :], [b_CHI], 512, ev_glu)
            for q_ in range(4):
                ibq = (tb * 4 + q_) * NIDX
                for c in range(16):
                    igather(cat[:, 16 + c, q_ * 128:(q_ + 1) * 128], ATM, ibq + 32 + c, [b_CHI], "gg")
            rms_rstd(cat, b_CLO, 0, 16, 512, 2048, RSTD[:, :], b_RSTD)
            for c in range(16):
                stt(cat[:, c, :], cat[:, c, :], gns[:, c:c + 1], RSTD[:, :], ALU.mult, ALU.mult, [b_CLO, b_gn, b_RSTD], [b_CLO])
            rms_rstd(cat, b_CHI, 16, 16, 512, 2048, RSTD[:, :], b_RSTD)
            for c in range(16):
                stt(cat[:, 16 + c, :], cat[:, 16 + c, :], gna[:, c:c + 1], RSTD[:, :], ALU.mult, ALU.mult, [b_CHI, b_gn, b_RSTD], [b_CHI])

            def ev_out(j, ps, bp):
                stt(xblk[:, j, :], ps[:, :], modT[:, 64 + j:65 + j], xblk[:, j, :], ALU.mult, ALU.add, [bp, b_mod, b_BA], [b_BA])
            gemm(w_out[l], 32, list(range(32)), lambda c: cat[:, c, :], [b_CLO, b_CHI], 512, ev_out)
            for q_ in range(4):
                tl_ = tb * 4 + q_
                dma(X1L[tl_ * D:(tl_ + 1) * D, :].rearrange("(c p) t -> p c t", p=128), xblk[:, :, q_ * 128:(q_ + 1) * 128], [b_BA], [], "stx")
            rms_rstd(xblk, b_BA, 0, 32, 512, D, RSTD[:, :], b_RSTD)
            bh = [b_CLO, b_CHI]
            for c in range(32):
                tt(cat[:, c, :], xblk[:, c, :], RSTD[:, :], ALU.mult, [b_BA, b_RSTD], bh)
                act(cat[:, c, :], cat[:, c, :], AF.Identity, bh + [b_AB], bh, bias=AB[:, 3, c:c + 1], scale=AB[:, 2, c:c + 1])

            def ev_q(j, ps, bp, t0=t0):
                o, bo = ntm()
                act(o[:, :], ps[:, :], AF.Copy, [bp], [bo])
                for q_ in range(4):
                    T_ = t0 // 128 + q_
                    dma(QL[T_ * 1024 + j * 128:T_ * 1024 + (j + 1) * 128, :], o[:, q_ * 128:(q_ + 1) * 128], [bo], [], "stq")
            gemm(w_q[l], 32, list(range(8)), lambda c: cat[:, c, :], bh, 512, ev_q)
            for t4 in range(4):
                s_ = st["wt"]; st["wt"] ^= 1
                ROW = WT[s_][:, :, :].rearrange("p a b -> p (a b)")
                for c in range(32):
                    if c % 4 == 0:
                        pt, bpt = nps()
                    tr(pt[:, (c % 4) * 128:(c % 4) * 128 + 128], cat[:, c, t4 * 128:(t4 + 1) * 128], ident[:, :], bh + [b_ident], [bpt])
                    if c % 4 == 3:
                        act(ROW[:, (c - 3) * 128:(c + 1) * 128], pt[:, :], AF.Copy, [bpt], [b_WT[s_]])
                dma(h2tm[t0 + t4 * 128:t0 + (t4 + 1) * 128, :], ROW, [b_WT[s_]], [], "sth")
        S.barrier()
        if dbg == "p4":
            break

        b_G = [S.buf() for _ in range(4)]
        G = BA[:, :].rearrange("p (s d) -> p s d", s=4)
        h2t = BBC[:, 0:4096]; b_h2 = S.buf()
        acc = BBC[:, 4096:8192]; b_acc = S.buf()
        x1t = BA[:, 0:4096].rearrange("p (c t) -> p c t", c=32); b_x1 = b_G[0]
        RT0 = 11264
        qtile = BBC[0:64, RT0:RT0 + 2048].rearrange("p (s h t) -> p s h t", s=2, h=8); b_qt = S.buf()
        v12 = BBC[:, RT0 + 2048:RT0 + 2304]; b_v12 = S.buf()
        i12f = BBC[:, RT0 + 2304:RT0 + 2560]; b_i12f = S.buf()
        tmpS = BBC[:, RT0 + 2560:RT0 + 2688]; b_tmpS = S.buf()
        cand = BBC[:, RT0 + 2688:RT0 + 4736]; b_cand = S.buf()
        vs = BBC[:, RT0 + 4736:RT0 + 4864]; b_vs = S.buf()
        misc = BBC[:, RT0 + 4864:RT0 + 4992]; b_misc = S.buf()
        WF = WT[0][:, :, :].rearrange("p a b -> p (a b)")
        ev = WF[:, 0:128]; gates = WF[:, 128:256]; af = WF[:, 256:384]; bf = WF[:, 384:512]
        iaf = WF[:, 512:640]; ibf = WF[:, 640:768]; idsf = WF[:, 768:896]; actv = WF[:, 896:1024]; wv = WF[:, 1024:1152]
        b_r = S.buf()
        WF1 = WT[1][:, :, :].rearrange("p a b -> p (a b)")
        EQ = WF1[:, 0:2048]; EQ2 = WF1[:, 2048:4096]; b_eq = S.buf()
        i12u = WF[:, 1152:1408].bitcast(U32); b_i12u = S.buf()
        icu = WF[:, 1408:1536].bitcast(U32); b_icu = S.buf()
        idsu = WF[:, 1536:1664].bitcast(U32); b_ids = S.buf()
        dma(k12[0:64, 0:128], k12_in[0:64, l, :], [], [b_k12], "ld0")
        dma(k12[0:64, 128:256], k12_in[64:128, l, :], [], [b_k12], "ld0")

        def fr(ap_):
            return ap_.tensor, ap_.offset

        def ap4(ap_, off, d1, d2, d3):
            t_, o_ = fr(ap_)
            return bass.AP(t_, o_ + off, [list(ap_.ap[0]), d1, d2, d3])

        def ap3(ap_, off, d1, d2):
            t_, o_ = fr(ap_)
            return bass.AP(t_, o_ + off, [list(ap_.ap[0]), d1, d2])

        for tile_i in range(NBL):
            QG = BBC[:, RT0:RT0 + 2048].rearrange("p (a t) -> p a t", a=16)

            dma(h2t, h2tm[tile_i * 128:(tile_i + 1) * 128, :], [], [b_h2], "ldB")
            for hs in range(16):
                r0q = tile_i * 1024 + (hs // 2) * 128 + (hs % 2) * 64
                dma(QG[0:64, hs, :], QL[r0q:r0q + 64, :], [], [b_qt], "ldA")
            banks = [nps() for _ in range(4)]
            for hs in range(16):
                h, side = hs // 2, hs % 2
                pb, bpb = banks[hs // 4]
                col = (hs % 4) * 128
                mm(pb[:, col:col + 128], QG[0:64, hs, :], k12[0:64, side * 128:(side + 1) * 128], True, True,
                   [b_qt, b_k12], [bpb])
            for bi_ in range(4):
                act(WF1[:, bi_ * 512:(bi_ + 1) * 512], banks[bi_][0][:, :], AF.Copy, [banks[bi_][1]], [b_eq])
            for hs in range(16):
                bpb = b_eq
                src = WF1[:, hs * 128:hs * 128 + 128]
                va = v12[:, hs * 16:hs * 16 + 8]; vb = v12[:, hs * 16 + 8:hs * 16 + 16]
                ia_ = i12u[:, hs * 16:hs * 16 + 8]; ib_ = i12u[:, hs * 16 + 8:hs * 16 + 16]
                S.op("dve", lambda e, o=va, i=src: e.max(out=o, in_=i), [bpb], [b_v12])
                S.op("dve", lambda e, o=ia_, m=va, i=src: e.max_index(out=o, in_max=m, in_values=i), [bpb, b_v12], [b_i12u])
                S.op("dve", lambda e, o=tmpS, m=va, i=src: e.match_replace(out=o, in_to_replace=m, in_values=i, imm_value=-1e30), [bpb, b_v12], [b_tmpS])
                S.op("dve", lambda e, o=vb, i=tmpS: e.max(out=o, in_=i), [b_tmpS], [b_v12])
                S.op("dve", lambda e, o=ib_, m=vb, i=tmpS: e.max_index(out=o, in_max=m, in_values=i), [b_tmpS, b_v12], [b_i12u])
            tt(ap4(cand, 0, [256, 8], [16, 16], [1, 16]), ap4(v12, 0, [32, 8], [1, 16], [0, 16]), ap4(v12, 16, [32, 8], [0, 16], [1, 16]),
               ALU.add, [b_v12], [b_cand])
            for h in range(8):
                ch = cand[:, h * 256:(h + 1) * 256]
                va = vs[:, h * 16:h * 16 + 8]; vb = vs[:, h * 16 + 8:h * 16 + 16]
                S.op("dve", lambda e, o=va, i=ch: e.max(out=o, in_=i), [b_cand], [b_vs])
                S.op("dve", lambda e, o=icu[:, h * 16:h * 16 + 8], m=va, i=ch: e.max_index(out=o, in_max=m, in_values=i), [b_cand, b_vs], [b_icu])
                S.op("dve", lambda e, o=ch, m=va, i=ch: e.match_replace(out=o, in_to_replace=m, in_values=i, imm_value=-1e30), [b_vs], [b_cand])
                S.op("dve", lambda e, o=vb, i=ch: e.max(out=o, in_=i), [b_cand], [b_vs])
                S.op("dve", lambda e, o=icu[:, h * 16 + 8:h * 16 + 16], m=vb, i=ch: e.max_index(out=o, in_max=m, in_values=i), [b_cand, b_vs], [b_icu])
            negm = misc[:, 0:8]; sume = misc[:, 8:16]; rsum = misc[:, 16:24]
            ts(negm, bass.AP(vs.tensor, vs.offset, [list(vs.ap[0]), [16, 8]]), -1.0, None, ALU.mult, None, [b_vs], [b_misc])
            for h in range(8):
                act(ev[:, h * 16:(h + 1) * 16], vs[:, h * 16:(h + 1) * 16], AF.Exp, [b_vs, b_misc], [b_r],
                    bias=negm[:, h:h + 1])
            S.op("dve", lambda e, o=sume, i=ap3(ev, 0, [16, 8], [1, 16]): e.tensor_reduce(out=o, in_=i, axis=AX.X, op=ALU.add), [b_r], [b_misc])
            recip(rsum, sume, [b_r, b_misc], [b_misc])
            tt(ap3(gates, 0, [16, 8], [1, 16]), ap3(ev, 0, [16, 8], [1, 16]), ap3(rsum, 0, [1, 8], [0, 16]), ALU.mult, [b_r, b_misc], [b_r])
            icf = af
            cp(icf, icu[:, :], [b_icu], [b_r])
            cp(i12f, i12u[:, :], [b_i12u], [b_i12f])
            E4 = ap4(EQ, 0, [256, 8], [16, 16], [1, 16])
            E4b = ap4(EQ2, 0, [256, 8], [16, 16], [1, 16])
            icb = ap4(icf, 0, [16, 8], [1, 16], [0, 16])
            tt(E4, icb, ap4(iota16[:, :], 16, [0, 8], [0, 16], [1, 16]), ALU.is_ge, [b_r, b_iota], [b_eq])
            tt(E4b, icb, ap4(iota16[:, :], 32, [0, 8], [0, 16], [1, 16]), ALU.is_lt, [b_r, b_iota], [b_eq])
            tt(E4, E4, E4b, ALU.mult, [b_eq], [b_eq])
            tt(E4b, E4, ap4(iota16[:, :], 0, [0, 8], [0, 16], [1, 16]), ALU.mult, [b_eq, b_iota], [b_eq])
            S.op("dve", lambda e, o=bf, i=ap3(EQ2, 0, [16, 128], [1, 16]): e.tensor_reduce(out=o, in_=i, axis=AX.X, op=ALU.add), [b_eq], [b_r])
            tt(E4b, E4, ap4(i12f, 0, [32, 8], [0, 16], [1, 16]), ALU.mult, [b_eq, b_i12f], [b_eq])
            S.op("dve", lambda e, o=iaf, i=ap3(EQ2, 0, [16, 128], [1, 16]): e.tensor_reduce(out=o, in_=i, axis=AX.X, op=ALU.add), [b_eq], [b_r])
            stt(bf, bf, -16.0, icf, ALU.mult, ALU.add, [b_r], [b_r])
            tt(E4, ap4(bf, 0, [16, 8], [1, 16], [0, 16]), ap4(iota16[:, :], 0, [0, 8], [0, 16], [1, 16]), ALU.is_equal, [b_r, b_iota], [b_eq])
            tt(E4, E4, ap4(i12f, 16, [32, 8], [0, 16], [1, 16]), ALU.mult, [b_eq, b_i12f], [b_eq])
            S.op("dve", lambda e, o=ibf, i=ap3(EQ, 0, [16, 128], [1, 16]): e.tensor_reduce(out=o, in_=i, axis=AX.X, op=ALU.add), [b_eq], [b_r])
            stt(idsf, iaf, 128.0, ibf, ALU.mult, ALU.add, [b_r], [b_r])
            ts(idsf, idsf, 0.0, float(NEXP - 1), ALU.max, ALU.min, [b_r], [b_r])
            cp(idsu[:, :], idsf, [b_r], [b_ids])
            if dbg == "p5r":
                dma(dbgR[tile_i], WF[:, 0:1152], [b_r], [], "stdbg")
                continue
            for k in range(128):
                s_ = k % 4
                S.dma("pool", lambda e, o=G[:, s_, :], tb_=peer_u[l], ix=idsu[:, k:k + 1]: e.indirect_dma_start(
                    out=o, out_offset=None, in_=tb_, in_offset=bass.IndirectOffsetOnAxis(ap=ix, axis=0)),
                    [b_ids], [b_G[s_]], "g%d" % s_)
                S.op("dve", lambda e, o=G[:, s_, :], hh=h2t, ao=actv[:, k:k + 1]: e.scalar_tensor_tensor(
                    out=o, in0=o, scalar=1.0, in1=hh, op0=ALU.mult, op1=ALU.mult, accum_out=ao),
                    [b_h2, b_G[s_]], [b_G[s_], b_r])
            act(wv, actv, AF.Gelu_apprx_tanh, [b_r], [b_r])
            tt(wv, wv, gates, ALU.mult, [b_r], [b_r])
            for k in range(128):
                s_ = k % 4
                S.dma("pool", lambda e, o=G[:, s_, :], tb_=peer_v[l], ix=idsu[:, k:k + 1]: e.indirect_dma_start(
                    out=o, out_offset=None, in_=tb_, in_offset=bass.IndirectOffsetOnAxis(ap=ix, axis=0)),
                    [b_ids], [b_G[s_]], "g%d" % s_)
                if k == 0:
                    ts(acc, G[:, s_, :], wv[:, 0:1], None, ALU.mult, None, [b_G[s_], b_r], [b_acc])
                else:
                    stt(acc, G[:, s_, :], wv[:, k:k + 1], acc, ALU.mult, ALU.add, [b_G[s_], b_r, b_acc], [b_acc])
            if dbg:
                dma(dbgR[tile_i], WF[:, 0:1152], [b_r], [], "stdbg")
            dma(x1t, X1L[tile_i * D:(tile_i + 1) * D, :].rearrange("(c p) t -> p c t", p=128), [], [b_x1], "ldC")
            for c in range(32):
                if c % 4 == 0:
                    pt, bpt = nps()
                tr(pt[:, (c % 4) * 128:(c % 4) * 128 + 128], acc[:, c * 128:(c + 1) * 128], ident[:, :], [b_acc, b_ident], [bpt])
                if c % 4 == 3:
                    for c2 in range(c - 3, c + 1):
                        stt(x1t[:, c2, :], pt[:, (c2 % 4) * 128:(c2 % 4) * 128 + 128], modT[:, 160 + c2:161 + c2], x1t[:, c2, :],
                            ALU.mult, ALU.add, [bpt, b_mod, b_x1], [b_x1])
            dma(CCin[tile_i * D:(tile_i + 1) * D, :].rearrange("(c p) t -> p c t", p=128), x1t, [b_x1], [], "stx")
        S.barrier()
        b_cci = S.buf(); b_cco = [S.buf(), S.buf()]
        HR = D // 2
        for t_ in range(NBL):
            for hf in range(2):
                i_ = (t_ * 2 + hf) % 2
                src = CCin[t_ * D + hf * HR:t_ * D + (hf + 1) * HR, :]
                S.cc(lambda e, gr=groups, a=src, o=CCout[i_]: e.collective_compute("AllGather", ALU.bypass, replica_groups=gr, ins=[a], outs=[o]),
                     [b_cci], [b_cco[i_]])
                for r_ in range(GC):
                    r0 = (r_ * NBL + t_) * D + hf * HR
                    dma(XTM[r0:r0 + HR, :], CCout[i_][r_ * HR:(r_ + 1) * HR, :], [b_cco[i_]], [], "stcc")
        S.barrier()
    S.barrier()
    dma(outT, XTM, [], [S.buf()], "outcp")
    S.emit()
    return nc


def host_inputs(inp, T, depth, b, dbg=False, j=0, GC=4):
    NGL = NG // GC; NHL = NH // GC; NKL = NKV // GC
    gs = slice(j * NGL, (j + 1) * NGL); hsl = slice(j * NHL, (j + 1) * NHL)
    f = np.float32
    ca = np.ascontiguousarray
    d = {}
    NB = T // 128; NBL = NB // GC
    d["xtm_in"] = ca(inp["x"][b, :T].reshape(NB, 128, D).transpose(0, 2, 1).reshape(NB * D, 128))
    idx = host_inputs_idx(T, j, GC)
    d["cT"] = ca(inp["c"][b].reshape(32, 128).T)
    d["w_ada"] = inp["w_ada"]
    d["b_adaT"] = ca(inp["b_ada"].reshape(192, 128).T)
    d["adaT"] = ca(inp["ada_layer"][:depth].reshape(depth, 192, 128).transpose(2, 0, 1))
    d["g1T"] = ca(inp["norm1_g"][:depth].reshape(depth, 32, 128).transpose(2, 0, 1))
    d["g2T"] = ca(inp["norm2_g"][:depth].reshape(depth, 32, 128).transpose(2, 0, 1))
    wi = inp["w_in"][:depth]
    d["w_in"] = ca(np.concatenate([wi[:, :, j * NGL * 16:(j + 1) * NGL * 16], wi[:, :, 2048 + j * NHL * 64:2048 + (j + 1) * NHL * 64],
                                   wi[:, :, 4096 + j * NKL * 64:4096 + (j + 1) * NKL * 64], wi[:, :, 4352 + j * NKL * 64:4352 + (j + 1) * NKL * 64]], axis=2))
    d["w_glu"] = inp["w_glu"][:depth]
    d["w_out"] = inp["w_out"][:depth]
    d["w_q"] = inp["peer_wq"][:depth]
    lre = inp["lam_re"][:depth, gs]; lim = inp["lam_im"][:depth, gs]; ldt = inp["log_dt"][:depth, gs]
    lreT = lre.transpose(2, 0, 1); limT = lim.transpose(2, 0, 1)
    d["lamS_re"] = ca(np.concatenate([lreT, lreT], 0))
    d["lamS_im"] = ca(np.concatenate([limT, limT], 0))
    d["ldtS"] = ca(np.broadcast_to(ldt[None], (128, depth, NGL)))
    d["lamR_re"] = ca(np.broadcast_to(lre[None], (16, depth, NGL, 64)))
    d["lamR_im"] = ca(np.broadcast_to(lim[None], (16, depth, NGL, 64)))
    d["ldtR"] = ca(np.broadcast_to(ldt[None, :, :, None], (16, depth, NGL, 64)))
    d["bT_re"] = ca(inp["b_re"][:depth, gs].transpose(3, 0, 1, 2))
    d["bT_im"] = ca(inp["b_im"][:depth, gs].transpose(3, 0, 1, 2))
    cre = inp["c_re"][:depth, gs].transpose(3, 0, 1, 2); cim = inp["c_im"][:depth, gs].transpose(3, 0, 1, 2)
    d["cW1"] = ca(np.concatenate([cre, cim], 0))
    d["cW2"] = ca(np.concatenate([cim, cre], 0))
    d["dT"] = ca(inp["d_skip"][:depth, gs].transpose(2, 0, 1))
    d["qgT"] = ca(inp["q_gain"][:depth][:, :, None])
    d["kgT"] = ca(inp["k_gain"][:depth][:, :, None])
    d["sinksB"] = ca(np.broadcast_to(inp["sinks"][:depth, hsl][None], (128, depth, NHL)))
    slopes = np.exp2(-8.0 * np.arange(1, NH + 1, dtype=np.float64) / NH)
    kk = np.arange(128)[:, None]; qq = np.arange(128)[None, :]
    dist_prev = qq + 128 - kk
    dist_cur = qq - kk
    bt = np.full((128, NH, 256), NEG, np.float64)
    for h in range(NH):
        bt[:, h, 0:128] = np.where(dist_prev < 128, -slopes[h] * dist_prev, NEG)
        bt[:, h, 128:256] = np.where(dist_cur >= 0, -slopes[h] * dist_cur, NEG)
    d["biasT"] = ca(bt[:, hsl].astype(f))
    d["gnsT"] = ca(inp["gn_ssm"][:depth].reshape(depth, 16, 128).transpose(2, 0, 1))
    d["gnaT"] = ca(inp["gn_attn"][:depth].reshape(depth, 16, 128).transpose(2, 0, 1))
    d["k12"] = ca(np.concatenate([inp["peer_k1"][:depth].transpose(2, 0, 1), inp["peer_k2"][:depth].transpose(2, 0, 1)], 0))
    nexp = 16384 if dbg in (False, "full") else 128
    for i in range(depth):
        d["peer_u%d" % i] = inp["peer_u"][i, :nexp]
        d["peer_v%d" % i] = inp["peer_v"][i, :nexp]
    d["ident"] = np.eye(128, dtype=f)
    io = np.arange(16, dtype=f)
    d["iota16"] = ca(np.broadcast_to(np.concatenate([io, 16 * io, 16 * io + 16])[None], (128, 48)))
    out = {k: np.asarray(v, dtype=f) for k, v in d.items()}
    out["idx"] = idx
    return out


def run(inp, T, depth, dbg=False, GC=4):
    nc = build_program(T, depth, dbg, GC)
    in_maps = []
    for b in range(2):
        for j in range(GC):
            m = host_inputs(inp, T, depth, b, dbg, j, GC)
            m["idx"] = host_inputs_idx(T, j, GC)
            in_maps.append(m)
    res = run_bass_kernel_spmd(nc, in_maps, core_ids=list(range(2 * GC)))
    return res.results


def host_inputs_idx(T, j, GC):
    NB = T // 128; NBL = NB // GC
    p = np.arange(128)
    idx = np.zeros((128, NBL * 48), np.uint32)
    for t in range(NBL):
        Tg = j * NBL + t
        for c in range(32):
            idx[:, t * 48 + c] = Tg * D + c * 128 + p
        for c in range(16):
            idx[:, t * 48 + 32 + c] = Tg * 2048 + c * 128 + p
    return idx


def untile(o, T):
    NB = T // 128
    return np.ascontiguousarray(o.reshape(NB, D, 128).transpose(0, 2, 1).reshape(T, D))


GCORES = 4


def kernel(**inputs):
    inp = {k: np.asarray(v) for k, v in inputs.items()}
    T = inp["x"].shape[1]
    res = run(inp, T, DEPTH, False, GCORES)
    out = np.stack([untile(res[b * GCORES]["outT"], T) for b in range(2)], 0)
    return out.astype(np.float32)
```

```python
import numpy as np
import concourse.bass as bass
import concourse.mybir as mybir
from concourse.bass_utils import run_bass_kernel_spmd

F32 = mybir.dt.float32
U32 = mybir.dt.uint32
AF = mybir.ActivationFunctionType
ALU = mybir.AluOpType
AX = mybir.AxisListType

D = 4096
DEPTH = 4
NG = 128
NH = 32
NKV = 4
INW = 4608
EPS = 1e-6
NEG = -30000.0


class Buf:
    __slots__ = ("w", "r", "name")

    def __init__(self, name=""):
        self.w = {}
        self.r = {}
        self.name = name


class Sched:
    ENG = ("pe", "act", "dve", "pool", "sp")

    def __init__(self, nc):
        self.nc = nc
        self.q = {e: [] for e in self.ENG}
        self.sems = {}
        self.latest = {}
        self.seen = {e: {} for e in self.ENG}
        for e in ("pe", "act", "dve", "pool"):
            self.sems[e] = nc.alloc_semaphore(name="done_" + e)
            self.latest[e] = 0
        self.bufs = []

    def buf(self, name=""):
        b = Buf(name)
        self.bufs.append(b)
        return b

    def _deps(self, reads, writes):
        deps = {}
        for b in reads:
            for k, v in b.w.items():
                if deps.get(k, 0) < v:
                    deps[k] = v
        for b in writes:
            for d in (b.w, b.r):
                for k, v in d.items():
                    if deps.get(k, 0) < v:
                        deps[k] = v
        return deps

    def _wait(self, eng, deps):
        seen = self.seen[eng]
        for k, v in deps.items():
            if k == "pe" and eng == "pe":
                continue
            if seen.get(k, 0) >= v:
                continue
            seen[k] = v
            self.q[eng].append(("wait", k, v))

    def _mark(self, tok, reads, writes):
        k, v = tok
        for b in reads:
            b.r[k] = v
        for b in writes:
            b.w = {k: v}
            b.r = {}

    def op(self, eng, fn, reads=(), writes=()):
        self._wait(eng, self._deps(reads, writes))
        self.latest[eng] += 1
        self.q[eng].append(("op", fn, eng, 1))
        self._mark((eng, self.latest[eng]), reads, writes)

    def dma(self, queue, fn, reads, writes, key):
        self._wait(queue, self._deps(reads, writes))
        if key not in self.sems:
            self.sems[key] = self.nc.alloc_semaphore(name="d_" + key)
            self.latest[key] = 0
        self.latest[key] += 16
        self.q[queue].append(("op", fn, key, 16))
        self._mark((key, self.latest[key]), reads, writes)

    def cc(self, fn, reads, writes):
        key = "cc"
        self._wait("pool", self._deps(reads, writes))
        if key not in self.sems:
            self.sems[key] = self.nc.alloc_semaphore(name="d_cc")
            self.latest[key] = 0
        self.latest[key] += 1
        self.q["pool"].append(("op", fn, key, 1))
        self._mark((key, self.latest[key]), reads, writes)

    def barrier(self):
        for e in self.ENG:
            self._wait(e, dict(self.latest))
        for b in self.bufs:
            b.w = {}
            b.r = {}

    def emit(self):
        nc = self.nc
        self.barrier()
        sems = self.sems

        def replay(name, e):
            for it in self.q[name]:
                if it[0] == "wait":
                    e.wait_ge(sems[it[1]], it[2])
                else:
                    it[1](e).then_inc(sems[it[2]], it[3])

        with nc.Block() as block:
            @block.tensor
            def _(e):
                replay("pe", e)

            @block.scalar
            def _(e):
                replay("act", e)

            @block.vector
            def _(e):
                replay("dve", e)

            @block.gpsimd
            def _(e):
                replay("pool", e)

            @block.sync
            def _(e):
                replay("sp", e)


def sb_ap(t, off, dims, np_=128, pstart=0):
    fs = 1
    for s in t.shape[1:]:
        fs *= s
    return bass.AP(t, pstart * fs + off, [[fs, np_]] + [list(d) for d in dims])


def build_program(T, depth, dbg=False, GC=4):
    nc = bass.Bass("TRN2", target_bir_lowering=False)
    S = Sched(nc)
    NTB = T // 512
    NB = T // 128
    NBL = NB // GC
    NIDX = 48
    NGL = NG // GC; NHL = NH // GC; NKL = NKV // GC
    CW = NGL * 16 + NHL * 64 + 2 * NKL * 64
    QOFF = NGL * 16; KOFF = QOFF + NHL * 64; VOFF = KOFF + NKL * 64

    def dint(name, shape, dt=F32):
        return nc.dram_tensor(name, list(shape), dt, kind="Internal").ap()

    def din(name, shape, dt=F32):
        return nc.dram_tensor(name, list(shape), dt, kind="ExternalInput").ap()

    def dscr(name, shape, dt=F32):
        return nc.dram_tensor(name, list(shape), dt, kind=("ExternalOutput" if dbg else "Internal")).ap()

    xtm_in = din("xtm_in", [NB * D, 128])
    idx_in = din("idx", [128, NBL * NIDX], U32)
    xmine_in = din("xmine", [NBL * D, 128])
    cT_in = din("cT", [128, 32])
    w_ada = din("w_ada", [D, 6 * D])
    badaT_in = din("b_adaT", [128, 192])
    adaT_in = din("adaT", [128, depth, 192])
    g1T_in = din("g1T", [128, depth, 32])
    g2T_in = din("g2T", [128, depth, 32])
    w_in = din("w_in", [depth, D, CW])
    w_glu = din("w_glu", [depth, 2048, 2048])
    w_out = din("w_out", [depth, D, D])
    w_q = din("w_q", [depth, D, 1024])
    lamS_re_in = din("lamS_re", [128, depth, NGL])
    lamS_im_in = din("lamS_im", [128, depth, NGL])
    ldtS_in = din("ldtS", [128, depth, NGL])
    lamR_re_in = din("lamR_re", [16, depth, NGL, 64])
    lamR_im_in = din("lamR_im", [16, depth, NGL, 64])
    ldtR_in = din("ldtR", [16, depth, NGL, 64])
    bT_re_in = din("bT_re", [16, depth, NGL, 64])
    bT_im_in = din("bT_im", [16, depth, NGL, 64])
    cW1_in = din("cW1", [128, depth, NGL, 16])
    cW2_in = din("cW2", [128, depth, NGL, 16])
    dT_in = din("dT", [16, depth, NGL])
    qgT_in = din("qgT", [depth, 64, 1])
    kgT_in = din("kgT", [depth, 64, 1])
    sinksB_in = din("sinksB", [128, depth, NHL])
    biasT_in = din("biasT", [128, NHL, 256])
    gnsT_in = din("gnsT", [128, depth, 16])
    gnaT_in = din("gnaT", [128, depth, 16])
    k12_in = din("k12", [128, depth, 128])
    NEXP = 16384 if dbg in (False, "full") else 128
    peer_u = [din("peer_u%d" % i, [NEXP, D]) for i in range(depth)]
    peer_v = [din("peer_v%d" % i, [NEXP, D]) for i in range(depth)]
    ident_in = din("ident", [128, 128])
    iota16_in = din("iota16", [128, 48])

    outT = nc.dram_tensor("outT", [NB * D, 128], F32, kind="ExternalOutput").ap()

    XTM = dint("XTM", [NB * D, 128])
    CCin = dint("CCin", [NBL * D, 128])
    CRg = max(1, (1 << 20) // (T * 4))
    CCg = [dint("CCg0", [GC * CRg, T]), dint("CCg1", [GC * CRg, T])]
    CCout = [dint("CCout0", [GC * (D // 2), 128]), dint("CCout1", [GC * (D // 2), 128])]
    zT = dscr("zT", [CW, T])
    gTl = dint("gTl", [NGL * 16, T])
    aTl = dint("aTl", [NHL * 64, T])
    GTM = dint("GTM", [NB * 2048, 128])
    ATM = dint("ATM", [NB * 2048, 128])
    X1L = dint("X1L", [NBL * D, 128])
    QL = dint("QL", [NBL * 1024, 128])
    h2tm = dscr("h2tm", [NBL * 128, D])
    dbgR = dscr("dbgR", [T // 128, 128, 1152]) if dbg else None

    def sb(name, shape, dt=F32):
        return nc.alloc_sbuf_tensor("sb_" + name, list(shape), dt)

    ident = sb("ident", [128, 128]); b_ident = S.buf()
    ones = sb("ones", [128, 128]); b_ones = S.buf()
    iota16 = sb("iota16", [128, 48]); b_iota = S.buf()
    condT = sb("condT", [128, 192]); b_cond = S.buf()
    modT = sb("modT", [128, 192]); b_mod = S.buf()
    AB = sb("AB", [128, 4, 32]); b_AB = S.buf()
    gpar = sb("gpar", [128, 2, 32]); b_gpar = S.buf()
    small = sb("small", [128, 64]); b_small = S.buf()
    BA = sb("BA", [128, 16384]); b_BA = S.buf("BA")
    BBC = sb("BBC", [128, 16384])

    class _View:
        def __init__(self, t, off):
            self.t = t; self.off = off

        def __getitem__(self, idx):
            p, c = idx
            return self.t[p, slice(c.start + self.off, c.stop + self.off)]
    BB = _View(BBC, 0); b_BB = S.buf("BB")
    BC = _View(BBC, 8192); b_BC = S.buf("BC")
    RSTD = sb("RSTD", [128, 512]); b_RSTD = S.buf("RSTD")
    k12 = sb("k12", [64, 256]); b_k12 = S.buf("k12")
    dtmp = sb("dtmp", [128, 8]); b_dtmp = S.buf("dtmp")
    IDX = sb("IDX", [128, NBL * NIDX], U32); b_IDX = S.buf("IDX")
    WT = [sb("WT0", [128, 32, 128]), sb("WT1", [128, 32, 128])]
    b_WT = [S.buf("WT0"), S.buf("WT1")]
    TM = [sb("TM%d" % i, [128, 512]) for i in range(8)]
    b_TM = [S.buf("TM%d" % i) for i in range(8)]
    PS = [nc.alloc_psum_tensor("ps%d" % i, [128, 512], F32) for i in range(8)]
    b_PS = [S.buf("ps%d" % i) for i in range(8)]
    st = {"ps": 0, "tm": 0, "wt": 0}

    def nps():
        i = st["ps"]; st["ps"] = (i + 1) % 8
        return PS[i], b_PS[i]

    def ntm():
        i = st["tm"]; st["tm"] = (i + 1) % 8
        return TM[i], b_TM[i]

    def dma(out, in_, reads, writes, key, queue="sp"):
        S.dma(queue, lambda e, o=out, i=in_: e.dma_start(out=o, in_=i), reads, writes, key)

    def act(out, in_, func, reads, writes, bias=None, scale=None, accum=None):
        kw = {}
        if bias is not None:
            kw["bias"] = bias
        if scale is not None:
            kw["scale"] = scale
        if accum is not None:
            kw["accum_out"] = accum
        S.op("act", lambda e, o=out, i=in_, f=func, kw=kw: e.activation(out=o, in_=i, func=f, **kw), reads, writes)

    def tt(out, a, b, op, reads, writes, eng="dve"):
        S.op(eng, lambda e, o=out, a=a, b=b, op=op: e.tensor_tensor(out=o, in0=a, in1=b, op=op), reads, writes)

    def ts(out, a, s1, s2, op0, op1, reads, writes, eng="dve"):
        if op1 is None:
            S.op(eng, lambda e, o=out, a=a, s1=s1, op0=op0: e.tensor_scalar(out=o, in0=a, scalar1=s1, scalar2=None, op0=op0), reads, writes)
        else:
            S.op(eng, lambda e, o=out, a=a, s1=s1, s2=s2, op0=op0, op1=op1: e.tensor_scalar(out=o, in0=a, scalar1=s1, scalar2=s2, op0=op0, op1=op1), reads, writes)

    def stt(out, a, s, b, op0, op1, reads, writes, eng="dve"):
        S.op(eng, lambda e, o=out, a=a, s=s, b=b, op0=op0, op1=op1: e.scalar_tensor_tensor(out=o, in0=a, scalar=s, in1=b, op0=op0, op1=op1), reads, writes)

    def cp(out, in_, reads, writes, eng="dve"):
        S.op(eng, lambda e, o=out, i=in_: e.tensor_copy(out=o, in_=i), reads, writes)

    def recip(out, in_, reads, writes):
        S.op("dve", lambda e, o=out, i=in_: e.reciprocal(out=o, in_=i), reads, writes)

    def mm(out, lhsT, rhs, start, stop, reads, writes):
        S.op("pe", lambda e, o=out, l=lhsT, r=rhs, s0=start, s1=stop: e.matmul(o, lhsT=l, rhs=r, start=s0, stop=s1), reads, writes)

    def tr(out, in_, idn, reads, writes):
        S.op("pe", lambda e, o=out, i=in_, d=idn: e.transpose(o, i, d), reads, writes)

    def memset(ap, val, writes, eng="dve"):
        S.op(eng, lambda e, a=ap, v=val: e.memset(a, v), [], writes)

    def rms_rstd(blk, b_blk, c0, nchunk, N, nfeat, rstd_ap, b_rstd):
        ps, bp = nps()
        for i in range(nchunk):
            sq, bsq = ntm()
            act(sq[:, 0:N], blk[:, c0 + i, 0:N], AF.Square, [b_blk], [bsq])
            mm(ps[:, 0:N], ones[:, :], sq[:, 0:N], i == 0, i == nchunk - 1, [b_ones, bsq], [bp])
        t1, bt1 = ntm()
        act(t1[:, 0:N], ps[:, 0:N], AF.Sqrt, [bp], [bt1], bias=small[:, 0:1], scale=1.0 / nfeat)
        recip(rstd_ap, t1[:, 0:N], [bt1, b_small], [b_rstd])

    def gemm(Wd, KC, ntiles, rhs_fn, rhs_bufs, N, evac):
        def load(j):
            s = st["wt"]; st["wt"] ^= 1
            dma(WT[s][:, 0:KC, :], Wd[:, j * 128:(j + 1) * 128].rearrange("(c p) n -> p c n", p=128),
                [], [b_WT[s]], "wt%d" % s)
            return s
        nxt = load(ntiles[0])
        for idx, j in enumerate(ntiles):
            s = nxt
            if idx + 1 < len(ntiles):
                nxt = load(ntiles[idx + 1])
            ps, bp = nps()
            for c in range(KC):
                mm(ps[:, 0:N], WT[s][:, c, :], rhs_fn(c), c == 0, c == KC - 1, [b_WT[s]] + rhs_bufs, [bp])
            evac(j, ps, bp)

    def xtile(T_):
        return XTM[T_ * D:(T_ + 1) * D, :].rearrange("(c p) t -> p c t", p=128)

    def xload(blk, tb, bufs, key):
        for q_ in range(4):
            dma(blk[:, :, q_ * 128:(q_ + 1) * 128], xtile(4 * tb + q_), [], bufs, key)

    def xstore(blk, tb, bufs, key):
        for q_ in range(4):
            dma(xtile(4 * tb + q_), blk[:, :, q_ * 128:(q_ + 1) * 128], bufs, [], key)

    def igather(out_, tab_, col_, bufs, key):
        S.dma("pool", lambda e, o=out_, tb_=tab_, ix=IDX[:, col_:col_ + 1]: e.indirect_dma_start(
            out=o, out_offset=None, in_=tb_, in_offset=bass.IndirectOffsetOnAxis(ap=ix, axis=0)),
            [b_IDX], bufs, key)

    dma(ident[:, :], ident_in, [], [b_ident], "c0")
    dma(iota16[:, :], iota16_in, [], [b_iota], "c1")
    memset(ones[:, :], 1.0, [b_ones])
    memset(small[:, 0:1], EPS, [b_small])
    memset(small[:, 1:2], float(np.pi / 2), [b_small])
    dma(XTM, xtm_in, [], [S.buf()], "xcp")
    dma(IDX[:, :], idx_in, [], [b_IDX], "c1")
    dma(CCin, xmine_in, [], [S.buf()], "xcp")
    S.barrier()

    cT = BB[:, 0:32]
    dma(cT, cT_in, [], [b_BB], "ld0")
    scT = BB[:, 32:64]
    act(scT, cT, AF.Silu, [b_BB], [b_BB])
    psc, bpc = nps()

    def cond_evac(j, ps, bp):
        cp(condT[:, j:j + 1], ps[:, 0:1], [bp], [b_cond])
    gemm(w_ada, 32, list(range(192)), lambda c: BB[:, 32 + c:33 + c], [b_BB], 1, cond_evac)
    dma(BC[:, 0:192], badaT_in, [], [b_BC], "ld0")
    tt(condT[:, :], condT[:, :], BC[:, 0:192], ALU.add, [b_cond, b_BC], [b_cond])
    S.barrier()

    for l in range(depth):
        last = l == depth - 1
        dma(BC[:, 0:192], adaT_in[:, l, :], [], [b_BC], "ld0")
        dma(gpar[:, 0, :], g1T_in[:, l, :], [], [b_gpar], "ld1")
        dma(gpar[:, 1, :], g2T_in[:, l, :], [], [b_gpar], "ld1")
        tt(modT[:, :], condT[:, :], BC[:, 0:192], ALU.add, [b_cond, b_BC], [b_mod])
        stt(AB[:, 0, :], modT[:, 32:64], 1.0, gpar[:, 0, :], ALU.add, ALU.mult, [b_mod, b_gpar], [b_AB])
        cp(AB[:, 1, :], modT[:, 0:32], [b_mod], [b_AB])
        stt(AB[:, 2, :], modT[:, 128:160], 1.0, gpar[:, 1, :], ALU.add, ALU.mult, [b_mod, b_gpar], [b_AB])
        cp(AB[:, 3, :], modT[:, 96:128], [b_mod], [b_AB])
        S.barrier()

        xblk = BA[:, :].rearrange("p (c t) -> p c t", c=32)
        rstd = BB[:, 0:512]
        for tb in range(NTB):
            t0 = tb * 512
            xload(xblk, tb, [b_BA], "ldA")
            rms_rstd(xblk, b_BA, 0, 32, 512, D, rstd, b_BB)
            for c in range(32):
                tt(xblk[:, c, :], xblk[:, c, :], rstd, ALU.mult, [b_BA, b_BB], [b_BA])
                act(xblk[:, c, :], xblk[:, c, :], AF.Identity, [b_BA, b_AB], [b_BA],
                    bias=AB[:, 1, c:c + 1], scale=AB[:, 0, c:c + 1])

            def ev1(j, ps, bp, t0=t0):
                o, bo = ntm()
                act(o[:, :], ps[:, :], AF.Copy, [bp], [bo])
                dma(zT[j * 128:(j + 1) * 128, t0:t0 + 512], o[:, :], [bo], [], "stz")
            gemm(w_in[l], 32, list(range(CW // 128)), lambda c: xblk[:, c, :], [b_BA], 512, ev1)
        S.barrier()
        if dbg == "p1":
            break

        LG = T.bit_length() - 1
        CT = BA[:, 0:T]; ST_ = BA[:, 4096:4096 + T]; XT = BA[:, 8192:8192 + T]; SS = BA[:, 12288:12288 + T]
        b_TB = S.buf("tables"); b_XT = S.buf("xt"); b_SS = S.buf("ss"); b_U = S.buf("U"); b_YB = S.buf("YB")
        b_PR = S.buf("params"); b_W12 = S.buf("W12"); b_BBc = S.buf("BBc"); b_DD = S.buf("DD")
        U = BB[0:16, 0:T]; YB = BB[0:16, 4096:4096 + T]

        def P(k):
            return BC[:, k * 128:(k + 1) * 128]
        lre, lim, ldt, rr, th, cc, sn, t1_, t2_ = [P(k) for k in range(9)]
        PWc = BC[:, 2048:3584].rearrange("p (k g) -> p k g", k=12)
        PWs = BC[:, 3584:5120].rearrange("p (k g) -> p k g", k=12)
        W1 = WT[0][:, 0:16, :].rearrange("p a (b h) -> p (a b) h", h=16)
        W2 = WT[0][:, 16:32, :].rearrange("p a (b h) -> p (a b) h", h=16)
        BB1 = WT[1][0:16, 0:16, :]
        BB2 = WT[1][0:16, 16:32, :]
        Dd = BC[0:16, 5120:7168].rearrange("p (g h) -> p g h", h=16)
        dTs = BC[0:16, 7168:7296]
        dma(BC[:, 0:NGL], lamS_re_in[:, l, :], [], [b_PR], "ld0")
        dma(BC[:, 128:128 + NGL], lamS_im_in[:, l, :], [], [b_PR], "ld0")
        dma(BC[:, 256:256 + NGL], ldtS_in[:, l, :], [], [b_PR], "ld0")
        dma(W1[:, 0:NGL, :], cW1_in[:, l, :, :], [], [b_W12], "ld1")
        dma(W2[:, 0:NGL, :], cW2_in[:, l, :, :], [], [b_W12], "ld1")
        dma(BC[0:16, 7168:7168 + NGL], dT_in[:, l, :], [], [b_DD], "ld2")
        ts(WT[0][64:128, 0:16, :], WT[0][64:128, 0:16, :], -1.0, None, ALU.mult, None, [b_W12], [b_W12])
        tt(Dd, sb_ap(ident, 0, [[0, 128], [1, 16]], np_=16),
           bass.AP(BBC, 8192 + 7168, [[16384, 16], [1, 128], [0, 16]]), ALU.mult, [b_ident, b_DD], [b_DD])
        pr = [b_PR]
        act(t1_, ldt, AF.Exp, pr, pr)
        tt(t2_, lre, t1_, ALU.mult, pr, pr)
        act(rr, t2_, AF.Exp, pr, pr)
        tt(th, lim, t1_, ALU.mult, pr, pr)
        act(cc, th, AF.Sin, pr, pr, bias=small[:, 1:2], scale=-0.125)
        act(sn, th, AF.Sin, pr, pr, scale=0.125)

        def csq(c_, s_, a_, b_, bufs, eng="dve"):
            tt(a_, c_, c_, ALU.mult, bufs, bufs, eng)
            tt(b_, s_, s_, ALU.mult, bufs, bufs, eng)
            stt(s_, c_, 2.0, s_, ALU.mult, ALU.mult, bufs, bufs)
            tt(c_, a_, b_, ALU.subtract, bufs, bufs, eng)
        for _ in range(3):
            csq(cc, sn, t1_, t2_, pr)
        cp(PWc[:, 0, :], cc, pr, pr)
        ts(PWs[:, 0, :], sn, -1.0, None, ALU.mult, None, pr, pr)
        for k in range(1, LG):
            tt(t1_, PWc[:, k - 1, :], PWc[:, k - 1, :], ALU.mult, pr, pr)
            tt(t2_, PWs[:, k - 1, :], PWs[:, k - 1, :], ALU.mult, pr, pr)
            tt(PWc[:, k, :], t1_, t2_, ALU.subtract, pr, pr)
            stt(PWs[:, k, :], PWc[:, k - 1, :], 2.0, PWs[:, k - 1, :], ALU.mult, ALU.mult, pr, pr)

        for gc in range(NGL // 16):
            g0 = gc * 16
            A = [BA[0:16, k * 1024:(k + 1) * 1024] for k in range(16)]
            ba = [b_TB, b_XT, b_SS]

            def v3(a):
                return a.rearrange("p (g q) -> p g q", q=64)
            dma(v3(A[0]), lamR_re_in[:, l, g0:g0 + 16, :], [], ba, "ld0")
            dma(v3(A[1]), lamR_im_in[:, l, g0:g0 + 16, :], [], ba, "ld0")
            dma(v3(A[2]), ldtR_in[:, l, g0:g0 + 16, :], [], ba, "ld0")
            dma(v3(A[3]), bT_re_in[:, l, g0:g0 + 16, :], [], ba, "ld0")
            dma(v3(A[4]), bT_im_in[:, l, g0:g0 + 16, :], [], ba, "ld0")
            act(A[5], A[2], AF.Exp, ba, ba)
            tt(A[6], A[0], A[5], ALU.mult, ba, ba)
            act(A[6], A[6], AF.Exp, ba, ba)
            tt(A[7], A[1], A[5], ALU.mult, ba, ba)
            act(A[8], A[7], AF.Sin, ba + [b_small], ba, bias=small[0:16, 1:2], scale=-0.125)
            act(A[9], A[7], AF.Sin, ba, ba, scale=0.125)
            for _ in range(3):
                csq(A[8], A[9], A[10], A[11], ba)
            tt(A[8], A[6], A[8], ALU.mult, ba, ba)
            tt(A[9], A[6], A[9], ALU.mult, ba, ba)
            tt(A[10], A[0], A[0], ALU.mult, ba, ba)
            tt(A[11], A[1], A[1], ALU.mult, ba, ba)
            tt(A[10], A[10], A[11], ALU.add, ba, ba)
            recip(A[10], A[10], ba, ba)
            ts(A[8], A[8], -1.0, None, ALU.add, None, ba, ba)
            tt(A[11], A[8], A[0], ALU.mult, ba, ba)
            tt(A[12], A[9], A[1], ALU.mult, ba, ba)
            tt(A[11], A[11], A[12], ALU.add, ba, ba)
            tt(A[11], A[11], A[10], ALU.mult, ba, ba)
            tt(A[12], A[9], A[0], ALU.mult, ba, ba)
            tt(A[13], A[8], A[1], ALU.mult, ba, ba)
            tt(A[12], A[12], A[13], ALU.subtract, ba, ba)
            tt(A[12], A[12], A[10], ALU.mult, ba, ba)
            tt(A[13], A[11], A[3], ALU.mult, ba, ba)
            tt(A[14], A[12], A[4], ALU.mult, ba, ba)
            tt(A[13], A[13], A[14], ALU.subtract, ba, ba)
            tt(A[14], A[11], A[4], ALU.mult, ba, ba)
            tt(A[15], A[12], A[3], ALU.mult, ba, ba)
            tt(A[14], A[14], A[15], ALU.add, ba, ba)
            cp(BB1[:, :, 0:64], v3(A[13]), ba, [b_BBc])
            cp(BB1[:, :, 64:128], v3(A[14]), ba, [b_BBc])
            ts(BB2[:, :, 0:64], v3(A[14]), -1.0, None, ALU.mult, None, ba, [b_BBc])
            cp(BB2[:, :, 64:128], v3(A[13]), ba, [b_BBc])

            for gi in range(16):
                g = g0 + gi
                dma(U, zT[16 * g:16 * g + 16, 0:T], [], [b_U], "ldU")
                memset(CT[:, 0:1], 1.0, [b_TB])
                memset(ST_[:, 0:1], 0.0, [b_TB])
                for k in range(LG):
                    n = 1 << k
                    pc = PWc[:, k, g:g + 1]; ps_ = PWs[:, k, g:g + 1]
                    ta = BA[:, 8192:8192 + n]; tb_ = BA[:, 10240:10240 + n]
                    ts(ta, ST_[:, 0:n], ps_, None, ALU.mult, None, [b_TB, b_PR], [b_XT])
                    stt(CT[:, n:2 * n], CT[:, 0:n], pc, ta, ALU.mult, ALU.subtract, [b_TB, b_PR, b_XT], [b_TB])
                    ts(tb_, CT[:, 0:n], ps_, None, ALU.mult, None, [b_TB, b_PR], [b_XT])
                    stt(ST_[:, n:2 * n], ST_[:, 0:n], pc, tb_, ALU.mult, ALU.add, [b_TB, b_PR, b_XT], [b_TB])
                for i in range(NTB):
                    sl = slice(i * 512, (i + 1) * 512)
                    p1, bp1 = nps(); p2, bp2 = nps()
                    mm(p1[:, :], BB1[:, gi, :], U[:, sl], True, True, [b_BBc, b_U], [bp1])
                    mm(p2[:, :], BB2[:, gi, :], U[:, sl], True, True, [b_BBc, b_U], [bp2])
                    tmp, btmp = ntm()
                    tt(tmp[:, :], CT[:, sl], p1[:, :], ALU.mult, [b_TB, bp1], [btmp])
                    tt(XT[:, sl], ST_[:, sl], p2[:, :], ALU.mult, [b_TB, bp2], [b_XT])
                    tt(XT[:, sl], XT[:, sl], tmp[:, :], ALU.add, [b_XT, btmp], [b_XT])
                rb = bass.AP(BBC, 8192 + 3 * 128 + g, [[16384, 128], [0, T]])
                S.op("dve", lambda e, o=SS, d0=rb, d1=XT: e.tensor_tensor_scan(out=o, data0=d0, data1=d1, initial=0.0, op0=ALU.mult, op1=ALU.add),
                     [b_PR, b_XT], [b_SS])
                for i in range(NTB):
                    sl = slice(i * 512, (i + 1) * 512)
                    q1, bq1 = ntm(); q2, bq2 = ntm()
                    tt(q1[:, :], CT[:, sl], SS[:, sl], ALU.mult, [b_TB, b_SS], [bq1], "pool")
                    tt(q2[:, :], ST_[:, sl], SS[:, sl], ALU.mult, [b_TB, b_SS], [bq2], "pool")
                    py, bpy = nps()
                    mm(py[0:16, :], W1[:, g, :], q1[:, :], True, False, [b_W12, bq1], [bpy])
                    mm(py[0:16, :], W2[:, g, :], q2[:, :], False, False, [b_W12, bq2], [bpy])
                    mm(py[0:16, :], Dd[:, g, :], U[:, sl], False, True, [b_DD, b_U], [bpy])
                    act(YB[:, sl], py[0:16, :], AF.Gelu_apprx_tanh, [bpy], [b_YB])
                dma(gTl[16 * g:16 * g + 16, 0:T], YB, [b_YB], [], "stg")
        S.barrier()
        if dbg == "p2a":
            break

        b_bias = S.buf(); b_es = S.buf(); b_kh = S.buf(); b_va = S.buf(); b_qh = S.buf(); b_raw = S.buf()
        b_vr = S.buf(); b_yh = S.buf(); b_yT = S.buf(); b_g = S.buf()
        biasT = BBC[:, 8192:8192 + NHL * 256].rearrange("p (h k) -> p h k", k=256)
        khT = BB[0:64, 0:T]
        Vaug = BBC[:, 4096:4096 + NB * 65].rearrange("p (n d) -> p n d", d=65)
        qhT = BA[0:64, 0:T]; RAW = BA[0:64, 4096:4096 + T]; VR = BA[0:64, 8192:8192 + T]
        yh = BA[:, 12288:12288 + NB * 64].rearrange("p (n d) -> p n d", d=64)
        yhT = WT[0][0:64, :, :].rearrange("p a b -> p (a b)")
        esink = small[:, 16:16 + NHL]
        dma(biasT, biasT_in, [], [b_bias], "ld0")
        dma(esink, sinksB_in[:, l, :], [], [b_es], "ld1")
        act(esink, esink, AF.Exp, [b_es], [b_es])
        dma(small[0:64, 48:49], qgT_in[l], [], [b_g], "ld2")
        dma(small[0:64, 49:50], kgT_in[l], [], [b_g], "ld2")
        memset(Vaug[:, :, 64:65], 1.0, [b_va])

        def qknorm(dst, b_dst, gcol):
            for i in range(NTB):
                sl = slice(i * 512, (i + 1) * 512)
                sq, bsq = ntm()
                act(sq[0:64, :], RAW[:, sl], AF.Square, [b_raw], [bsq])
                ps, bp = nps()
                mm(ps[0:64, :], ones[0:64, 0:64], sq[0:64, :], True, True, [b_ones, bsq], [bp])
                t1, bt1 = ntm()
                act(t1[0:64, :], ps[0:64, :], AF.Sqrt, [bp, b_small], [bt1], bias=small[0:64, 0:1], scale=1.0 / 64)
                t2, bt2 = ntm()
                recip(t2[0:64, :], t1[0:64, :], [bt1], [bt2])
                stt(dst[:, sl], RAW[:, sl], small[0:64, gcol:gcol + 1], t2[0:64, :], ALU.mult, ALU.mult, [b_raw, b_g, bt2], [b_dst])

        for kv in range(NKL):
            dma(RAW, zT[KOFF + 64 * kv:KOFF + 64 * kv + 64, 0:T], [], [b_raw], "ldA")
            qknorm(khT, b_kh, 49)
            dma(VR, zT[VOFF + 64 * kv:VOFF + 64 * kv + 64, 0:T], [], [b_vr], "ldB")
            for n in range(NB):
                if n % 4 == 0:
                    pv, bpv = nps()
                tr(pv[:, (n % 4) * 64:(n % 4) * 64 + 64], VR[:, n * 128:(n + 1) * 128], ident[0:64, 0:64], [b_vr, b_ident], [bpv])
                if n % 4 == 3:
                    cp(Vaug[:, n - 3:n + 1, 0:64], pv[:, 0:256].rearrange("p (n d) -> p n d", d=64), [bpv], [b_va])
            for hq in range(8):
                h = kv * 8 + hq
                dma(RAW, zT[QOFF + 64 * h:QOFF + 64 * h + 64, 0:T], [], [b_raw], "ldA")
                qknorm(qhT, b_qh, 48)
                for n in range(NB):
                    blk = slice(n * 128, (n + 1) * 128)
                    sc, bsc = nps()
                    if n > 0:
                        mm(sc[:, 0:128], khT[:, (n - 1) * 128:n * 128], qhT[:, blk], True, True, [b_kh, b_qh], [bsc])
                    mm(sc[:, 128:256], khT[:, blk], qhT[:, blk], True, True, [b_kh, b_qh], [bsc])
                    cols = slice(0, 256) if n > 0 else slice(128, 256)
                    s_sb, bs = ntm()
                    stt(s_sb[:, cols], sc[:, cols], 0.125, biasT[:, h, cols], ALU.mult, ALU.add, [bsc, b_bias], [bs])
                    e_sb, be = ntm()
                    act(e_sb[:, cols], s_sb[:, cols], AF.Exp, [bs], [be])
                    po, bpo = nps()
                    if n > 0:
                        mm(po[:, 0:65], e_sb[:, 0:128], Vaug[:, n - 1, :], True, False, [be, b_va], [bpo])
                    mm(po[:, 0:65], e_sb[:, 128:256], Vaug[:, n, :], n == 0, True, [be, b_va], [bpo])
                    tt(dtmp[:, 0:1], po[:, 64:65], esink[:, h:h + 1], ALU.add, [bpo, b_es], [b_dtmp])
                    recip(dtmp[:, 1:2], dtmp[:, 0:1], [b_dtmp], [b_dtmp])
                    ts(yh[:, n, :], po[:, 0:64], dtmp[:, 1:2], None, ALU.mult, None, [bpo, b_dtmp], [b_yh])
                for n in range(NB):
                    if n % 4 == 0:
                        pt, bpt = nps()
                    tr(pt[0:64, (n % 4) * 128:(n % 4) * 128 + 128], yh[:, n, :], ident[:, :], [b_yh, b_ident], [bpt])
                    if n % 4 == 3:
                        act(yhT[:, (n - 3) * 128:(n + 1) * 128], pt[0:64, :], AF.Copy, [bpt], [b_yT])
                dma(aTl[64 * h:64 * h + 64, 0:T], yhT[:, 0:T], [b_yT], [], "sta")
        S.barrier()
        groups = [list(range(g_ * GC, (g_ + 1) * GC)) for g_ in range(2)]
        b_cco = [S.buf(), S.buf()]
        CR = max(1, (1 << 20) // (T * 4))
        cnt_ = 0
        for (srcT, dstT, rows_l) in ((gTl, GTM, NGL * 16), (aTl, ATM, NHL * 64)):
            for r0 in range(0, rows_l, CR):
                i_ = cnt_ % 2; cnt_ += 1
                co = CCg[i_]
                S.cc(lambda e, gr=groups, a=srcT[r0:r0 + CR, :], o=co: e.collective_compute("AllGather", ALU.bypass, replica_groups=gr, ins=[a], outs=[o]),
                     [], [b_cco[i_]])
                for r_ in range(GC):
                    dma(dstT.rearrange("(n r) t -> r n t", r=2048)[r_ * rows_l + r0:r_ * rows_l + r0 + CR, :, :],
                        co[r_ * CR:(r_ + 1) * CR, :].rearrange("r (n t) -> r n t", t=128), [b_cco[i_]], [], "stcg")
        S.barrier()
        if dbg == "p2":
            break

        b_CLO = S.buf(); b_CHI = S.buf(); b_gn = S.buf()
        xblk = BA[:, :].rearrange("p (c t) -> p c t", c=32)
        cat = BBC[:, :].rearrange("p (c t) -> p c t", c=32)
        gns = small[:, 16:32]; gna = small[:, 32:48]
        dma(gns, gnsT_in[:, l, :], [], [b_gn], "ld0")
        dma(gna, gnaT_in[:, l, :], [], [b_gn], "ld0")
        for tb in range(NBL // 4):
            t0 = tb * 512
            for q_ in range(4):
                ibq = (tb * 4 + q_) * NIDX
                tlq = tb * 4 + q_
                dma(xblk[:, :, q_ * 128:(q_ + 1) * 128], CCin[tlq * D:(tlq + 1) * D, :].rearrange("(c p) t -> p c t", p=128), [], [b_BA], "ldA")
                for c in range(16):
                    igather(cat[:, 16 + c, q_ * 128:(q_ + 1) * 128], GTM, ibq + 32 + c, [b_CHI], "gg")

            def ev_glu(j, ps, bp):
                sg, bsg = ntm()
                act(sg[:, :], ps[:, :], AF.Sigmoid, [bp], [bsg])
                tt(cat[:, j, :], cat[:, 16 + j, :], sg[:, :], ALU.mult, [b_CHI, bsg], [b_CLO])
            gemm(w_glu[l], 16, list(range(16)), lambda c: cat[:, 16 + c, :], [b_CHI], 512, ev_glu)
            for q_ in range(4):
                ibq = (tb * 4 + q_) * NIDX
                for c in range(16):
                    igather(cat[:, 16 + c, q_ * 128:(q_ + 1) * 128], ATM, ibq + 32 + c, [b_CHI], "gg")
            rms_rstd(cat, b_CLO, 0, 16, 512, 2048, RSTD[:, :], b_RSTD)
            for c in range(16):
                stt(cat[:, c, :], cat[:, c, :], gns[:, c:c + 1], RSTD[:, :], ALU.mult, ALU.mult, [b_CLO, b_gn, b_RSTD], [b_CLO])
            rms_rstd(cat, b_CHI, 16, 16, 512, 2048, RSTD[:, :], b_RSTD)
            for c in range(16):
                stt(cat[:, 16 + c, :], cat[:, 16 + c, :], gna[:, c:c + 1], RSTD[:, :], ALU.mult, ALU.mult, [b_CHI, b_gn, b_RSTD], [b_CHI])

            def ev_out(j, ps, bp):
                stt(xblk[:, j, :], ps[:, :], modT[:, 64 + j:65 + j], xblk[:, j, :], ALU.mult, ALU.add, [bp, b_mod, b_BA], [b_BA])
            gemm(w_out[l], 32, list(range(32)), lambda c: cat[:, c, :], [b_CLO, b_CHI], 512, ev_out)
            for q_ in range(4):
                tl_ = tb * 4 + q_
                dma(X1L[tl_ * D:(tl_ + 1) * D, :].rearrange("(c p) t -> p c t", p=128), xblk[:, :, q_ * 128:(q_ + 1) * 128], [b_BA], [], "stx")
            rms_rstd(xblk, b_BA, 0, 32, 512, D, RSTD[:, :], b_RSTD)
            bh = [b_CLO, b_CHI]
            for c in range(32):
                tt(cat[:, c, :], xblk[:, c, :], RSTD[:, :], ALU.mult, [b_BA, b_RSTD], bh)
                act(cat[:, c, :], cat[:, c, :], AF.Identity, bh + [b_AB], bh, bias=AB[:, 3, c:c + 1], scale=AB[:, 2, c:c + 1])

            def ev_q(j, ps, bp, t0=t0):
                o, bo = ntm()
                act(o[:, :], ps[:, :], AF.Copy, [bp], [bo])
                for q_ in range(4):
                    T_ = t0 // 128 + q_
                    dma(QL[T_ * 1024 + j * 128:T_ * 1024 + (j + 1) * 128, :], o[:, q_ * 128:(q_ + 1) * 128], [bo], [], "stq")
            gemm(w_q[l], 32, list(range(8)), lambda c: cat[:, c, :], bh, 512, ev_q)
            for t4 in range(4):
                s_ = st["wt"]; st["wt"] ^= 1
                ROW = WT[s_][:, :, :].rearrange("p a b -> p (a b)")
                for c in range(32):
                    if c % 4 == 0:
                        pt, bpt = nps()
                    tr(pt[:, (c % 4) * 128:(c % 4) * 128 + 128], cat[:, c, t4 * 128:(t4 + 1) * 128], ident[:, :], bh + [b_ident], [bpt])
                    if c % 4 == 3:
                        act(ROW[:, (c - 3) * 128:(c + 1) * 128], pt[:, :], AF.Copy, [bpt], [b_WT[s_]])
                dma(h2tm[t0 + t4 * 128:t0 + (t4 + 1) * 128, :], ROW, [b_WT[s_]], [], "sth")
        S.barrier()
        if dbg == "p4":
            break

        b_G = [S.buf() for _ in range(4)]
        G = BA[:, :].rearrange("p (s d) -> p s d", s=4)
        h2t = BBC[:, 0:4096]; b_h2 = S.buf()
        acc = BBC[:, 4096:8192]; b_acc = S.buf()
        x1t = BA[:, 0:4096].rearrange("p (c t) -> p c t", c=32); b_x1 = b_G[0]
        RT0 = 11264
        qtile = BBC[0:64, RT0:RT0 + 2048].rearrange("p (s h t) -> p s h t", s=2, h=8); b_qt = S.buf()
        v12 = BBC[:, RT0 + 2048:RT0 + 2304]; b_v12 = S.buf()
        i12f = BBC[:, RT0 + 2304:RT0 + 2560]; b_i12f = S.buf()
        tmpS = BBC[:, RT0 + 2560:RT0 + 2688]; b_tmpS = S.buf()
        cand = BBC[:, RT0 + 2688:RT0 + 4736]; b_cand = S.buf()
        vs = BBC[:, RT0 + 4736:RT0 + 4864]; b_vs = S.buf()
        misc = BBC[:, RT0 + 4864:RT0 + 4992]; b_misc = S.buf()
        WF = WT[0][:, :, :].rearrange("p a b -> p (a b)")
        ev = WF[:, 0:128]; gates = WF[:, 128:256]; af = WF[:, 256:384]; bf = WF[:, 384:512]
        iaf = WF[:, 512:640]; ibf = WF[:, 640:768]; idsf = WF[:, 768:896]; actv = WF[:, 896:1024]; wv = WF[:, 1024:1152]
        b_r = S.buf()
        WF1 = WT[1][:, :, :].rearrange("p a b -> p (a b)")
        EQ = WF1[:, 0:2048]; EQ2 = WF1[:, 2048:4096]; b_eq = S.buf()
        i12u = WF[:, 1152:1408].bitcast(U32); b_i12u = S.buf()
        icu = WF[:, 1408:1536].bitcast(U32); b_icu = S.buf()
        idsu = WF[:, 1536:1664].bitcast(U32); b_ids = S.buf()
        dma(k12[0:64, 0:128], k12_in[0:64, l, :], [], [b_k12], "ld0")
        dma(k12[0:64, 128:256], k12_in[64:128, l, :], [], [b_k12], "ld0")

        def fr(ap_):
            return ap_.tensor, ap_.offset

        def ap4(ap_, off, d1, d2, d3):
            t_, o_ = fr(ap_)
            return bass.AP(t_, o_ + off, [list(ap_.ap[0]), d1, d2, d3])

        def ap3(ap_, off, d1, d2):
            t_, o_ = fr(ap_)
            return bass.AP(t_, o_ + off, [list(ap_.ap[0]), d1, d2])

        for tile_i in range(NBL):
            QG = BBC[:, RT0:RT0 + 2048].rearrange("p (a t) -> p a t", a=16)

            dma(h2t, h2tm[tile_i * 128:(tile_i + 1) * 128, :], [], [b_h2], "ldB")
            for hs in range(16):
                r0q = tile_i * 1024 + (hs // 2) * 128 + (hs % 2) * 64
                dma(QG[0:64, hs, :], QL[r0q:r0q + 64, :], [], [b_qt], "ldA")
            banks = [nps() for _ in range(4)]
            for hs in range(16):
                h, side = hs // 2, hs % 2
                pb, bpb = banks[hs // 4]
                col = (hs % 4) * 128
                mm(pb[:, col:col + 128], QG[0:64, hs, :], k12[0:64, side * 128:(side + 1) * 128], True, True,
                   [b_qt, b_k12], [bpb])
            for bi_ in range(4):
                act(WF1[:, bi_ * 512:(bi_ + 1) * 512], banks[bi_][0][:, :], AF.Copy, [banks[bi_][1]], [b_eq])
            for hs in range(16):
                bpb = b_eq
                src = WF1[:, hs * 128:hs * 128 + 128]
                va = v12[:, hs * 16:hs * 16 + 8]; vb = v12[:, hs * 16 + 8:hs * 16 + 16]
                ia_ = i12u[:, hs * 16:hs * 16 + 8]; ib_ = i12u[:, hs * 16 + 8:hs * 16 + 16]
                S.op("dve", lambda e, o=va, i=src: e.max(out=o, in_=i), [bpb], [b_v12])
                S.op("dve", lambda e, o=ia_, m=va, i=src: e.max_index(out=o, in_max=m, in_values=i), [bpb, b_v12], [b_i12u])
                S.op("dve", lambda e, o=tmpS, m=va, i=src: e.match_replace(out=o, in_to_replace=m, in_values=i, imm_value=-1e30), [bpb, b_v12], [b_tmpS])
                S.op("dve", lambda e, o=vb, i=tmpS: e.max(out=o, in_=i), [b_tmpS], [b_v12])
                S.op("dve", lambda e, o=ib_, m=vb, i=tmpS: e.max_index(out=o, in_max=m, in_values=i), [b_tmpS, b_v12], [b_i12u])
            tt(ap4(cand, 0, [256, 8], [16, 16], [1, 16]), ap4(v12, 0, [32, 8], [1, 16], [0, 16]), ap4(v12, 16, [32, 8], [0, 16], [1, 16]),
               ALU.add, [b_v12], [b_cand])
            for h in range(8):
                ch = cand[:, h * 256:(h + 1) * 256]
                va = vs[:, h * 16:h * 16 + 8]; vb = vs[:, h * 16 + 8:h * 16 + 16]
                S.op("dve", lambda e, o=va, i=ch: e.max(out=o, in_=i), [b_cand], [b_vs])
                S.op("dve", lambda e, o=icu[:, h * 16:h * 16 + 8], m=va, i=ch: e.max_index(out=o, in_max=m, in_values=i), [b_cand, b_vs], [b_icu])
                S.op("dve", lambda e, o=ch, m=va, i=ch: e.match_replace(out=o, in_to_replace=m, in_values=i, imm_value=-1e30), [b_vs], [b_cand])
                S.op("dve", lambda e, o=vb, i=ch: e.max(out=o, in_=i), [b_cand], [b_vs])
                S.op("dve", lambda e, o=icu[:, h * 16 + 8:h * 16 + 16], m=vb, i=ch: e.max_index(out=o, in_max=m, in_values=i), [b_cand, b_vs], [b_icu])
            negm = misc[:, 0:8]; sume = misc[:, 8:16]; rsum = misc[:, 16:24]
            ts(negm, bass.AP(vs.tensor, vs.offset, [list(vs.ap[0]), [16, 8]]), -1.0, None, ALU.mult, None, [b_vs], [b_misc])
            for h in range(8):
                act(ev[:, h * 16:(h + 1) * 16], vs[:, h * 16:(h + 1) * 16], AF.Exp, [b_vs, b_misc], [b_r],
                    bias=negm[:, h:h + 1])
            S.op("dve", lambda e, o=sume, i=ap3(ev, 0, [16, 8], [1, 16]): e.tensor_reduce(out=o, in_=i, axis=AX.X, op=ALU.add), [b_r], [b_misc])
            recip(rsum, sume, [b_r, b_misc], [b_misc])
            tt(ap3(gates, 0, [16, 8], [1, 16]), ap3(ev, 0, [16, 8], [1, 16]), ap3(rsum, 0, [1, 8], [0, 16]), ALU.mult, [b_r, b_misc], [b_r])
            icf = af
            cp(icf, icu[:, :], [b_icu], [b_r])
            cp(i12f, i12u[:, :], [b_i12u], [b_i12f])
            E4 = ap4(EQ, 0, [256, 8], [16, 16], [1, 16])
            E4b = ap4(EQ2, 0, [256, 8], [16, 16], [1, 16])
            icb = ap4(icf, 0, [16, 8], [1, 16], [0, 16])
            tt(E4, icb, ap4(iota16[:, :], 16, [0, 8], [0, 16], [1, 16]), ALU.is_ge, [b_r, b_iota], [b_eq])
            tt(E4b, icb, ap4(iota16[:, :], 32, [0, 8], [0, 16], [1, 16]), ALU.is_lt, [b_r, b_iota], [b_eq])
            tt(E4, E4, E4b, ALU.mult, [b_eq], [b_eq])
            tt(E4b, E4, ap4(iota16[:, :], 0, [0, 8], [0, 16], [1, 16]), ALU.mult, [b_eq, b_iota], [b_eq])
            S.op("dve", lambda e, o=bf, i=ap3(EQ2, 0, [16, 128], [1, 16]): e.tensor_reduce(out=o, in_=i, axis=AX.X, op=ALU.add), [b_eq], [b_r])
            tt(E4b, E4, ap4(i12f, 0, [32, 8], [0, 16], [1, 16]), ALU.mult, [b_eq, b_i12f], [b_eq])
            S.op("dve", lambda e, o=iaf, i=ap3(EQ2, 0, [16, 128], [1, 16]): e.tensor_reduce(out=o, in_=i, axis=AX.X, op=ALU.add), [b_eq], [b_r])
            stt(bf, bf, -16.0, icf, ALU.mult, ALU.add, [b_r], [b_r])
            tt(E4, ap4(bf, 0, [16, 8], [1, 16], [0, 16]), ap4(iota16[:, :], 0, [0, 8], [0, 16], [1, 16]), ALU.is_equal, [b_r, b_iota], [b_eq])
            tt(E4, E4, ap4(i12f, 16, [32, 8], [0, 16], [1, 16]), ALU.mult, [b_eq, b_i12f], [b_eq])
            S.op("dve", lambda e, o=ibf, i=ap3(EQ, 0, [16, 128], [1, 16]): e.tensor_reduce(out=o, in_=i, axis=AX.X, op=ALU.add), [b_eq], [b_r])
            stt(idsf, iaf, 128.0, ibf, ALU.mult, ALU.add, [b_r], [b_r])
            ts(idsf, idsf, 0.0, float(NEXP - 1), ALU.max, ALU.min, [b_r], [b_r])
            cp(idsu[:, :], idsf, [b_r], [b_ids])
            if dbg == "p5r":
                dma(dbgR[tile_i], WF[:, 0:1152], [b_r], [], "stdbg")
                continue
            for k in range(128):
                s_ = k % 4
                S.dma("pool", lambda e, o=G[:, s_, :], tb_=peer_u[l], ix=idsu[:, k:k + 1]: e.indirect_dma_start(
                    out=o, out_offset=None, in_=tb_, in_offset=bass.IndirectOffsetOnAxis(ap=ix, axis=0)),
                    [b_ids], [b_G[s_]], "g%d" % s_)
                S.op("dve", lambda e, o=G[:, s_, :], hh=h2t, ao=actv[:, k:k + 1]: e.scalar_tensor_tensor(
                    out=o, in0=o, scalar=1.0, in1=hh, op0=ALU.mult, op1=ALU.mult, accum_out=ao),
                    [b_h2, b_G[s_]], [b_G[s_], b_r])
            act(wv, actv, AF.Gelu_apprx_tanh, [b_r], [b_r])
            tt(wv, wv, gates, ALU.mult, [b_r], [b_r])
            for k in range(128):
                s_ = k % 4
                S.dma("pool", lambda e, o=G[:, s_, :], tb_=peer_v[l], ix=idsu[:, k:k + 1]: e.indirect_dma_start(
                    out=o, out_offset=None, in_=tb_, in_offset=bass.IndirectOffsetOnAxis(ap=ix, axis=0)),
                    [b_ids], [b_G[s_]], "g%d" % s_)
                if k == 0:
                    ts(acc, G[:, s_, :], wv[:, 0:1], None, ALU.mult, None, [b_G[s_], b_r], [b_acc])
                else:
                    stt(acc, G[:, s_, :], wv[:, k:k + 1], acc, ALU.mult, ALU.add, [b_G[s_], b_r, b_acc], [b_acc])
            if dbg:
                dma(dbgR[tile_i], WF[:, 0:1152], [b_r], [], "stdbg")
            dma(x1t, X1L[tile_i * D:(tile_i + 1) * D, :].rearrange("(c p) t -> p c t", p=128), [], [b_x1], "ldC")
            for c in range(32):
                if c % 4 == 0:
                    pt, bpt = nps()
                tr(pt[:, (c % 4) * 128:(c % 4) * 128 + 128], acc[:, c * 128:(c + 1) * 128], ident[:, :], [b_acc, b_ident], [bpt])
                if c % 4 == 3:
                    for c2 in range(c - 3, c + 1):
                        stt(x1t[:, c2, :], pt[:, (c2 % 4) * 128:(c2 % 4) * 128 + 128], modT[:, 160 + c2:161 + c2], x1t[:, c2, :],
                            ALU.mult, ALU.add, [bpt, b_mod, b_x1], [b_x1])
            dma(CCin[tile_i * D:(tile_i + 1) * D, :].rearrange("(c p) t -> p c t", p=128), x1t, [b_x1], [], "stx")
        S.barrier()
        b_cci = S.buf(); b_cco = [S.buf(), S.buf()]
        HR = D // 2
        for t_ in range(NBL):
            for hf in range(2):
                i_ = (t_ * 2 + hf) % 2
                src = CCin[t_ * D + hf * HR:t_ * D + (hf + 1) * HR, :]
                S.cc(lambda e, gr=groups, a=src, o=CCout[i_]: e.collective_compute("AllGather", ALU.bypass, replica_groups=gr, ins=[a], outs=[o]),
                     [b_cci], [b_cco[i_]])
                for r_ in range(GC):
                    r0 = (r_ * NBL + t_) * D + hf * HR
                    dma(XTM[r0:r0 + HR, :], CCout[i_][r_ * HR:(r_ + 1) * HR, :], [b_cco[i_]], [], "stcc")
        S.barrier()
    S.barrier()
    dma(outT, XTM, [], [S.buf()], "outcp")
    S.emit()
    return nc


def host_inputs(inp, T, depth, b, dbg=False, j=0, GC=4):
    NGL = NG // GC; NHL = NH // GC; NKL = NKV // GC
    gs = slice(j * NGL, (j + 1) * NGL); hsl = slice(j * NHL, (j + 1) * NHL)
    f = np.float32
    ca = np.ascontiguousarray
    d = {}
    NB = T // 128; NBL = NB // GC
    d["xtm_in"] = ca(inp["x"][b, :T].reshape(NB, 128, D).transpose(0, 2, 1).reshape(NB * D, 128))
    idx = host_inputs_idx(T, j, GC)
    d["xmine"] = ca(d["xtm_in"].reshape(NB, D, 128)[j * NBL:(j + 1) * NBL].reshape(NBL * D, 128))
    d["cT"] = ca(inp["c"][b].reshape(32, 128).T)
    d["w_ada"] = inp["w_ada"]
    d["b_adaT"] = ca(inp["b_ada"].reshape(192, 128).T)
    d["adaT"] = ca(inp["ada_layer"][:depth].reshape(depth, 192, 128).transpose(2, 0, 1))
    d["g1T"] = ca(inp["norm1_g"][:depth].reshape(depth, 32, 128).transpose(2, 0, 1))
    d["g2T"] = ca(inp["norm2_g"][:depth].reshape(depth, 32, 128).transpose(2, 0, 1))
    wi = inp["w_in"][:depth]
    d["w_in"] = ca(np.concatenate([wi[:, :, j * NGL * 16:(j + 1) * NGL * 16], wi[:, :, 2048 + j * NHL * 64:2048 + (j + 1) * NHL * 64],
                                   wi[:, :, 4096 + j * NKL * 64:4096 + (j + 1) * NKL * 64], wi[:, :, 4352 + j * NKL * 64:4352 + (j + 1) * NKL * 64]], axis=2))
    d["w_glu"] = inp["w_glu"][:depth]
    d["w_out"] = inp["w_out"][:depth]
    d["w_q"] = inp["peer_wq"][:depth]
    lre = inp["lam_re"][:depth, gs]; lim = inp["lam_im"][:depth, gs]; ldt = inp["log_dt"][:depth, gs]
    lreT = lre.transpose(2, 0, 1); limT = lim.transpose(2, 0, 1)
    d["lamS_re"] = ca(np.concatenate([lreT, lreT], 0))
    d["lamS_im"] = ca(np.concatenate([limT, limT], 0))
    d["ldtS"] = ca(np.broadcast_to(ldt[None], (128, depth, NGL)))
    d["lamR_re"] = ca(np.broadcast_to(lre[None], (16, depth, NGL, 64)))
    d["lamR_im"] = ca(np.broadcast_to(lim[None], (16, depth, NGL, 64)))
    d["ldtR"] = ca(np.broadcast_to(ldt[None, :, :, None], (16, depth, NGL, 64)))
    d["bT_re"] = ca(inp["b_re"][:depth, gs].transpose(3, 0, 1, 2))
    d["bT_im"] = ca(inp["b_im"][:depth, gs].transpose(3, 0, 1, 2))
    cre = inp["c_re"][:depth, gs].transpose(3, 0, 1, 2); cim = inp["c_im"][:depth, gs].transpose(3, 0, 1, 2)
    d["cW1"] = ca(np.concatenate([cre, cim], 0))
    d["cW2"] = ca(np.concatenate([cim, cre], 0))
    d["dT"] = ca(inp["d_skip"][:depth, gs].transpose(2, 0, 1))
    d["qgT"] = ca(inp["q_gain"][:depth][:, :, None])
    d["kgT"] = ca(inp["k_gain"][:depth][:, :, None])
    d["sinksB"] = ca(np.broadcast_to(inp["sinks"][:depth, hsl][None], (128, depth, NHL)))
    slopes = np.exp2(-8.0 * np.arange(1, NH + 1, dtype=np.float64) / NH)
    kk = np.arange(128)[:, None]; qq = np.arange(128)[None, :]
    dist_prev = qq + 128 - kk
    dist_cur = qq - kk
    bt = np.full((128, NH, 256), NEG, np.float64)
    for h in range(NH):
        bt[:, h, 0:128] = np.where(dist_prev < 128, -slopes[h] * dist_prev, NEG)
        bt[:, h, 128:256] = np.where(dist_cur >= 0, -slopes[h] * dist_cur, NEG)
    d["biasT"] = ca(bt[:, hsl].astype(f))
    d["gnsT"] = ca(inp["gn_ssm"][:depth].reshape(depth, 16, 128).transpose(2, 0, 1))
    d["gnaT"] = ca(inp["gn_attn"][:depth].reshape(depth, 16, 128).transpose(2, 0, 1))
    d["k12"] = ca(np.concatenate([inp["peer_k1"][:depth].transpose(2, 0, 1), inp["peer_k2"][:depth].transpose(2, 0, 1)], 0))
    nexp = 16384 if dbg in (False, "full") else 128
    for i in range(depth):
        d["peer_u%d" % i] = inp["peer_u"][i, :nexp]
        d["peer_v%d" % i] = inp["peer_v"][i, :nexp]
    d["ident"] = np.eye(128, dtype=f)
    io = np.arange(16, dtype=f)
    d["iota16"] = ca(np.broadcast_to(np.concatenate([io, 16 * io, 16 * io + 16])[None], (128, 48)))
    out = {k: np.asarray(v, dtype=f) for k, v in d.items()}
    out["idx"] = idx
    return out


def run(inp, T, depth, dbg=False, GC=4):
    nc = build_program(T, depth, dbg, GC)
    in_maps = []
    for b in range(2):
        for j in range(GC):
            m = host_inputs(inp, T, depth, b, dbg, j, GC)
            m["idx"] = host_inputs_idx(T, j, GC)
            in_maps.append(m)
    res = run_bass_kernel_spmd(nc, in_maps, core_ids=list(range(2 * GC)))
    return res.results


def host_inputs_idx(T, j, GC):
    NB = T // 128; NBL = NB // GC
    p = np.arange(128)
    idx = np.zeros((128, NBL * 48), np.uint32)
    for t in range(NBL):
        Tg = j * NBL + t
        for c in range(32):
            idx[:, t * 48 + c] = Tg * D + c * 128 + p
        for c in range(16):
            idx[:, t * 48 + 32 + c] = Tg * 2048 + c * 128 + p
    return idx


def untile(o, T):
    NB = T // 128
    return np.ascontiguousarray(o.reshape(NB, D, 128).transpose(0, 2, 1).reshape(T, D))


GCORES = 4


def kernel(**inputs):
    inp = {k: np.asarray(v) for k, v in inputs.items()}
    T = inp["x"].shape[1]
    res = run(inp, T, DEPTH, False, GCORES)
    out = np.stack([untile(res[b * GCORES]["outT"], T) for b in range(2)], 0)
    return out.astype(np.float32)
```
